# Optimizing a Trainium2 kernel written in Bass

```python
import jax, jax.numpy as jnp
from jax import lax
import numpy as np

D_MODEL = 1024
BATCH = 16
SEQ = 2048
DEPTH = 1

CHUNK = 64
D_MIX = D_MODEL
D_CONV = D_MIX // 2
CONV_GROUPS = 8
CONV_GROUP_DIM = D_CONV // CONV_GROUPS
CONV_WIDTH = 3
D_RWKV = D_MIX - D_CONV
RWKV_HEAD_DIM = 64
RWKV_HEADS = D_RWKV // RWKV_HEAD_DIM
DECAY_LORA = 64
AAA_LORA = 64
GATE_LORA = 128
N_EXPERTS = 32
TOP_K = 4
D_EXPERT = D_MODEL
SWIGLU_LIMIT = 7.0
SWIGLU_ALPHA = 1.702
NORM_EPS = 1e-5
GN_EPS = 64e-5
MOE_BLOCK = 256

COLS_CONV = 3 * D_CONV
COLS_RWKV = 3 * D_RWKV + DECAY_LORA + AAA_LORA + GATE_LORA
D_IN_PROJ = COLS_CONV + COLS_RWKV

kernel_name = 'hymba_conv_rwkv7_moe_adaln_block'


def rms_norm(x, gain):
    xf = x.astype(jnp.float32)
    y = xf * lax.rsqrt(jnp.mean(xf * xf, axis=-1, keepdims=True) + NORM_EPS)
    return (y * gain.astype(jnp.float32)).astype(x.dtype)


def modulate(h, shift, scale):
    return h * (1 + scale[:, None, :]) + shift[:, None, :]


def causal_shift(p):
    return jnp.pad(p, ((0, 0), (1, 0), (0, 0)))[:, :-1]


def short_conv_mixer(p_conv, conv_w, conv_gn):
    b_gate, c_gate, h = jnp.split(p_conv, 3, axis=-1)
    u = c_gate * h
    y = lax.conv_general_dilated(
        u, conv_w[:, None, :].astype(u.dtype), window_strides=(1,),
        padding=[(CONV_WIDTH - 1, 0)], dimension_numbers=('NWC', 'WIO', 'NWC'),
        feature_group_count=D_CONV)
    y = (b_gate * y).astype(jnp.float32)
    bn, t, _ = y.shape
    yg = y.reshape(bn, t, CONV_GROUPS, CONV_GROUP_DIM)
    yg = yg * lax.rsqrt(jnp.mean(yg * yg, axis=-1, keepdims=True) + NORM_EPS)
    return (yg.reshape(bn, t, D_CONV) * conv_gn.astype(jnp.float32)).astype(p_conv.dtype)


def rwkv7_mixer(p_rwkv, mu, w0, w_up, a0, a_up, g_up, k_k, k_a, r_k, ln_w, ln_b):
    p = p_rwkv + (causal_shift(p_rwkv) - p_rwkv) * mu
    s1, s2, s3 = D_RWKV, 2 * D_RWKV, 3 * D_RWKV
    r, k, v, dw, da, dg = jnp.split(p, [s1, s2, s3, s3 + DECAY_LORA, s3 + DECAY_LORA + AAA_LORA], axis=-1)
    w_log = -jax.nn.softplus(-(w0 + jnp.tanh(dw) @ w_up)) - 0.5
    decay = jnp.exp(-jnp.exp(w_log.astype(jnp.float32)))
    a = jax.nn.sigmoid(a0 + da @ a_up)
    g = (jax.nn.sigmoid(dg) @ g_up).astype(jnp.float32)
    bn, t, _ = r.shape

    def heads(z):
        return z.reshape(bn, t, RWKV_HEADS, RWKV_HEAD_DIM).astype(jnp.float32)

    r, k, v, a, decay = heads(r), heads(k), heads(v), heads(a), heads(decay)
    hn = (RWKV_HEADS, RWKV_HEAD_DIM)
    kk = k * k_k.reshape(hn).astype(jnp.float32)
    kk = kk / jnp.maximum(jnp.sqrt(jnp.sum(kk * kk, axis=-1, keepdims=True)), 1e-12)
    k = k * (1 + (a - 1) * k_a.reshape(hn).astype(jnp.float32))

    def step(state, inp):
        r_t, w_t, k_t, v_t, kk_t, a_t = inp
        sa = jnp.einsum('bhij,bhj->bhi', state, -kk_t)
        state = (state * w_t[:, :, None, :]
                 + sa[..., :, None] * (kk_t * a_t)[..., None, :]
                 + v_t[..., :, None] * k_t[..., None, :])
        return state, jnp.einsum('bhij,bhj->bhi', state, r_t)

    xs = tuple(jnp.moveaxis(z, 1, 0) for z in (r, decay, k, v, kk, a))
    state0 = jnp.zeros((bn, RWKV_HEADS, RWKV_HEAD_DIM, RWKV_HEAD_DIM), jnp.float32)
    _, o = lax.scan(step, state0, xs)
    o = jnp.moveaxis(o, 0, 1)
    mean = jnp.mean(o, axis=-1, keepdims=True)
    var = jnp.mean(jnp.square(o - mean), axis=-1, keepdims=True)
    o = (o - mean) * lax.rsqrt(var + GN_EPS) * ln_w.reshape(hn).astype(jnp.float32) + ln_b.reshape(hn).astype(jnp.float32)
    o = o + jnp.sum(r * k * r_k.reshape(hn).astype(jnp.float32), axis=-1, keepdims=True) * v
    return (o.reshape(bn, t, D_RWKV) * g).astype(p_rwkv.dtype)


def moe_ffn(h, router_w, router_b, w1, b1, w2, b2):
    bn, t, d = h.shape
    xf = h.reshape(-1, d)
    n_tok = xf.shape[0]
    n_rows = n_tok * TOP_K
    logits = (xf @ router_w + router_b).astype(jnp.float32)
    top_logits, top_idx = lax.top_k(logits, TOP_K)
    gates = jax.nn.softmax(top_logits, axis=-1)
    flat_e = top_idx.reshape(-1)
    flat_tok = jnp.repeat(jnp.arange(n_tok, dtype=jnp.int32), TOP_K)
    flat_g = gates.reshape(-1)
    order = jnp.argsort(flat_e)
    e_sorted, tok_sorted, g_sorted = flat_e[order], flat_tok[order], flat_g[order]
    counts = jnp.bincount(flat_e, length=N_EXPERTS)
    padded = (counts + MOE_BLOCK - 1) // MOE_BLOCK * MOE_BLOCK
    pad_end = jnp.cumsum(padded)
    pad_start = pad_end - padded
    start = jnp.cumsum(counts) - counts
    dest = pad_start[e_sorted] + jnp.arange(n_rows, dtype=jnp.int32) - start[e_sorted]
    n_blocks = -(-n_rows // MOE_BLOCK) + N_EXPERTS
    m_pad = n_blocks * MOE_BLOCK
    row_tok = jnp.zeros((m_pad,), jnp.int32).at[dest].set(tok_sorted)
    row_gate = jnp.zeros((m_pad,), jnp.float32).at[dest].set(g_sorted)
    block_expert = jnp.minimum(
        jnp.searchsorted(pad_end, jnp.arange(n_blocks, dtype=jnp.int32) * MOE_BLOCK, side='right'),
        N_EXPERTS - 1)

    def expert_block(args):
        e, toks, gts = args
        xb = xf[toks]
        hb = xb @ w1[e] + b1[e]
        x_glu = jnp.minimum(hb[:, ::2], SWIGLU_LIMIT)
        x_lin = jnp.clip(hb[:, 1::2], -SWIGLU_LIMIT, SWIGLU_LIMIT)
        act = x_glu * jax.nn.sigmoid(SWIGLU_ALPHA * x_glu) * (x_lin + 1)
        yb = act @ w2[e] + b2[e]
        return yb * gts[:, None].astype(yb.dtype)

    y_rows = lax.map(expert_block, (block_expert,
                                    row_tok.reshape(n_blocks, MOE_BLOCK),
                                    row_gate.reshape(n_blocks, MOE_BLOCK)))
    y = jax.ops.segment_sum(y_rows.reshape(m_pad, d), row_tok, num_segments=n_tok)
    return y.reshape(bn, t, d)


def setup_inputs(seed: int = 0) -> dict:
    key = jax.random.key(seed)
    ks = jax.random.split(key, 30)
    L = DEPTH

    def nrm(k, shape, s):
        return jax.random.normal(k, shape, jnp.float32) * s

    return {
        'x': nrm(ks[0], (BATCH, SEQ, D_MODEL), 1.0),
        'c': nrm(ks[1], (BATCH, D_MODEL), 1.0),
        'ada_w': nrm(ks[2], (L, D_MODEL, 6 * D_MODEL), 0.5 * D_MODEL ** -0.5),
        'ada_b': nrm(ks[3], (L, 6 * D_MODEL), 0.01),
        'norm1_g': 1.0 + nrm(ks[4], (L, D_MODEL), 0.05),
        'w_in': nrm(ks[5], (L, D_MODEL, D_IN_PROJ), D_MODEL ** -0.5),
        'conv_w': nrm(ks[6], (L, CONV_WIDTH, D_CONV), CONV_WIDTH ** -0.5),
        'conv_gn': 1.0 + nrm(ks[7], (L, D_CONV), 0.05),
        'rwkv_mu': jax.random.uniform(ks[8], (L, COLS_RWKV), jnp.float32),
        'rwkv_w0': jax.random.uniform(ks[9], (L, D_RWKV), jnp.float32, -6.0, 1.0),
        'rwkv_w_up': nrm(ks[10], (L, DECAY_LORA, D_RWKV), 0.5 * DECAY_LORA ** -0.5),
        'rwkv_a0': nrm(ks[11], (L, D_RWKV), 0.1),
        'rwkv_a_up': nrm(ks[12], (L, AAA_LORA, D_RWKV), AAA_LORA ** -0.5),
        'rwkv_g_up': nrm(ks[13], (L, GATE_LORA, D_RWKV), GATE_LORA ** -0.5),
        'rwkv_k_k': 0.85 + nrm(ks[14], (L, D_RWKV), 0.05),
        'rwkv_k_a': 1.0 + nrm(ks[15], (L, D_RWKV), 0.05),
        'rwkv_r_k': nrm(ks[16], (L, D_RWKV), 0.1),
        'rwkv_ln_w': 1.0 + nrm(ks[17], (L, D_RWKV), 0.05),
        'rwkv_ln_b': nrm(ks[18], (L, D_RWKV), 0.01),
        'w_out': nrm(ks[19], (L, D_MIX, D_MODEL), D_MIX ** -0.5),
        'norm2_g': 1.0 + nrm(ks[20], (L, D_MODEL), 0.05),
        'router_w': nrm(ks[21], (L, D_MODEL, N_EXPERTS), D_MODEL ** -0.5),
        'router_b': nrm(ks[22], (L, N_EXPERTS), 0.01),
        'exp_w1': nrm(ks[23], (L, N_EXPERTS, D_MODEL, 2 * D_EXPERT), D_MODEL ** -0.5),
        'exp_b1': nrm(ks[24], (L, N_EXPERTS, 2 * D_EXPERT), 0.01),
        'exp_w2': nrm(ks[25], (L, N_EXPERTS, D_EXPERT, D_MODEL), D_EXPERT ** -0.5),
        'exp_b2': nrm(ks[26], (L, N_EXPERTS, D_MODEL), 0.01),
        'final_g': 1.0 + nrm(ks[27], (D_MODEL,), 0.05),
    }


def reference(x, c, ada_w, ada_b, norm1_g, w_in, conv_w, conv_gn, rwkv_mu, rwkv_w0, rwkv_w_up,
              rwkv_a0, rwkv_a_up, rwkv_g_up, rwkv_k_k, rwkv_k_a, rwkv_r_k, rwkv_ln_w, rwkv_ln_b,
              w_out, norm2_g, router_w, router_b, exp_w1, exp_b1, exp_w2, exp_b2, final_g):
    h = x
    c_act = jax.nn.silu(c)
    for l in range(DEPTH):
        mod = c_act @ ada_w[l] + ada_b[l]
        sh1, sc1, gt1, sh2, sc2, gt2 = jnp.split(mod, 6, axis=-1)
        u = modulate(rms_norm(h, norm1_g[l]), sh1, sc1)
        proj = u @ w_in[l]
        y_conv = short_conv_mixer(proj[..., :COLS_CONV], conv_w[l], conv_gn[l])
        y_rwkv = rwkv7_mixer(proj[..., COLS_CONV:], rwkv_mu[l], rwkv_w0[l], rwkv_w_up[l],
                             rwkv_a0[l], rwkv_a_up[l], rwkv_g_up[l], rwkv_k_k[l], rwkv_k_a[l],
                             rwkv_r_k[l], rwkv_ln_w[l], rwkv_ln_b[l])
        mix = jnp.concatenate([y_conv, y_rwkv], axis=-1) @ w_out[l]
        h = h + gt1[:, None, :] * mix
        u2 = modulate(rms_norm(h, norm2_g[l]), sh2, sc2)
        h = h + gt2[:, None, :] * moe_ffn(u2, router_w[l], router_b[l], exp_w1[l], exp_b1[l],
                                           exp_w2[l], exp_b2[l])
    return rms_norm(h, final_g)
```

```python
import numpy as np
from contextlib import ExitStack
import concourse.bass as bass
import concourse.mybir as mybir
from concourse.bass_utils import run_bass_kernel_spmd

F32 = mybir.dt.float32
BF = mybir.dt.bfloat16
AF = mybir.ActivationFunctionType
ALU = mybir.AluOpType

D = 1024
NORM_EPS = 1e-5
GN_EPS = 64e-5
NDS = 12


class Buf:
    __slots__ = ("lw", "rd")

    def __init__(self):
        self.lw = None
        self.rd = {}


class TT:
    def __init__(self, t, nsub=0):
        self.t = t
        self.b = Buf()
        self.subs = [Buf() for _ in range(nsub)]

    def __getitem__(self, idx):
        return self.t[idx]


class _Rec:
    def __init__(self):
        self.call = None

    def __getattr__(self, name):
        def f(*a, **kw):
            self.call = (name, a, kw)
            return self
        return f


class KB:
    def __init__(self, nc):
        self.nc = nc
        self.eng = {"pe": nc.tensor, "act": nc.scalar, "dve": nc.vector, "pool": nc.gpsimd, "sp": nc.sync}
        self.sem = {n: nc.alloc_semaphore("s_" + n) for n in ["pe", "act", "dve", "pool"]}
        self.cnt = {n: 0 for n in ["pe", "act", "dve", "pool"]}
        self.waited = {}
        self.dsems = {q: [nc.alloc_semaphore("d%s%d" % (q, i)) for i in range(NDS)] for q in ("sp", "pool")}
        self.dtot = {q: [0] * NDS for q in ("sp", "pool")}
        self.dnext = {"sp": 0, "pool": 0}
        self.nid = 0
        self.rec = None
        self.last_tile = 2

    def _wait(self, eng, tok):
        if tok is None:
            return
        key, sem, val = tok
        if eng == "pe" and key == "pe":
            return
        if key == "pe":
            val = max(val, self.cnt["pe"])
        if self.waited.get((eng, key), 0) >= val:
            return
        self.waited[(eng, key)] = val
        self.eng[eng].wait_ge(sem, val)

    def _deps(self, eng, R, W):
        for b in R:
            self._wait(eng, b.lw)
        for b in W:
            self._wait(eng, b.lw)
            for t in b.rd.values():
                self._wait(eng, t)

    def _post(self, tok, R, W):
        for b in R:
            b.rd[tok[0]] = tok
        for b in W:
            b.lw = tok
            b.rd = {}

    def op(self, eng, fn, R=(), W=()):
        R = [x.b if isinstance(x, TT) else x for x in R]
        W = [x.b if isinstance(x, TT) else x for x in W]
        if self.rec is not None:
            r = _Rec()
            fn(r)
            self.rec.append(("op", eng, r.call, R, W))
            return None
        if eng == "pe":
            tid = self._pe_tile(fn)
            if tid != self.last_tile and (tid in (0, 1) or self.last_tile in (0, 1)) and self.cnt["pe"] > 0:
                self.eng["pe"].wait_ge(self.sem["pe"], self.cnt["pe"])
            self.last_tile = tid
        self._deps(eng, R, W)
        ins = fn(self.eng[eng])
        self.cnt[eng] += 1
        ins.then_inc(self.sem[eng], 1)
        tok = (eng, self.sem[eng], self.cnt[eng])
        self._post(tok, R, W)
        return tok

    def dma(self, out, in_, R=(), W=(), q="sp", indirect=None):
        R = [x.b if isinstance(x, TT) else x for x in R]
        W = [x.b if isinstance(x, TT) else x for x in W]
        if self.rec is not None:
            self.rec.append(("dma", out, in_, R, W, q, indirect))
            return None
        i = self.dnext[q]
        self.dnext[q] = (i + 1) % NDS
        sem = self.dsems[q][i]
        key = "d%s%d" % (q, i)
        if self.dtot[q][i] > 0:
            self._wait(q, (key, sem, self.dtot[q][i]))
        self._deps(q, R, W)
        if indirect is None:
            ins = self.eng[q].dma_start(out=out, in_=in_)
        else:
            kind, idx = indirect
            off = bass.IndirectOffsetOnAxis(ap=idx, axis=0)
            ins = self.eng[q].indirect_dma_start(out=out, out_offset=(off if kind == "scatter" else None), in_=in_,
                                                 in_offset=(off if kind == "gather" else None))
        self.dtot[q][i] += 16
        ins.then_inc(sem, 16)
        tok = (key, sem, self.dtot[q][i])
        self._post(tok, R, W)
        return tok

    def _pe_tile(self, fn):
        r = _Rec()
        fn(r)
        name, a, kw = r.call
        ap = kw.get("in_") if name == "transpose" else (kw.get("lhsT") if "lhsT" in kw else (a[1] if len(a) > 1 else None))
        if ap is None:
            return 2
        st, sz = ap.start_partition(), ap.partition_size()
        if sz > 64:
            return 2
        return 0 if st < 64 else 1

    def begin_rec(self):
        self.rec = []

    def end_rec(self):
        r, self.rec = self.rec, None
        return r

    def _play1(self, it):
        if it[0] == "op":
            _, eng, (name, a, kw), R, W = it
            self.op(eng, lambda e: getattr(e, name)(*a, **kw), R, W)
        else:
            _, out, in_, R, W, q, indirect = it
            self.dma(out, in_, R, W, q, indirect)

    def play_merged(self, X, Y=()):
        nx, ny = len(X), len(Y)
        ix = iy = 0
        def is_pe(it):
            return it[0] == "op" and it[1] == "pe"
        while ix < nx or iy < ny:
            if iy >= ny or (ix < nx and ix * ny <= iy * nx):
                self._play1(X[ix]); ix += 1
                while ix < nx and is_pe(X[ix - 1]) and is_pe(X[ix]):
                    self._play1(X[ix]); ix += 1
            else:
                self._play1(Y[iy]); iy += 1
                while iy < ny and is_pe(Y[iy - 1]) and is_pe(Y[iy]):
                    self._play1(Y[iy]); iy += 1

    def sb(self, name, shape, dt=F32, nsub=0, scope=None):
        self.nid += 1
        nm = "%s_%d" % (name, self.nid)
        if scope is None:
            return TT(self.nc.alloc_sbuf_tensor(nm, list(shape), dt), nsub)
        return TT(scope.enter_context(self.nc.sbuf_tensor(nm, list(shape), dt)), nsub)

    def barrier(self):
        names = ["pe", "act", "dve", "pool", "sp"]
        for e in names:
            for n in ["pe", "act", "dve", "pool"]:
                if n != e and self.cnt[n] > 0:
                    self._wait(e, (n, self.sem[n], self.cnt[n]))
            for q in ("sp", "pool"):
                for i in range(NDS):
                    if self.dtot[q][i] > 0:
                        self._wait(e, ("d%s%d" % (q, i), self.dsems[q][i], self.dtot[q][i]))

    def finish(self):
        for q in ("sp", "pool"):
            for i in range(NDS):
                if self.dtot[q][i] > 0:
                    self.eng["sp"].wait_ge(self.dsems[q][i], self.dtot[q][i])
        for n in ["pe", "act", "dve", "pool"]:
            if self.cnt[n] > 0:
                self.eng["sp"].wait_ge(self.sem[n], self.cnt[n])


KSTOP = 0
GVAR = 0
PMODE = 1


def build(T, NB, E, GM=256, dbg=None):
    nc = bass.Bass("TRN2", target_bir_lowering=False)
    k = KB(nc)
    NT = T // 128
    NG = T // GM
    TPG = GM // 128
    EG = min(512, T)

    def din(name, shape):
        return nc.dram_tensor(name, list(shape), F32, kind="ExternalInput").ap()

    x_d = din("x", [NB, T, D])
    cT_d = din("cT", [128, 8, NB])
    adaw_d = din("ada_w", [D, 6 * D])
    adab_fm_d = din("ada_b_fm", [128, 48])
    adab_row_d = din("ada_b_row", [1, 6 * D])
    g12_d = din("g12_fm", [128, 2, 8])
    win_d = din("w_in", [D, 3328])
    convw_d = din("conv_w_fm", [128, 4, 3])
    convgn_d = din("conv_gn_fm", [128, 4])
    mu_d = din("mu_fm", [128, 14])
    vecs_d = din("vecs_fm", [128, 7, 4])
    lora_d = din("lora_up", [128, 512])
    gup_d = din("g_up", [128, 512])
    wout_d = din("w_out", [D, D])
    rw_d = din("router_w", [D, 32])
    rb_d = din("router_b_bc", [128, 32])
    w1_d = din("w1", [E * D, 2 * D])
    b1_d = din("b1_rows", [E * 128, 16])
    w2_d = din("w2", [E * D, D])
    b2_d = din("b2", [E, D])
    NTC = NB * (T // 128)
    BLK = 512
    NBLK = (NTC * 128 * 4) // BLK + 32
    bs_d = din("blkstart", [128, NBLK])
    iop_d = din("iotaP", [128, 8])
    xs_d = nc.dram_tensor("xs_scr", [NBLK * BLK, D], BF, kind="Internal").ap()
    yb_d = nc.dram_tensor("yb_scr", [NBLK * BLK, D], F32, kind="Internal").ap()
    fg_d = din("final_g_bc", [128, D])
    cm_d = din("cmats", [128, 7, 128])
    rm_d = din("resetmask", [128, GM])
    cm2_d = din("cmats2", [128, 4, 256])
    sel_d = din("selb", [NB, NB, 128])
    ones_d = din("ones_row", [1, NB])
    g2row_d = din("g2_row", [NB, D])
    modscr_d = nc.dram_tensor("modscr", [NB, 4, D], F32, kind="Internal").ap()
    out_d = nc.dram_tensor("out", [NB, T, D], F32, kind="ExternalOutput").ap()
    h1_d = nc.dram_tensor("h1_scratch", [NB, T, D], F32, kind=("ExternalOutput" if dbg else "Internal")).ap()

    banks = [TT(nc.alloc_psum_tensor("psb%d" % i, [128, 512], F32)) for i in range(6)]
    PSX = ExitStack()
    banks += [TT(PSX.enter_context(nc.psum_tensor("psbx%d" % i, [128, 512], F32))) for i in range(2)]
    bbanks = []
    bstate = {"n": 0, "b": 0, "pool": None, "nx": 0, "ny": 0}

    reserved = set()

    def psum():
        if bstate["pool"] == "X":
            bstate["nx"] += 1
            return banks[bstate["nx"] % 6]
        if bstate["pool"] == "Y":
            bstate["ny"] += 1
            return banks[6 + bstate["ny"] % 2]
        while True:
            i = bstate["n"] % len(banks)
            bstate["n"] += 1
            if i not in reserved:
                return banks[i]

    def psum_bf():
        b = bbanks[bstate["b"] % 2]
        bstate["b"] += 1
        return b

    def mm(ps, out_ap, pairs, R):
        n = len(pairs)
        tok = None
        for i, (l, r) in enumerate(pairs):
            tok = k.op("pe", lambda e, l=l, r=r, i=i: e.matmul(out_ap, l, r, start=(i == 0), stop=(i == n - 1)),
                       R=R, W=[ps])
        return tok

    P0 = ExitStack()
    MX = ExitStack()
    ME = ExitStack()
    cm = k.sb("cm", [128, 7, 128])
    k.dma(cm[:], cm_d[:, :, :], W=[cm])
    ident, m_su, m_sl, m_ui, bones = (cm[:, i, :] for i in range(5))
    cmb = k.sb("cmb", [128, 128], BF)
    k.op("dve", lambda e: e.tensor_copy(out=cmb[:], in_=ident), R=[cm], W=[cmb])
    smallw = k.sb("smallw", [128, 48 + 16 + 12 + 4 + 14 + 28])
    o = 0
    adab_fm = smallw[:, o:o + 48]; k.dma(adab_fm, adab_fm_d[:, :], W=[smallw]); o += 48
    g12 = smallw[:, o:o + 16]; k.dma(g12, g12_d.rearrange("p a b -> p (a b)"), W=[smallw]); o += 16
    cw = smallw[:, o:o + 12]; k.dma(cw, convw_d.rearrange("p a b -> p (a b)"), W=[smallw]); o += 12
    cgn = smallw[:, o:o + 4]; k.dma(cgn, convgn_d[:, :], W=[smallw]); o += 4
    mu = smallw[:, o:o + 14]; k.dma(mu, mu_d[:, :], W=[smallw]); o += 14
    vecs = smallw[:, o:o + 28]; k.dma(vecs, vecs_d.rearrange("p a b -> p (a b)"), W=[smallw]); o += 28

    def vec(v, i):
        return vecs[:, v * 4 + i:v * 4 + i + 1]

    rw = k.sb("rw", [128, 8, 32]); k.dma(rw[:], rw_d.rearrange("(c p) n -> p c n", p=128), W=[rw])
    rb = k.sb("rb", [128, 32]); k.dma(rb[:], rb_d[:, :], W=[rb])
    cT = k.sb("cT", [128, 8 * NB]); k.dma(cT[:], cT_d.rearrange("p a b -> p (a b)"), W=[cT])

    small = k.sb("small", [128, 64])
    gtb = [[None, None] for _ in range(NB)]
    GS = [k.sb("GS", [128, 32]) for _ in range(NB)]
    nstat = [k.sb("nstat", [128, 4]) for _ in range(2)]
    xn = [k.sb("xn", [128, D])]
    junk = k.sb("junk", [128, D], BF)
    cact = k.sb("cact", [128, 8 * NB])
    csg = k.sb("csg", [128, 8 * NB])
    modfm = [k.sb("modfm", [128, 48]) for _ in range(NB)]
    winb = k.sb("winb", [128, 8, 3328], BF, scope=MX)
    woutb = k.sb("woutb", [128, 8, D], BF, scope=MX)
    rmask = k.sb("rmask", [128, GM], scope=MX)
    k.dma(rmask[:], rm_d[:, :], W=[rmask])
    lora = k.sb("lora", [128, 512], scope=MX); k.dma(lora[:], lora_d[:, :], W=[lora])
    gup = k.sb("gup", [128, 512], scope=MX); k.dma(gup[:], gup_d[:, :], W=[gup])
    gt1t = k.sb("gtb1", [128, D], scope=MX)
    selb = k.sb("selb", [NB, NB, 128], scope=P0)
    k.dma(selb[:], sel_d[:, :, :], W=[selb])
    stage = [k.sb("stage", [128, 2048], scope=P0) for _ in range(2)]
    sidx = {"n": 0}

    def next_stage():
        s = stage[sidx["n"] % 2]
        sidx["n"] += 1
        return s
    awbs = [k.sb("awb", [128, 8, 512], scope=P0) for _ in range(2)]
    modrow = k.sb("modrow", [NB, 4, D], scope=P0)
    adabr = k.sb("adabr", [NB, 4, D], scope=P0)
    g2row = k.sb("g2row", [NB, D], scope=P0)
    k.dma(g2row[:], g2row_d[:, :], W=[g2row])

    k.op("act", lambda e: e.activation(out=csg[:], in_=cT[:], func=AF.Sigmoid), R=[cT], W=[csg])
    k.op("dve", lambda e: e.tensor_tensor(out=cact[:], in0=cT[:], in1=csg[:], op=ALU.mult), R=[cT, csg], W=[cact])
    ps_fm = psum()
    reserved.add(banks.index(ps_fm))
    for blk in range(12):
        awb = awbs[blk % 2]
        for h in range(4):
            st = next_stage()
            k.dma(st[:, 0:1024].rearrange("p (c n) -> p c n", c=2),
                  adaw_d[h * 256:(h + 1) * 256, blk * 512:(blk + 1) * 512].rearrange("(c p) n -> p c n", p=128),
                  W=[st])
            k.op("act", lambda e, st=st, h=h, awb=awb: e.activation(
                out=awb[:, 2 * h:2 * h + 2, :], in_=st[:, 0:1024].rearrange("p (c n) -> p c n", c=2), func=AF.Copy),
                R=[st], W=[awb])
        for jj in range(4):
            j = blk * 4 + jj
            mm(ps_fm, ps_fm[:, j * NB:(j + 1) * NB],
               [(awb[:, kc, jj * 128:(jj + 1) * 128], cact[:, kc * NB:(kc + 1) * NB]) for kc in range(8)],
               R=[awb, cact])
        if blk in (4, 5, 6, 7, 8, 9, 10, 11):
            ps_r = psum()
            pairs = [(cact[:, kc * NB:(kc + 1) * NB], awb[:, kc, :]) for kc in range(8)]
            mm(ps_r, ps_r[0:NB, :], pairs, R=[awb, cact])
            which = {4: 0, 5: 0, 10: 1, 11: 1, 6: 2, 7: 2, 8: 3, 9: 3}[blk]
            half = blk % 2
            k.op("act", lambda e, ps_r=ps_r, which=which, half=half: e.activation(
                out=modrow[:, which, half * 512:(half + 1) * 512], in_=ps_r[0:NB, :], func=AF.Copy),
                R=[ps_r], W=[modrow])
    for b in range(NB):
        k.op("dve", lambda e, b=b: e.tensor_tensor(out=modfm[b][:], in0=ps_fm[:, b:48 * NB:NB], in1=adab_fm, op=ALU.add),
             R=[ps_fm, smallw], W=[modfm[b]])
    reserved.clear()
    for b in range(NB):
        for which, c0 in ((0, 2 * D), (1, 5 * D), (2, 3 * D), (3, 4 * D)):
            k.dma(adabr[b:b + 1, which, :], adab_row_d[0:1, c0:c0 + D], W=[adabr])
    k.op("dve", lambda e: e.tensor_tensor(out=modrow[:], in0=modrow[:], in1=adabr[:], op=ALU.add),
         R=[adabr], W=[modrow])
    k.op("dve", lambda e: e.scalar_tensor_tensor(out=modrow[:, 3, :], in0=modrow[:, 3, :], scalar=1.0, in1=g2row[:],
                                                 op0=ALU.add, op1=ALU.mult), R=[g2row], W=[modrow])
    k.dma(modscr_d[:, :, :], modrow[:, 0:4, :], R=[modrow])
    for b in range(NB):
        for n, (sc0, sh0) in enumerate(((8, 0), (32, 24))):
            k.op("dve", lambda e, b=b, n=n, sc0=sc0: e.scalar_tensor_tensor(
                out=GS[b][:, n * 16:n * 16 + 8], in0=modfm[b][:, sc0:sc0 + 8], scalar=1.0,
                in1=g12[:, n * 8:(n + 1) * 8], op0=ALU.add, op1=ALU.mult), R=[modfm[b], smallw], W=[GS[b]])
            k.op("dve", lambda e, b=b, n=n, sh0=sh0: e.tensor_copy(
                out=GS[b][:, n * 16 + 8:n * 16 + 16], in_=modfm[b][:, sh0:sh0 + 8]), R=[modfm[b]], W=[GS[b]])

    nctr = {"n": 0}

    def rstd_of(src_ap, srcbuf):
        i = nctr["n"] % 2
        nctr["n"] += 1
        st = nstat[i]
        k.op("act", lambda e: e.activation(out=junk[:], in_=src_ap, func=AF.Square, accum_out=st[:, 0:1]),
             R=[srcbuf], W=[junk, st])
        k.op("dve", lambda e: e.tensor_scalar(out=st[:, 1:2], in0=st[:, 0:1], scalar1=1.0 / D, scalar2=NORM_EPS,
                                              op0=ALU.mult, op1=ALU.add), R=[st], W=[st])
        k.op("act", lambda e: e.activation(out=st[:, 2:3], in_=st[:, 1:2], func=AF.Sqrt), R=[st], W=[st])
        k.op("dve", lambda e: e.reciprocal(out=st[:, 3:4], in_=st[:, 2:3]), R=[st], W=[st])
        return st, i

    def norm_T(src_ap, srcbuf, gs, goff, evac):
        st, i = rstd_of(src_ap, srcbuf)
        i = 0
        k.op("dve", lambda e: e.tensor_scalar(out=xn[i][:], in0=src_ap, scalar1=st[:, 3:4], scalar2=None, op0=ALU.mult),
             R=[srcbuf, st], W=[xn[i]])
        for hb in range(2):
            ps = psum()
            for c in range(4):
                kc = hb * 4 + c
                k.op("pe", lambda e, ps=ps, c=c, kc=kc: e.transpose(out=ps[:, c * 128:(c + 1) * 128],
                                                                     in_=xn[i][:, kc * 128:(kc + 1) * 128], identity=ident),
                     R=[xn[i], cm], W=[ps])
            for c in range(4):
                kc = hb * 4 + c
                evac(kc, ps[:, c * 128:(c + 1) * 128], gs[:, goff + kc:goff + kc + 1], gs[:, goff + 8 + kc:goff + 9 + kc], ps)

    for kc in range(8):
        for half in range(2):
            st = next_stage()
            k.dma(st[:, 0:1664], win_d[kc * 128:(kc + 1) * 128, half * 1664:(half + 1) * 1664], W=[st])
            k.op("act" if half else "dve", lambda e, st=st, kc=kc, half=half: (
                e.activation(out=winb[:, kc, half * 1664:(half + 1) * 1664], in_=st[:, 0:1664], func=AF.Copy) if half else
                e.tensor_copy(out=winb[:, kc, half * 1664:(half + 1) * 1664], in_=st[:, 0:1664])), R=[st], W=[winb])
    for kc in range(8):
        st = next_stage()
        k.dma(st[:, 0:D], wout_d[kc * 128:(kc + 1) * 128, :], W=[st])
        k.op("act", lambda e, st=st, kc=kc: e.activation(out=woutb[:, kc, :], in_=st[:, 0:D], func=AF.Copy), R=[st], W=[woutb])

    k.barrier()
    P0.close()
    def ksb(*a, **kw):
        return k.sb(*a, scope=MX, **kw)
    xgs = [ksb("xg", [128, TPG, D], nsub=TPG) for _ in range(2)]
    uT = ksb("uT", [128, 8, GM], BF)
    mixTs = [ksb("mixT", [128, 8, GM], BF, nsub=8) for _ in range(2)]
    pbuf = [ksb("pbuf", [128, GM + 1]) for _ in range(14)]
    pl12, pl13 = ksb("pl12", [128, GM]), ksb("pl13", [128, GM])
    rkv = [[ksb("rkv", [128, GM]) for _ in range(3)] for _ in range(2)]
    ggs = [ksb("gg", [128, GM]) for _ in range(2)]
    bonuss = [ksb("bonus", [128, GM]) for _ in range(2)]
    PLs = [ksb("PL", [128, 2 * TPG]) for _ in range(2)]
    ucv = [ksb("ucv", [128, GM + 2]) for _ in range(4)]
    NTMP = 26
    tmps = [ksb("tmp", [128, GM]) for _ in range(NTMP)]
    Rtbs = [ksb("Rtb", [128, GM], BF) for _ in range(2)]
    TTts = [[ksb("TTt", [128, 512], BF) for _ in range(TPG)] for _ in range(2)]
    padss = [[[ksb("pads", [128, 3, 128], BF) for _ in range(TPG)] for _ in range(2)] for _ in range(2)]
    UTpad = [ksb("UTpad", [128, 128], BF) for _ in range(2)]
    Xs = [[ksb("Xs", [128, 2, 128], BF) for _ in range(2)] for _ in range(TPG)]
    XTs = [[ksb("XTs", [128, 2, 128], BF) for _ in range(2)] for _ in range(TPG)]
    Tms = [[ksb("Tms", [128, 2, 128], BF) for _ in range(2)] for _ in range(TPG)]
    MakT = [ksb("MakT", [128, 2, 128], BF) for _ in range(TPG)]
    MT = [ksb("MT", [128, 2, 128], BF) for _ in range(TPG)]
    Mbr = [ksb("Mbr", [128, 2, 128], BF) for _ in range(TPG)]
    Mkr = [ksb("Mkr", [128, 2, 128], BF) for _ in range(TPG)]
    Atbs = [ksb("Atb", [128, GM], BF) for _ in range(2)]
    Btbs = [ksb("Btb", [128, GM], BF) for _ in range(2)]
    Ktbs = [ksb("Ktb", [128, GM], BF) for _ in range(2)]
    Sdec = ksb("Sdec", [128, 128])
    cm2 = ksb("cm2", [128, 4, 256])
    k.dma(cm2[:], cm2_d[:, :, :], W=[cm2])
    mk2_su, mk2_sl, mk2_ui, mk2_id = (cm2[:, i_, :] for i_ in range(4))
    Ah = [ksb("Ah", [128, 128], BF) for _ in range(TPG)]
    WT2 = [ksb("WT2", [128, 128]) for _ in range(TPG)]
    STf = [ksb("STf", [128, 128]) for _ in range(4)]
    STb = [ksb("STb", [128, 128], BF) for _ in range(4)]

    for hh in range(2):
        for tb in range(TPG):
            for par_ in range(2):
                k.op("dve", lambda e: e.memset(padss[par_][hh][tb][:], 0.0), W=[padss[par_][hh][tb]])
        k.op("dve", lambda e, hh=hh: e.memset(UTpad[hh][:], 0.0), W=[UTpad[hh]])

    def proj(j):
        ps = psum()
        mm(ps, ps[:, 0:GM], [(winb[:, kc, j * 128:(j + 1) * 128], uT[:, kc, :]) for kc in range(8)], R=[winb, uT])
        return ps

    def bsum(src, srcbuf):
        ps = psum()
        mm(ps, ps[:, 0:GM], [(bones, src)], R=[cm, srcbuf])
        return ps

    def rsqrt_ps(ps, scale, eps, dst):
        k.op("dve", lambda e: e.tensor_scalar(out=dst[:], in0=ps[:, 0:GM], scalar1=scale, scalar2=eps, op0=ALU.mult, op1=ALU.add),
             R=[ps], W=[dst])
        k.op("act", lambda e: e.activation(out=dst[:], in_=dst[:], func=AF.Sqrt), R=[dst], W=[dst])
        k.op("dve", lambda e: e.reciprocal(out=dst[:], in_=dst[:]), R=[dst], W=[dst])

    def stage_U(b_, g_):
        xg_ = xgs[(b_ * NG + g_) % 2]
        for tt in range(TPG):
            k.dma(xg_[:, tt, :], x_d[b_, g_ * GM + tt * 128:g_ * GM + (tt + 1) * 128, :], W=[xg_.subs[tt]])

            def ev(kc, psap, sc, bi, ps, tt=tt):
                k.op("act", lambda e: e.activation(out=uT[:, kc, tt * 128:(tt + 1) * 128], in_=psap, func=AF.Identity,
                                                   scale=sc, bias=bi), R=[ps, GS[b_]], W=[uT])
            norm_T(xg_[:, tt, :], xg_.subs[tt], GS[b_], 0, ev)

    h1ctr = {"n": 0}
    def hist_reset():
        for q in range(14):
            k.op("dve", lambda e: e.memset(pbuf[q][:, 0:1], 0.0), W=[pbuf[q]])
        for ci in range(4):
            k.op("dve", lambda e: e.memset(ucv[ci][:, 0:2], 0.0), W=[ucv[ci]])

    for b in range(NB):
        for g in range(NG):
            t0 = g * GM
            n_cur = b * NG + g
            xg = xgs[n_cur % 2]
            mixT = mixTs[n_cur % 2]

            def conv_branch(mixT):
              for ci in range(4):
                  tB, tC, t1, yb, ysq, rr = tmps[6 * (ci % 2) + 3:6 * (ci % 2) + 9]
                  ps = proj(ci)
                  k.op("act", lambda e, ps=ps: e.activation(out=tB[:], in_=ps[:, 0:GM], func=AF.Copy), R=[ps], W=[tB])
                  ps = proj(4 + ci)
                  k.op("act", lambda e, ps=ps: e.activation(out=tC[:], in_=ps[:, 0:GM], func=AF.Copy), R=[ps], W=[tC])
                  ps = proj(8 + ci)
                  u = ucv[ci]
                  k.op("dve", lambda e, ps=ps: e.tensor_tensor(out=u[:, 2:2 + GM], in0=tC[:], in1=ps[:, 0:GM], op=ALU.mult),
                       R=[tC, ps], W=[u])
                  k.op("dve", lambda e: e.tensor_scalar(out=t1[:], in0=u[:, 0:GM], scalar1=cw[:, ci * 3:ci * 3 + 1], scalar2=None,
                                                        op0=ALU.mult), R=[u, smallw], W=[t1])
                  for kk_ in (1, 2):
                      k.op("dve", lambda e, kk_=kk_: e.scalar_tensor_tensor(out=t1[:], in0=u[:, kk_:kk_ + GM],
                                                                             scalar=cw[:, ci * 3 + kk_:ci * 3 + kk_ + 1], in1=t1[:],
                                                                             op0=ALU.mult, op1=ALU.add), R=[u, smallw], W=[t1])
                  k.op("dve", lambda e: e.tensor_tensor(out=yb[:], in0=tB[:], in1=t1[:], op=ALU.mult), R=[tB, t1], W=[yb])
                  k.op("act", lambda e: e.activation(out=ysq[:], in_=yb[:], func=AF.Square), R=[yb], W=[ysq])
                  psn = bsum(ysq[:], ysq)
                  rsqrt_ps(psn, 1.0 / 64, NORM_EPS, rr)
                  k.op("dve", lambda e: e.scalar_tensor_tensor(out=mixT[:, ci, :], in0=yb[:], scalar=cgn[:, ci:ci + 1], in1=rr[:],
                                                               op0=ALU.mult, op1=ALU.mult), R=[yb, rr, smallw], W=[mixT.subs[ci]])
                  k.op("dve", lambda e: e.tensor_copy(out=u[:, 0:2], in_=u[:, GM:GM + 2]), R=[], W=[u])
            def lerp_tile(q, dst):
                ps = proj(12 + q)
                pb = pbuf[q]
                d_ = tmps[24 + (q % 2)]
                k.op("act", lambda e: e.activation(out=pb[:, 1:GM + 1], in_=ps[:, 0:GM], func=AF.Copy), R=[ps], W=[pb])
                k.op("dve", lambda e: e.tensor_tensor(out=d_[:], in0=pb[:, 0:GM], in1=pb[:, 1:GM + 1], op=ALU.subtract),
                     R=[pb], W=[d_])
                k.op("dve", lambda e: e.scalar_tensor_tensor(out=dst[:], in0=d_[:], scalar=mu[:, q:q + 1], in1=pb[:, 1:GM + 1],
                                                             op0=ALU.mult, op1=ALU.add), R=[d_, pb, smallw], W=[dst])
                k.op("dve", lambda e: e.tensor_copy(out=pb[:, 0:1], in_=pb[:, GM:GM + 1]), R=[], W=[pb])
            th, sgm = tmps[1], tmps[2]

            def front(n_):
                b_, g_ = divmod(n_, NG)
                if g_ == 0:
                    hist_reset()
                stage_U(b_, g_)
                conv_branch(mixTs[n_ % 2])
                lerp_tile(12, pl12)
                lerp_tile(13, pl13)
                k.op("act", lambda e: e.activation(out=th[0:64, :], in_=pl12[0:64, :], func=AF.Tanh), R=[pl12], W=[th])
                k.op("act", lambda e: e.activation(out=sgm[:], in_=pl13[:], func=AF.Sigmoid), R=[pl13], W=[sgm])
                tile_A1(0, 0)

            def tile_A1(i, par):
                (ld, av, kk, kksq, rinv, kkn, tq, kmod, prod, lcum, Pinc, Pexc, Pinv, At, Bt, Kt, Rt) = tmps[3:20]
                gg, bonus, PL = ggs[par], bonuss[par], PLs[par]
                Atb, Btb, Ktb, Rtb, TTt, pads = Atbs[par], Btbs[par], Ktbs[par], Rtbs[par], TTts[par], padss[par]
                rl, kl, vl = rkv[par]
                lerp_tile(i, rl)
                lerp_tile(4 + i, kl)
                lerp_tile(8 + i, vl)
                cs = slice(i * 128, (i + 1) * 128)
                ps = psum()
                mm(ps, ps[:, 0:GM], [(lora[0:64, cs], th[0:64, :])], R=[lora, th])
                k.op("act", lambda e, ps=ps: e.activation(out=ld[:], in_=ps[:, 0:GM], func=AF.Sigmoid, bias=vec(0, i), scale=1.0),
                     R=[ps, smallw], W=[ld])
                k.op("dve", lambda e: e.tensor_scalar(out=ld[:], in0=ld[:], scalar1=-0.6065306597126334, scalar2=None, op0=ALU.mult),
                     R=[], W=[ld])
                ps = psum()
                mm(ps, ps[:, 0:GM], [(lora[64:128, cs], pl12[64:128, :])], R=[lora, pl12])
                k.op("act", lambda e, ps=ps: e.activation(out=av[:], in_=ps[:, 0:GM], func=AF.Sigmoid, bias=vec(1, i), scale=1.0),
                     R=[ps, smallw], W=[av])
                ps = psum()
                mm(ps, ps[:, 0:GM], [(gup[:, cs], sgm[:])], R=[gup, sgm])
                k.op("act", lambda e, ps=ps: e.activation(out=gg[:], in_=ps[:, 0:GM], func=AF.Copy), R=[ps], W=[gg])
                k.op("dve", lambda e: e.tensor_scalar(out=kk[:], in0=kl[:], scalar1=vec(2, i), scalar2=None, op0=ALU.mult),
                     R=[kl, smallw], W=[kk])
                k.op("act", lambda e: e.activation(out=kksq[:], in_=kk[:], func=AF.Square), R=[kk], W=[kksq])
                psn = bsum(kksq[:], kksq)
                k.op("act", lambda e, psn=psn: e.activation(out=rinv[:], in_=psn[:, 0:GM], func=AF.Sqrt), R=[psn], W=[rinv])
                k.op("dve", lambda e: e.tensor_scalar(out=rinv[:], in0=rinv[:], scalar1=1e-12, scalar2=None, op0=ALU.max), R=[], W=[rinv])
                k.op("dve", lambda e: e.reciprocal(out=rinv[:], in_=rinv[:]), R=[], W=[rinv])
                k.op("dve", lambda e: e.tensor_tensor(out=kkn[:], in0=kk[:], in1=rinv[:], op=ALU.mult), R=[kk, rinv], W=[kkn])
                k.op("dve", lambda e: e.tensor_scalar(out=tq[:], in0=av[:], scalar1=-1.0, scalar2=vec(3, i), op0=ALU.add, op1=ALU.mult),
                     R=[av, smallw], W=[tq])
                k.op("dve", lambda e: e.scalar_tensor_tensor(out=kmod[:], in0=tq[:], scalar=1.0, in1=kl[:], op0=ALU.add, op1=ALU.mult),
                     R=[tq, kl], W=[kmod])
                k.op("dve", lambda e: e.scalar_tensor_tensor(out=prod[:], in0=rl[:], scalar=vec(4, i), in1=kmod[:], op0=ALU.mult,
                                                             op1=ALU.mult), R=[rl, kmod, smallw], W=[prod])
                psrk = bsum(prod[:], prod)
                k.op("dve", lambda e, psrk=psrk: e.tensor_tensor(out=bonus[:], in0=psrk[:, 0:GM], in1=vl[:], op=ALU.mult),
                     R=[psrk, vl], W=[bonus])
                k.op("dve", lambda e: e.tensor_tensor_scan(out=lcum[:], data0=rmask[:], data1=ld[:], initial=0.0, op0=ALU.mult,
                                                           op1=ALU.add), R=[rmask, ld], W=[lcum])
                k.op("act", lambda e: e.activation(out=Pinc[:], in_=lcum[:], func=AF.Exp), R=[lcum], W=[Pinc])
                k.op("dve", lambda e: e.tensor_tensor(out=Pexc[:], in0=lcum[:], in1=ld[:], op=ALU.subtract), R=[lcum, ld], W=[Pexc])
                k.op("act", lambda e: e.activation(out=Pexc[:], in_=Pexc[:], func=AF.Exp), R=[], W=[Pexc])
                k.op("act", lambda e: e.activation(out=Pinv[:], in_=lcum[:], func=AF.Exp, scale=-1.0), R=[lcum], W=[Pinv])
                k.op("dve", lambda e: e.scalar_tensor_tensor(out=At[:], in0=kkn[:], scalar=-1.0, in1=Pexc[:], op0=ALU.mult, op1=ALU.mult),
                     R=[kkn, Pexc], W=[At])
                k.op("dve", lambda e: e.tensor_tensor(out=Bt[:], in0=kkn[:], in1=av[:], op=ALU.mult), R=[kkn, av], W=[Bt])
                k.op("dve", lambda e: e.tensor_tensor(out=Bt[:], in0=Bt[:], in1=Pinv[:], op=ALU.mult), R=[Pinv], W=[Bt])
                k.op("dve", lambda e: e.tensor_tensor(out=Kt[:], in0=kmod[:], in1=Pinv[:], op=ALU.mult), R=[kmod, Pinv], W=[Kt])
                k.op("dve", lambda e: e.tensor_tensor(out=Rt[:], in0=rl[:], in1=Pinc[:], op=ALU.mult), R=[rl, Pinc], W=[Rt])
                k.op("act", lambda e: e.activation(out=Rtb[:], in_=Rt[:], func=AF.Copy), R=[Rt], W=[Rtb])
                for src_, dst_ in ((At, Atb), (Bt, Btb), (Kt, Ktb)):
                    k.op("pool", lambda e: e.tensor_copy(out=dst_[:], in_=src_[:]), R=[src_], W=[dst_])
                for tb in range(TPG):
                    cl = slice(tb * 128, (tb + 1) * 128)
                    ps = psum()
                    for n_, src in enumerate((At, Bt, Kt, vl)):
                        k.op("pe", lambda e: e.transpose(out=ps[:, n_ * 128:(n_ + 1) * 128], in_=src[:, cl], identity=ident),
                             R=[src, cm], W=[ps])
                    k.op("act", lambda e: e.activation(out=TTt[tb][:], in_=ps[:, :], func=AF.Copy), R=[ps], W=[TTt[tb]])
                    for hh in range(2):
                        k.op("dve" if hh else "act", lambda e: (e.tensor_copy(
                            out=pads[hh][tb][:, :, hh * 64:hh * 64 + 64],
                            in_=TTt[tb][:, 128:512].rearrange("p (a c) -> p a c", a=3)[:, :, hh * 64:hh * 64 + 64]) if hh else
                            e.activation(out=pads[hh][tb][:, :, hh * 64:hh * 64 + 64],
                                         in_=TTt[tb][:, 128:512].rearrange("p (a c) -> p a c", a=3)[:, :, hh * 64:hh * 64 + 64],
                                         func=AF.Copy)),
                            R=[TTt[tb]], W=[pads[hh][tb]])
                k.op("act", lambda e: e.activation(out=PL[:], in_=Pinc[:, 63:GM:64], func=AF.Copy), R=[Pinc], W=[PL])

            def tile_X(i, par):
                Osb, dd, dsq, rs = tmps[20:24]
                gg, bonus, PL = ggs[par], bonuss[par], PLs[par]
                Atb, Btb, Ktb, Rtb, TTt, pads = Atbs[par], Btbs[par], Ktbs[par], Rtbs[par], TTts[par], padss[par]
                for tb in range(TPG):
                    cl = slice(tb * 128, (tb + 1) * 128)
                    specs = [(Btb, Atb, mk2_su, Xs[tb][0]), (Atb, Btb, mk2_sl, XTs[tb][0]), (Atb, Ktb, mk2_sl, MakT[tb]),
                             (Btb, Rtb, mk2_ui, Mbr[tb]), (Ktb, Rtb, mk2_ui, Mkr[tb])]
                    gps = [psum() for _ in specs]
                    for hh in range(2):
                        pr = slice(hh * 64, hh * 64 + 64)
                        for (l, r, mask2, dst), ps in zip(specs, gps):
                            mm(ps, ps[:, hh * 128:(hh + 1) * 128], [(l[pr, cl], r[pr, cl])], R=[l, r])
                    for (l, r, mask2, dst), ps in zip(specs, gps):
                        k.op("dve", lambda e: e.tensor_tensor(out=dst[:].rearrange("p a b -> p (a b)"), in0=ps[:, 0:256], in1=mask2,
                                                              op=ALU.mult), R=[ps, cm2], W=[dst])
                    k.op("dve", lambda e: e.tensor_tensor(out=Tms[tb][0][:].rearrange("p a b -> p (a b)"),
                                                          in0=Xs[tb][0][:].rearrange("p a b -> p (a b)"), in1=mk2_id, op=ALU.add),
                         R=[Xs[tb][0], cm2], W=[Tms[tb][0]])
                cur = 0
                for lvl in range(1, 6):
                    nxt = 1 - cur
                    pss = {}
                    for tb in range(TPG):
                        if lvl < 5:
                            ps = psum()
                            for hh in range(2):
                                mm(ps, ps[:, hh * 128:(hh + 1) * 128], [(XTs[tb][cur][:, hh, :], Xs[tb][cur][:, hh, :])],
                                   R=[XTs[tb][cur], Xs[tb][cur]])
                            pss[(tb, 0)] = ps
                        ps = psum()
                        for hh in range(2):
                            mm(ps, ps[:, hh * 128:(hh + 1) * 128], [(Xs[tb][cur][:, hh, :], XTs[tb][cur][:, hh, :])],
                               R=[XTs[tb][cur], Xs[tb][cur]])
                        pss[(tb, 1)] = ps
                    for tb in range(TPG):
                        if lvl < 5:
                            ps = pss[(tb, 0)]
                            k.op("act", lambda e: e.activation(out=Xs[tb][nxt][:].rearrange("p a b -> p (a b)"), in_=ps[:, 0:256],
                                                               func=AF.Copy), R=[ps], W=[Xs[tb][nxt]])
                        ps = pss[(tb, 1)]
                        k.op("act", lambda e: e.activation(out=XTs[tb][nxt][:].rearrange("p a b -> p (a b)"), in_=ps[:, 0:256],
                                                           func=AF.Copy), R=[ps], W=[XTs[tb][nxt]])
                    for tb in range(TPG):
                        ps = psum()
                        for hh in range(2):
                            mm(ps, ps[:, hh * 128:(hh + 1) * 128], [(XTs[tb][nxt][:, hh, :], Tms[tb][cur][:, hh, :])],
                               R=[XTs[tb][nxt], Tms[tb][cur]])
                        pss[(tb, 2)] = ps
                    for tb in range(TPG):
                        ps = pss[(tb, 2)]
                        k.op("dve", lambda e: e.tensor_tensor(out=Tms[tb][nxt][:].rearrange("p a b -> p (a b)"), in0=ps[:, 0:256],
                                                              in1=Tms[tb][cur][:].rearrange("p a b -> p (a b)"), op=ALU.add),
                             R=[ps, Tms[tb][cur]], W=[Tms[tb][nxt]])
                    cur = nxt
                pA, pM = {}, {}
                for tb in range(TPG):
                    ps = psum()
                    for hh in range(2):
                        mm(ps, ps[:, hh * 128:(hh + 1) * 128], [(TTt[tb][:, 0:128], Tms[tb][cur][:, hh, :])], R=[TTt[tb], Tms[tb][cur]])
                    pA[tb] = ps
                    ps = psum()
                    for hh in range(2):
                        mm(ps, ps[:, hh * 128:(hh + 1) * 128], [(MakT[tb][:, hh, :], Tms[tb][cur][:, hh, :])], R=[MakT[tb], Tms[tb][cur]])
                    pM[tb] = ps
                for tb in range(TPG):
                    for hh in range(2):
                        pr = slice(hh * 64, hh * 64 + 64)
                        k.op("act", lambda e: e.activation(out=Ah[tb][pr, :], in_=pA[tb][pr, hh * 128:(hh + 1) * 128], func=AF.Copy),
                             R=[pA[tb]], W=[Ah[tb]])
                    k.op("act", lambda e: e.activation(out=MT[tb][:].rearrange("p a b -> p (a b)"), in_=pM[tb][:, 0:256], func=AF.Copy),
                         R=[pM[tb]], W=[MT[tb]])
                for tb in range(TPG):
                    ps = psum()
                    for hh in range(2):
                        mm(ps, ps[:, hh * 64:(hh + 1) * 64], [(MT[tb][:, hh, :], TTt[tb][:, 384 + hh * 64:384 + hh * 64 + 64])],
                           R=[MT[tb], TTt[tb]])
                    k.op("act", lambda e: e.activation(out=WT2[tb][:], in_=ps[:, 0:128], func=AF.Copy), R=[ps], W=[WT2[tb]])
                for tb in range(TPG):
                    for c in range(2):
                        ro = slice(64 * c, 64 * c + 64)
                        cc0 = tb * 128 + 64 * c
                        k.op("dve", lambda e: e.tensor_scalar(out=Sdec[:], in0=STf[i][:], scalar1=PL[:, 2 * tb + c:2 * tb + c + 1], scalar2=None,
                                                              op0=ALU.mult), R=[STf[i], PL], W=[Sdec])
                        ps = psum()
                        mm(ps, ps[:, 0:128], [(Ah[tb][:], STb[i][:])], R=[Ah[tb], STb[i]])
                        for hh in range(2):
                            hs = slice(hh * 64, hh * 64 + 64)
                            k.op("dve", lambda e: e.tensor_tensor(out=UTpad[hh][ro, hs], in0=ps[ro, hs], in1=WT2[tb][ro, hs], op=ALU.add),
                                 R=[ps, WT2[tb]], W=[UTpad[hh]])
                        pss_ = psum()
                        mm(pss_, pss_[:, 0:128],
                           [(pads[0][tb][ro, 0, :], UTpad[0][ro, :]), (pads[1][tb][ro, 0, :], UTpad[1][ro, :]),
                            (pads[0][tb][ro, 1, :], pads[0][tb][ro, 2, :]), (pads[1][tb][ro, 1, :], pads[1][tb][ro, 2, :])],
                           R=[pads[0][tb], pads[1][tb], UTpad[0], UTpad[1]])
                        k.op("dve", lambda e: e.scalar_tensor_tensor(out=STf[i][:], in0=pss_[:, 0:128], scalar=PL[:, 2 * tb + c:2 * tb + c + 1],
                                                                     in1=Sdec[:], op0=ALU.mult, op1=ALU.add),
                             R=[pss_, PL, Sdec], W=[STf[i]])
                        pso = psum()
                        mm(pso, pso[:, 0:64],
                           [(STb[i][:], Rtb[:, cc0:cc0 + 64]),
                            (UTpad[0][ro, :], Mbr[tb][ro, 0, ro]), (UTpad[1][ro, :], Mbr[tb][ro, 1, ro]),
                            (pads[0][tb][ro, 2, :], Mkr[tb][ro, 0, ro]), (pads[1][tb][ro, 2, :], Mkr[tb][ro, 1, ro])],
                           R=[STb[i], Rtb, UTpad[0], UTpad[1], Mbr[tb], Mkr[tb], pads[0][tb], pads[1][tb]])
                        k.op("act", lambda e: e.activation(out=STb[i][:], in_=STf[i][:], func=AF.Copy), R=[STf[i]], W=[STb[i]])
                        k.op("act", lambda e: e.activation(out=Osb[:, cc0:cc0 + 64], in_=pso[:, 0:64], func=AF.Copy), R=[pso], W=[Osb])
                psm = bsum(Osb[:], Osb)
                k.op("dve", lambda e, psm=psm: e.scalar_tensor_tensor(out=dd[:], in0=psm[:, 0:GM], scalar=-1.0 / 64, in1=Osb[:],
                                                                        op0=ALU.mult, op1=ALU.add), R=[psm, Osb], W=[dd])
                k.op("act", lambda e: e.activation(out=dsq[:], in_=dd[:], func=AF.Square), R=[dd], W=[dsq])
                psv = bsum(dsq[:], dsq)
                rsqrt_ps(psv, 1.0 / 64, GN_EPS, rs)
                k.op("dve", lambda e: e.tensor_tensor(out=dd[:], in0=dd[:], in1=rs[:], op=ALU.mult), R=[rs], W=[dd])
                k.op("dve", lambda e: e.tensor_scalar(out=dd[:], in0=dd[:], scalar1=vec(5, i), scalar2=vec(6, i), op0=ALU.mult, op1=ALU.add),
                     R=[smallw], W=[dd])
                k.op("dve", lambda e: e.tensor_tensor(out=dd[:], in0=dd[:], in1=bonus[:], op=ALU.add), R=[bonus], W=[dd])
                k.op("dve", lambda e: e.tensor_tensor(out=mixT[:, 4 + i, :], in0=dd[:], in1=gg[:], op=ALU.mult), R=[dd, gg],
                     W=[mixT.subs[4 + i]])
            if n_cur == 0:
                front(0)
            if g == 0:
                k.dma(gt1t[:], modscr_d[b, 0, :].partition_broadcast(128), W=[gt1t])
                for ci in range(4):
                    k.op("dve", lambda e: e.memset(STf[ci][:], 0.0), W=[STf[ci]])
                    k.op("dve", lambda e: e.memset(STb[ci][:], 0.0), W=[STb[ci]])
            for i in range(4):
                bstate["pool"] = "X"
                k.begin_rec(); tile_X(i, i % 2); X_ = k.end_rec()
                bstate["pool"] = "Y"
                k.begin_rec()
                if i < 3:
                    tile_A1(i + 1, (i + 1) % 2)
                elif n_cur + 1 < NB * NG:
                    front(n_cur + 1)
                Y_ = k.end_rec()
                bstate["pool"] = None
                if PMODE == 0:
                    k.play_merged(X_)
                    k.play_merged(Y_)
                else:
                    k.play_merged(X_, Y_)
            for tt in range(TPG):
                for half in range(2):
                    hsl = slice(half * 512, (half + 1) * 512)
                    ps = psum()
                    mm(ps, ps[:, :], [(mixT[:, c_, tt * 128:(tt + 1) * 128], woutb[:, c_, hsl]) for c_ in range(8)],
                       R=[woutb] + mixT.subs)
                    k.op("dve", lambda e, ps=ps, hsl=hsl: e.tensor_tensor(out=ps[:, :], in0=ps[:, :], in1=gt1t[:, hsl],
                                                                          op=ALU.mult), R=[gt1t], W=[ps])
                    k.op("dve", lambda e, ps=ps, hsl=hsl, tt=tt: e.tensor_tensor(out=xg[:, tt, hsl], in0=ps[:, :], in1=xg[:, tt, hsl],
                                                                                 op=ALU.add), R=[ps], W=[xg.subs[tt]])
                k.dma(h1_d[b, t0 + tt * 128:t0 + (tt + 1) * 128, :], xg[:, tt, :], R=[xg.subs[tt]], q="pool")

    if dbg == "h1":
        k.finish()
        return nc

    k.barrier()
    MX.close()
    PSX.close()
    del banks[6:]
    bbanks.extend(TT(nc.alloc_psum_tensor("psbb%d" % i, [128, 1024], BF)) for i in range(2))
    ME_P = ExitStack()
    I32 = mybir.dt.int32
    ltri_f, ones_f = cm[:, 5, :], cm[:, 6, :]

    def psb(*a, **kw):
        return k.sb(*a, scope=ME_P, **kw)
    cmb2 = psb("cmb2", [128, 2, 128], BF)
    k.op("dve", lambda e: e.tensor_copy(out=cmb2[:], in_=cm[:, 5:7, :]), R=[cm], W=[cmb2])
    lgs = psb("lgs", [128, NTC, 32])
    ranks = psb("ranks", [128, NTC, 32])
    m8s = psb("m8s", [128, NTC, 8])
    g4 = psb("g4", [128, NTC, 4])
    dest_f = psb("dest_f", [128, NTC * 4])
    dest_i = psb("dest_i", [128, NTC * 4], I32)
    cnt = psb("cnt", [128, 32])
    lay = psb("lay", [128, 5, 32])
    bs = psb("bs", [128, NBLK]); k.dma(bs[:], bs_d[:, :], W=[bs])
    iop = psb("iop", [128, 8]); k.dma(iop[:], iop_d[:, :], W=[iop])
    be = psb("be", [128, 2, NBLK])
    idxW_f = psb("idxW_f", [128, NBLK, 8])
    idxW_i = psb("idxW_i", [128, NBLK * 8], I32)
    idxb_f = psb("idxb_f", [128, 2, NBLK])
    idxb_i = psb("idxb_i", [128, 2 * NBLK], I32)
    k.op("dve", lambda e: e.memset(cnt[:], 0.0), W=[cnt])
    k.op("dve", lambda e: e.memset(dest_f[:], 0.0), W=[dest_f])

    ME_R = ExitStack()

    def rsb(*a, **kw):
        return k.sb(*a, scope=ME_R, **kw)
    u2tm = rsb("u2tm", [128, NTC, D], BF, nsub=NTC)
    u2f = rsb("u2f", [128, 8, 128])
    h2 = [rsb("h2", [128, D]) for _ in range(2)]
    G2bc = [rsb("G2bc", [128, D]) for _ in range(NB)]
    SH2bc = [rsb("SH2bc", [128, D]) for _ in range(NB)]
    maskb = rsb("maskb", [128, 32], BF)
    rt_ = rsb("rt", [128, 64])
    for b in range(NB):
        k.dma(SH2bc[b][:], modscr_d[b, 2, :].partition_broadcast(128), W=[SH2bc[b]])
        k.dma(G2bc[b][:], modscr_d[b, 3, :].partition_broadcast(128), W=[G2bc[b]])
    for tile in range(NTC):
        b, tt = tile // NT, tile % NT
        ht = h2[tile % 2]
        k.dma(ht[:], h1_d[b, tt * 128:(tt + 1) * 128, :], W=[ht])

        def ev2(kc, psap, sc, bi, ps):
            k.op("act", lambda e: e.activation(out=u2f[:, kc, :], in_=psap, func=AF.Identity, scale=sc, bias=bi),
                 R=[ps, GS[b]], W=[u2f])
        norm_T(ht[:], ht, GS[b], 16, ev2)
        k.op("dve", lambda e: e.tensor_tensor(out=ht[:], in0=xn[0][:], in1=G2bc[b][:], op=ALU.mult), R=[xn[0], G2bc[b]], W=[ht])
        k.op("dve", lambda e: e.tensor_tensor(out=u2tm[:, tile, :], in0=ht[:], in1=SH2bc[b][:], op=ALU.add),
             R=[ht, SH2bc[b]], W=[u2tm.subs[tile]])
        ps = psum()
        mm(ps, ps[:, 0:32], [(u2f[:, kc, :], rw[:, kc, :]) for kc in range(8)], R=[u2f, rw])
        lg = lgs[:, tile, :]
        m8 = m8s[:, tile, :]
        k.op("dve", lambda e: e.tensor_tensor(out=lg, in0=ps[:, 0:32], in1=rb[:], op=ALU.add), R=[ps, rb], W=[lgs])
        k.op("dve", lambda e: e.max(out=m8, in_=lg), R=[lgs], W=[m8s])
        k.op("dve", lambda e: e.tensor_scalar(out=maskb[:], in0=lg, scalar1=m8s[:, tile, 3:4], scalar2=None, op0=ALU.is_ge),
             R=[lgs, m8s], W=[maskb])
        psr = psum()
        mm(psr, psr[:, 0:32], [(cmb2[:, 0, :], maskb[:])], R=[cmb2, maskb])
        k.op("dve", lambda e: e.tensor_tensor(out=ranks[:, tile, :], in0=psr[:, 0:32], in1=cnt[:], op=ALU.add),
             R=[psr, cnt], W=[ranks])
        psc = psum()
        mm(psc, psc[:, 0:32], [(cmb2[:, 1, :], maskb[:])], R=[cmb2, maskb])
        k.op("dve", lambda e: e.tensor_tensor(out=cnt[:], in0=psc[:, 0:32], in1=cnt[:], op=ALU.add), R=[psc], W=[cnt])
        negm, ssum, ex4 = rt_[:, 0:1], rt_[:, 1:2], rt_[:, 4:8]
        k.op("dve", lambda e: e.tensor_scalar(out=negm, in0=m8s[:, tile, 0:1], scalar1=-1.0, scalar2=None, op0=ALU.mult),
             R=[m8s], W=[rt_])
        k.op("act", lambda e: e.activation(out=ex4, in_=m8s[:, tile, 0:4], func=AF.Exp, bias=negm, scale=1.0), R=[m8s], W=[rt_])
        k.op("dve", lambda e: e.tensor_reduce(out=ssum, in_=ex4, axis=mybir.AxisListType.X, op=ALU.add), R=[], W=[rt_])
        k.op("dve", lambda e: e.reciprocal(out=ssum, in_=ssum), R=[], W=[rt_])
        k.op("dve", lambda e: e.tensor_scalar(out=g4[:, tile, :], in0=ex4, scalar1=ssum, scalar2=None, op0=ALU.mult),
             R=[rt_], W=[g4])
    nbt, padded, pend, pstart, ones32 = (lay[:, i, :] for i in range(5))
    k.op("dve", lambda e: e.memset(ones32, 1.0), W=[lay])
    k.op("dve", lambda e: e.tensor_scalar(out=nbt, in0=cnt[:], scalar1=0.0, scalar2=None, op0=ALU.is_gt), R=[cnt], W=[lay])
    for j in range(1, (NTC * 128) // BLK):
        k.op("dve", lambda e: e.scalar_tensor_tensor(out=nbt, in0=cnt[:], scalar=float(BLK * j), in1=nbt, op0=ALU.is_gt,
                                                     op1=ALU.add), R=[cnt], W=[lay])
    k.op("dve", lambda e: e.tensor_scalar(out=padded, in0=nbt, scalar1=float(BLK), scalar2=None, op0=ALU.mult), R=[], W=[lay])
    k.op("dve", lambda e: e.tensor_tensor_scan(out=pend, data0=ones32, data1=padded, initial=0.0, op0=ALU.mult, op1=ALU.add),
         R=[], W=[lay])
    k.op("dve", lambda e: e.tensor_tensor(out=pstart, in0=pend, in1=padded, op=ALU.subtract), R=[], W=[lay])
    k.op("dve", lambda e: e.memset(be[:, 0, :], 0.0), W=[be])
    for e_ in range(32):
        k.op("dve", lambda e: e.scalar_tensor_tensor(out=be[:, 0, :], in0=bs[:], scalar=lay[:, 2, e_:e_ + 1], in1=be[:, 0, :],
                                                     op0=ALU.is_ge, op1=ALU.add), R=[bs, lay], W=[be])
    k.op("dve", lambda e: e.tensor_scalar(out=be[:, 0, :], in0=be[:, 0, :], scalar1=float(E - 1), scalar2=None, op0=ALU.min),
         R=[], W=[be])
    k.op("dve", lambda e: e.tensor_scalar(out=be[:, 1, :], in0=be[:, 0, :], scalar1=float(D), scalar2=None, op0=ALU.mult),
         R=[], W=[be])
    for kc in range(8):
        k.op("dve", lambda e: e.tensor_scalar(out=idxW_f[:, :, kc], in0=be[:, 1, :], scalar1=iop[:, kc:kc + 1], scalar2=None,
                                              op0=ALU.add), R=[be, iop], W=[idxW_f])
    k.op("dve", lambda e: e.tensor_copy(out=idxW_i[:], in_=idxW_f[:].rearrange("p a b -> p (a b)")), R=[idxW_f], W=[idxW_i])
    k.op("dve", lambda e: e.tensor_scalar(out=idxb_f[:, 0, :], in0=be[:, 0, :], scalar1=128.0, scalar2=iop[:, 0:1],
                                          op0=ALU.mult, op1=ALU.add), R=[be, iop], W=[idxb_f])
    k.op("dve", lambda e: e.tensor_copy(out=idxb_f[:, 1, :], in_=be[:, 0, :]), R=[be], W=[idxb_f])
    k.op("dve", lambda e: e.tensor_copy(out=idxb_i[:], in_=idxb_f[:].rearrange("p a b -> p (a b)")), R=[idxb_f], W=[idxb_i])
    for tile in range(NTC):
        k.op("dve", lambda e: e.tensor_tensor(out=ranks[:, tile, :], in0=ranks[:, tile, :], in1=pstart, op=ALU.add),
             R=[lay], W=[ranks])
        for kk_ in range(4):
            k.op("dve", lambda e: e.scalar_tensor_tensor(out=rt_[:, 32:64], in0=lgs[:, tile, :], scalar=m8s[:, tile, kk_:kk_ + 1],
                                                         in1=ranks[:, tile, :], op0=ALU.is_equal, op1=ALU.mult,
                                                         accum_out=dest_f[:, tile * 4 + kk_:tile * 4 + kk_ + 1]),
                 R=[lgs, m8s, ranks], W=[rt_, dest_f])
    k.op("dve", lambda e: e.tensor_copy(out=dest_i[:], in_=dest_f[:]), R=[dest_f], W=[dest_i])
    if dbg == "moe":
        dbg_d = nc.dram_tensor("dbg", [128, NTC * 4 + NTC * 4 + 32 + NBLK], F32, kind="ExternalOutput").ap()
        k.dma(dbg_d[:, 0:NTC * 4], dest_f[:], R=[dest_f])
        k.dma(dbg_d[:, NTC * 4:NTC * 8], g4[:].rearrange("p a b -> p (a b)"), R=[g4])
        k.dma(dbg_d[:, NTC * 8:NTC * 8 + 32], cnt[:], R=[cnt])
        k.dma(dbg_d[:, NTC * 8 + 32:NTC * 8 + 32 + NBLK], be[:, 0, :], R=[be])
    for tile in range(NTC):
        for kk_ in range(4):
            c_ = tile * 4 + kk_
            k.dma(xs_d[:, :], u2tm[:, tile, :], R=[u2tm.subs[tile], dest_i], q="pool", indirect=("scatter", dest_i[:, c_:c_ + 1]))
    k.barrier()
    ME_R.close()

    ME_X = ExitStack()

    def xsb(*a, **kw):
        return k.sb(*a, scope=ME_X, **kw)
    W1b = [xsb("W1b", [128, 8, 2 * D], BF, nsub=8) for _ in range(2)]
    W2b = xsb("W2b", [128, 8, D], BF, nsub=8)
    xrow = [xsb("xrow", [128, 4, D], BF) for _ in range(2)]
    xsT = [xsb("xsT", [128, 8, BLK], BF, nsub=8) for _ in range(2)]
    actT = [xsb("actT", [128, 8, BLK], BF, nsub=8) for _ in range(2)]
    et = [[xsb("et", [128, BLK]) for _ in range(4)] for _ in range(2)]
    yrow = [xsb("yrow", [128, D]) for _ in range(2)]
    b2bc = [xsb("b2bc", [128, D]) for _ in range(2)]
    b1blk = [xsb("b1blk", [128, 16]) for _ in range(2)]
    ectr = {"n": 0, "y": 0}
    def load_weights(blk):
        w1 = W1b[blk % 2]
        b1t, b2t = b1blk[blk % 2], b2bc[blk % 2]
        k.dma(b1t[:], b1_d[:, :], R=[idxb_i], W=[b1t], q="pool", indirect=("gather", idxb_i[:, blk:blk + 1]))
        k.dma(b2t[:], b2_d[:, :], R=[idxb_i], W=[b2t], q="pool", indirect=("gather", idxb_i[:, NBLK + blk:NBLK + blk + 1]))
        for kc in range(8):
            c_ = blk * 8 + kc
            k.dma(w1[:, kc, :], w1_d[:, :], R=[idxW_i], W=[w1.subs[kc]], q="pool", indirect=("gather", idxW_i[:, c_:c_ + 1]))

    def load_w2(blk):
        for fc in range(8):
            c_ = blk * 8 + fc
            k.dma(W2b[:, fc, :], w2_d[:, :], R=[idxW_i], W=[W2b.subs[fc]], q="pool", indirect=("gather", idxW_i[:, c_:c_ + 1]))

    def load_x(blk):
        k.dma(xrow[blk % 2][:], xs_d[blk * BLK:(blk + 1) * BLK, :].rearrange("(s p) d -> p s d", p=128), W=[xrow[blk % 2]])

    def transposes(blk):
        xT = xsT[blk % 2]
        xr = xrow[blk % 2]
        for kc in range(8):
            if kc % 2 == 0:
                pb = psum_bf()
            po = (kc % 2) * 512
            for s_ in range(4):
                k.op("pe", lambda e: e.transpose(out=pb[:, po + s_ * 128:po + (s_ + 1) * 128], in_=xr[:, s_, kc * 128:(kc + 1) * 128],
                                                 identity=cmb[:]), R=[xr, cmb], W=[pb])
            k.op("act", lambda e: e.activation(out=xT[:, kc, :], in_=pb[:, po:po + 512], func=AF.Copy), R=[pb], W=[xT.subs[kc]])

    load_weights(0)
    load_x(0)
    transposes(0)
    for blk in range(NBLK):
        w1 = W1b[blk % 2]
        b1t, b2t = b1blk[blk % 2], b2bc[blk % 2]
        load_w2(blk)
        if blk + 1 < NBLK:
            load_weights(blk + 1)
            load_x(blk + 1)
        xT = xsT[blk % 2]
        aT = actT[blk % 2]
        for fc in range(8):
            xg_, sg_, xl_, tt_ = et[ectr["n"] % 2]
            ectr["n"] += 1
            psg = psum()
            mm(psg, psg[:, :], [(w1[:, kc, fc * 128:(fc + 1) * 128], xT[:, kc, :]) for kc in range(8)], R=w1.subs + xT.subs)
            psl = psum()
            mm(psl, psl[:, :], [(w1[:, kc, D + fc * 128:D + (fc + 1) * 128], xT[:, kc, :]) for kc in range(8)], R=w1.subs + xT.subs)
            k.op("dve", lambda e: e.tensor_scalar(out=xg_[:], in0=psg[:, :], scalar1=b1t[:, fc:fc + 1], scalar2=7.0,
                                                  op0=ALU.add, op1=ALU.min), R=[psg, b1t], W=[xg_])
            k.op("act", lambda e: e.activation(out=sg_[:], in_=xg_[:], func=AF.Sigmoid, scale=1.702), R=[xg_], W=[sg_])
            k.op("dve", lambda e: e.tensor_scalar(out=xl_[:], in0=psl[:, :], scalar1=b1t[:, 8 + fc:9 + fc], scalar2=7.0,
                                                  op0=ALU.add, op1=ALU.min), R=[psl, b1t], W=[xl_])
            k.op("dve", lambda e: e.tensor_scalar(out=xl_[:], in0=xl_[:], scalar1=-7.0, scalar2=1.0, op0=ALU.max, op1=ALU.add),
                 R=[], W=[xl_])
            k.op("dve", lambda e: e.tensor_tensor(out=tt_[:], in0=xg_[:], in1=sg_[:], op=ALU.mult), R=[xg_, sg_], W=[tt_])
            k.op("dve", lambda e: e.tensor_tensor(out=aT[:, fc, :], in0=tt_[:], in1=xl_[:], op=ALU.mult), R=[tt_, xl_],
                 W=[aT.subs[fc]])
        if blk + 1 < NBLK:
            transposes(blk + 1)
        for s_ in range(4):
            yr = yrow[ectr["y"] % 2]
            ectr["y"] += 1
            for half in range(2):
                hsl = slice(half * 512, (half + 1) * 512)
                ps = psum()
                mm(ps, ps[:, :], [(aT[:, fc, s_ * 128:(s_ + 1) * 128], W2b[:, fc, hsl]) for fc in range(8)], R=aT.subs + W2b.subs)
                k.op("dve", lambda e: e.tensor_tensor(out=yr[:, hsl], in0=ps[:, :], in1=b2t[:, hsl], op=ALU.add), R=[ps, b2t], W=[yr])
            k.dma(yb_d[blk * BLK + s_ * 128:blk * BLK + (s_ + 1) * 128, :], yr[:], R=[yr])
    k.barrier()
    ME_X.close()

    ME_C = ExitStack()

    def csb(*a, **kw):
        return k.sb(*a, scope=ME_C, **kw)
    ygat = [[csb("ygat", [128, D]) for _ in range(4)] for _ in range(2)]
    h3 = [csb("h3", [128, D]) for _ in range(2)]
    fg = csb("fg", [128, D]); k.dma(fg[:], fg_d[:, :], W=[fg])
    for b in range(NB):
        gtb[b][1] = csb("gtb2", [128, D])
        k.dma(gtb[b][1][:], modscr_d[b, 1, :].partition_broadcast(128), W=[gtb[b][1]])
    for tile in range(NTC):
        b, tt = tile // NT, tile % NT
        ht = h3[tile % 2]
        yg = ygat[tile % 2]
        for kk_ in range(4):
            c_ = tile * 4 + kk_
            k.dma(yg[kk_][:], yb_d[:, :], R=[dest_i], W=[yg[kk_]], q="pool", indirect=("gather", dest_i[:, c_:c_ + 1]))
        k.dma(ht[:], h1_d[b, tt * 128:(tt + 1) * 128, :], W=[ht])
        k.op("dve", lambda e: e.tensor_scalar(out=yg[0][:], in0=yg[0][:], scalar1=g4[:, tile, 0:1], scalar2=None, op0=ALU.mult),
             R=[g4], W=[yg[0]])
        for kk_ in range(1, 4):
            k.op("dve", lambda e: e.scalar_tensor_tensor(out=yg[0][:], in0=yg[kk_][:], scalar=g4[:, tile, kk_:kk_ + 1], in1=yg[0][:],
                                                         op0=ALU.mult, op1=ALU.add), R=[yg[kk_], g4], W=[yg[0]])
        if dbg == "moe":
            k.dma(out_d[b, tt * 128:(tt + 1) * 128, :], yg[0][:], R=[yg[0]])
            continue
        k.op("dve", lambda e: e.tensor_tensor(out=yg[0][:], in0=yg[0][:], in1=gtb[b][1][:], op=ALU.mult), R=[gtb[b][1]], W=[yg[0]])
        k.op("dve", lambda e: e.tensor_tensor(out=ht[:], in0=ht[:], in1=yg[0][:], op=ALU.add), R=[yg[0]], W=[ht])
        st, i = rstd_of(ht[:], ht)
        k.op("dve", lambda e: e.scalar_tensor_tensor(out=ht[:], in0=ht[:], scalar=st[:, 3:4], in1=fg[:], op0=ALU.mult, op1=ALU.mult),
             R=[st, fg], W=[ht])
        k.dma(out_d[b, tt * 128:(tt + 1) * 128, :], ht[:], R=[ht])
    k.finish()
    return nc


def make_consts(NB, GM):
    idx = np.arange(128)
    same = (idx[:, None] // 64) == (idx[None, :] // 64)
    ident = np.eye(128, dtype=np.float32)
    m_su = (same & (idx[:, None] < idx[None, :])).astype(np.float32)
    m_sl = (same & (idx[:, None] > idx[None, :])).astype(np.float32)
    m_ui = (same & (idx[:, None] <= idx[None, :])).astype(np.float32)
    bones = same.astype(np.float32)
    ltri = (idx[:, None] < idx[None, :]).astype(np.float32)
    cm = np.ascontiguousarray(np.stack([ident, m_su, m_sl, m_ui, bones, ltri, np.ones((128, 128), np.float32)], axis=1))
    cm2 = np.ascontiguousarray(np.stack([np.concatenate([m, m], axis=1) for m in (m_su, m_sl, m_ui, ident)], axis=1))
    rm = np.ones((128, GM), np.float32)
    rm[:, ::64] = 0.0
    selb = np.zeros((NB, NB, 128), np.float32)
    for b in range(NB):
        selb[b, b, :] = 1.0
    return cm, rm, selb, np.ones((1, NB), np.float32), cm2


def fm(v, n):
    return np.ascontiguousarray(np.asarray(v, np.float32).reshape(n, 128).T)


def prep_shared(inp, NB, GM, E, T=2048):
    f = lambda a: np.asarray(a, np.float32)
    cm, rm, selb, ones, cm2 = make_consts(NB, GM)
    w1 = f(inp["exp_w1"])[0][:E]
    w1 = np.ascontiguousarray(np.concatenate([w1[:, :, 0::2], w1[:, :, 1::2]], axis=2))
    b1 = f(inp["exp_b1"])[0][:E]
    b1d = np.concatenate([b1[:, 0::2], b1[:, 1::2]], axis=1)
    b1_rows = np.ascontiguousarray(b1d.reshape(E, 16, 128).transpose(0, 2, 1).reshape(E * 128, 16))
    NBLK = (NB * T * 4) // 512 + 32
    blkstart = np.ascontiguousarray(np.broadcast_to((np.arange(NBLK, dtype=np.float32) * 512.0)[None, :], (128, NBLK)))
    iotaP = np.ascontiguousarray((np.arange(8, dtype=np.float32)[None, :] * 128.0 + np.arange(128, dtype=np.float32)[:, None]))
    vecs = np.stack([fm(f(inp[n])[0], 4) for n in
                     ("rwkv_w0", "rwkv_a0", "rwkv_k_k", "rwkv_k_a", "rwkv_r_k", "rwkv_ln_w", "rwkv_ln_b")], axis=1)
    sh = {
        "ada_w": np.ascontiguousarray(f(inp["ada_w"])[0]),
        "ada_b_fm": fm(f(inp["ada_b"])[0], 48),
        "ada_b_row": np.ascontiguousarray(f(inp["ada_b"])[0][None, :]),
        "g12_fm": np.ascontiguousarray(np.stack([fm(f(inp["norm1_g"])[0], 8), fm(f(inp["norm2_g"])[0], 8)], axis=1)),
        "w_in": np.ascontiguousarray(f(inp["w_in"])[0]),
        "conv_w_fm": np.ascontiguousarray(f(inp["conv_w"])[0].reshape(3, 4, 128).transpose(2, 1, 0)),
        "conv_gn_fm": fm(f(inp["conv_gn"])[0], 4),
        "mu_fm": fm(f(inp["rwkv_mu"])[0], 14),
        "vecs_fm": np.ascontiguousarray(vecs),
        "lora_up": np.ascontiguousarray(np.concatenate([f(inp["rwkv_w_up"])[0], f(inp["rwkv_a_up"])[0]], axis=0)),
        "g_up": np.ascontiguousarray(f(inp["rwkv_g_up"])[0]),
        "w_out": np.ascontiguousarray(f(inp["w_out"])[0]),
        "router_w": np.ascontiguousarray(f(inp["router_w"])[0][:, :32]),
        "router_b_bc": np.ascontiguousarray(np.broadcast_to(f(inp["router_b"])[0][None, :32], (128, 32))),
        "w1": w1.reshape(E * D, 2 * D),
        "b1_rows": b1_rows,
        "blkstart": blkstart, "iotaP": iotaP,
        "g2_row": np.ascontiguousarray(np.broadcast_to(f(inp["norm2_g"])[0][None, :], (NB, D))),
        "w2": np.ascontiguousarray(f(inp["exp_w2"])[0][:E]).reshape(E * D, D),
        "b2": np.ascontiguousarray(f(inp["exp_b2"])[0][:E]),
        "final_g_bc": np.ascontiguousarray(np.broadcast_to(f(inp["final_g"])[None, :], (128, D))),
        "cmats": cm, "cmats2": cm2, "resetmask": rm, "selb": selb, "ones_row": ones,
    }
    return sh


def core_inputs(sh, x, c, b0, NB):
    m = dict(sh)
    m["x"] = np.ascontiguousarray(x[b0:b0 + NB])
    cc = np.asarray(c[b0:b0 + NB], np.float32)
    m["cT"] = np.ascontiguousarray(cc.reshape(NB, 8, 128).transpose(2, 1, 0))
    return m


def kernel(**inputs):
    x = np.asarray(inputs["x"], np.float32)
    c = np.asarray(inputs["c"], np.float32)
    B, T, _ = x.shape
    n = 8
    NB = B // n
    E = 32
    GM = 256
    sh = prep_shared(inputs, NB, GM, E, T)
    nc = build(T, NB, E, GM)
    in_maps = [core_inputs(sh, x, c, i * NB, NB) for i in range(n)]
    res = run_bass_kernel_spmd(nc, in_maps, core_ids=list(range(n)))
    return np.concatenate([r["out"] for r in res.results], axis=0).astype(np.float32)
```

```python
import numpy as np
from contextlib import ExitStack
import concourse.bass as bass
import concourse.mybir as mybir
from concourse.bass_utils import run_bass_kernel_spmd

F32 = mybir.dt.float32
BF = mybir.dt.bfloat16
AF = mybir.ActivationFunctionType
ALU = mybir.AluOpType

D = 1024
NORM_EPS = 1e-5
GN_EPS = 64e-5
NDS = 12


class Buf:
    __slots__ = ("lw", "rd")

    def __init__(self):
        self.lw = None
        self.rd = {}


class TT:
    def __init__(self, t, nsub=0):
        self.t = t
        self.b = Buf()
        self.subs = [Buf() for _ in range(nsub)]

    def __getitem__(self, idx):
        return self.t[idx]


class _Rec:
    def __init__(self):
        self.call = None

    def __getattr__(self, name):
        def f(*a, **kw):
            self.call = (name, a, kw)
            return self
        return f


class KB:
    def __init__(self, nc):
        self.nc = nc
        self.eng = {"pe": nc.tensor, "act": nc.scalar, "dve": nc.vector, "pool": nc.gpsimd, "sp": nc.sync}
        self.sem = {n: nc.alloc_semaphore("s_" + n) for n in ["pe", "act", "dve", "pool"]}
        self.cnt = {n: 0 for n in ["pe", "act", "dve", "pool"]}
        self.waited = {}
        self.dsems = {q: [nc.alloc_semaphore("d%s%d" % (q, i)) for i in range(NDS)] for q in ("sp", "pool")}
        self.dtot = {q: [0] * NDS for q in ("sp", "pool")}
        self.dnext = {"sp": 0, "pool": 0}
        self.nid = 0
        self.bregs = {}
        self.rec = None
        self.last_tile = 2

    def _wait(self, eng, tok):
        if tok is None:
            return
        key, sem, val = tok
        if eng == "pe" and key == "pe":
            return
        if self.waited.get((eng, key), 0) >= val:
            return
        self.waited[(eng, key)] = val
        self.eng[eng].wait_ge(sem, val)

    def _deps(self, eng, R, W):
        for b in R:
            self._wait(eng, b.lw)
        for b in W:
            self._wait(eng, b.lw)
            for t in b.rd.values():
                self._wait(eng, t)

    def _post(self, tok, R, W):
        for b in R:
            b.rd[tok[0]] = tok
        for b in W:
            b.lw = tok
            b.rd = {}

    def op(self, eng, fn, R=(), W=()):
        R = [x.b if isinstance(x, TT) else x for x in R]
        W = [x.b if isinstance(x, TT) else x for x in W]
        if self.rec is not None:
            r = _Rec()
            fn(r)
            self.rec.append(("op", eng, r.call, R, W))
            return None
        if eng == "pe":
            tid = self._pe_tile(fn)
            if tid != self.last_tile and tid in (0, 1) and self.last_tile in (0, 1) and self.cnt["pe"] > 0:
                self.eng["pe"].wait_ge(self.sem["pe"], self.cnt["pe"])
            self.last_tile = tid
        self._deps(eng, R, W)
        ins = fn(self.eng[eng])
        self.cnt[eng] += 1
        ins.then_inc(self.sem[eng], 1)
        tok = (eng, self.sem[eng], self.cnt[eng])
        self._post(tok, R, W)
        return tok

    def dma(self, out, in_, R=(), W=(), q="sp", indirect=None):
        R = [x.b if isinstance(x, TT) else x for x in R]
        W = [x.b if isinstance(x, TT) else x for x in W]
        if self.rec is not None:
            self.rec.append(("dma", out, in_, R, W, q, indirect))
            return None
        i = self.dnext[q]
        self.dnext[q] = (i + 1) % NDS
        sem = self.dsems[q][i]
        key = "d%s%d" % (q, i)
        if self.dtot[q][i] > 0:
            self._wait(q, (key, sem, self.dtot[q][i]))
        self._deps(q, R, W)
        if indirect is None:
            ins = self.eng[q].dma_start(out=out, in_=in_)
        else:
            kind, idx = indirect[0], indirect[1]
            off = bass.IndirectOffsetOnAxis(ap=idx, axis=0)
            extra = {}
            if len(indirect) > 2:
                if indirect[2] not in self.bregs:
                    self.bregs[indirect[2]] = self.eng[q].to_reg(indirect[2])
                extra = dict(bounds_check=self.bregs[indirect[2]], oob_is_err=False)
            ins = self.eng[q].indirect_dma_start(out=out, out_offset=(off if kind == "scatter" else None), in_=in_,
                                                 in_offset=(off if kind == "gather" else None), **extra)
        self.dtot[q][i] += 16
        ins.then_inc(sem, 16)
        tok = (key, sem, self.dtot[q][i])
        self._post(tok, R, W)
        return tok

    def _pe_tile(self, fn):
        r = _Rec()
        fn(r)
        name, a, kw = r.call
        ap = kw.get("in_") if name == "transpose" else (kw.get("lhsT") if "lhsT" in kw else (a[1] if len(a) > 1 else None))
        if ap is None:
            return 2
        st, sz = ap.start_partition(), ap.partition_size()
        if sz > 64:
            return 2
        return 0 if st < 64 else 1

    def begin_rec(self):
        self.rec = []

    def end_rec(self):
        r, self.rec = self.rec, None
        return r

    def _play1(self, it):
        if it[0] == "op":
            _, eng, (name, a, kw), R, W = it
            self.op(eng, lambda e: getattr(e, name)(*a, **kw), R, W)
        else:
            _, out, in_, R, W, q, indirect = it
            self.dma(out, in_, R, W, q, indirect)

    def play_merged(self, X, Y=()):
        nx, ny = len(X), len(Y)
        ix = iy = 0
        def is_pe(it):
            return it[0] == "op" and it[1] == "pe"
        while ix < nx or iy < ny:
            if iy >= ny or (ix < nx and ix * ny <= iy * nx):
                self._play1(X[ix]); ix += 1
                while ix < nx and is_pe(X[ix - 1]) and is_pe(X[ix]):
                    self._play1(X[ix]); ix += 1
            else:
                self._play1(Y[iy]); iy += 1
                while iy < ny and is_pe(Y[iy - 1]) and is_pe(Y[iy]):
                    self._play1(Y[iy]); iy += 1

    def sb(self, name, shape, dt=F32, nsub=0, scope=None):
        self.nid += 1
        nm = "%s_%d" % (name, self.nid)
        if scope is None:
            return TT(self.nc.alloc_sbuf_tensor(nm, list(shape), dt), nsub)
        return TT(scope.enter_context(self.nc.sbuf_tensor(nm, list(shape), dt)), nsub)

    def barrier(self):
        names = ["pe", "act", "dve", "pool", "sp"]
        for e in names:
            for n in ["pe", "act", "dve", "pool"]:
                if n != e and self.cnt[n] > 0:
                    self._wait(e, (n, self.sem[n], self.cnt[n]))
            for q in ("sp", "pool"):
                for i in range(NDS):
                    if self.dtot[q][i] > 0:
                        self._wait(e, ("d%s%d" % (q, i), self.dsems[q][i], self.dtot[q][i]))

    def finish(self):
        for q in ("sp", "pool"):
            for i in range(NDS):
                if self.dtot[q][i] > 0:
                    self.eng["sp"].wait_ge(self.dsems[q][i], self.dtot[q][i])
        for n in ["pe", "act", "dve", "pool"]:
            if self.cnt[n] > 0:
                self.eng["sp"].wait_ge(self.sem[n], self.cnt[n])


KSTOP = 0
GVAR = 0
PMODE = 1


def build(T, NB, E, GM=256, dbg=None):
    nc = bass.Bass("TRN2", target_bir_lowering=False)
    k = KB(nc)
    NT = T // 128
    NG = T // GM
    TPG = GM // 128
    EG = min(512, T)

    def din(name, shape):
        return nc.dram_tensor(name, list(shape), F32, kind="ExternalInput").ap()

    x_d = din("x", [NB, T, D])
    cT_d = din("cT", [128, 8, NB])
    adaw_d = din("ada_w", [D, 6 * D])
    adab_fm_d = din("ada_b_fm", [128, 48])
    adab_row_d = din("ada_b_row", [1, 6 * D])
    g12_d = din("g12_fm", [128, 2, 8])
    win_d = din("w_in", [D, 3328])
    convw_d = din("conv_w_fm", [128, 4, 3])
    convgn_d = din("conv_gn_fm", [128, 4])
    mu_d = din("mu_fm", [128, 14])
    vecs_d = din("vecs_fm", [128, 7, 4])
    lora_d = din("lora_up", [128, 512])
    gup_d = din("g_up", [128, 512])
    wout_d = din("w_out", [D, D])
    rw_d = din("router_w", [D, 32])
    rb_d = din("router_b_bc", [128, 32])
    w1_d = din("w1", [E * D, 2 * D])
    b1_d = din("b1_rows", [E * 128, 16])
    w2_d = din("w2", [E * D, D])
    b2_d = din("b2", [E, D])
    NTC = NB * (T // 128)
    BLK = 512
    NBLK = (NTC * 128 * 4) // BLK + 32
    bs_d = din("blkstart", [128, NBLK])
    iop_d = din("iotaP", [128, 8])
    xs_d = nc.dram_tensor("xs_scr", [NBLK * BLK, D], BF, kind="Internal").ap()
    yb_d = nc.dram_tensor("yb_scr", [NBLK * BLK, D], F32, kind="Internal").ap()
    fg_d = din("final_g_bc", [128, D])
    cm_d = din("cmats", [128, 7, 128])
    rm_d = din("resetmask", [128, GM])
    cm2_d = din("cmats2", [128, 4, 256])
    sel_d = din("selb", [NB, NB, 128])
    ones_d = din("ones_row", [1, NB])
    g2row_d = din("g2_row", [NB, D])
    modscr_d = nc.dram_tensor("modscr", [NB, 4, D], F32, kind="Internal").ap()
    out_d = nc.dram_tensor("out", [NB, T, D], F32, kind="ExternalOutput").ap()
    h1_d = nc.dram_tensor("h1_scratch", [NB, T, D], F32, kind=("ExternalOutput" if dbg else "Internal")).ap()

    banks = [TT(nc.alloc_psum_tensor("psb%d" % i, [128, 512], F32)) for i in range(6)]
    PSX = ExitStack()
    banks += [TT(PSX.enter_context(nc.psum_tensor("psbx%d" % i, [128, 512], F32))) for i in range(2)]
    bbanks = []
    bstate = {"n": 0, "b": 0, "pool": None, "nx": 0, "ny": 0}

    reserved = set()

    def psum():
        if bstate["pool"] == "X":
            bstate["nx"] += 1
            return banks[bstate["nx"] % 6]
        if bstate["pool"] == "Y":
            bstate["ny"] += 1
            return banks[6 + bstate["ny"] % 2]
        while True:
            i = bstate["n"] % len(banks)
            bstate["n"] += 1
            if i not in reserved:
                return banks[i]

    def psum_bf():
        b = bbanks[bstate["b"] % 2]
        bstate["b"] += 1
        return b

    def mm(ps, out_ap, pairs, R):
        n = len(pairs)
        tok = None
        for i, (l, r) in enumerate(pairs):
            tok = k.op("pe", lambda e, l=l, r=r, i=i: e.matmul(out_ap, l, r, start=(i == 0), stop=(i == n - 1)),
                       R=R, W=[ps])
        return tok

    P0 = ExitStack()
    MX = ExitStack()
    ME = ExitStack()
    cm = k.sb("cm", [128, 7, 128])
    k.dma(cm[:], cm_d[:, :, :], W=[cm])
    ident, m_su, m_sl, m_ui, bones = (cm[:, i, :] for i in range(5))
    cmb = k.sb("cmb", [128, 128], BF)
    k.op("dve", lambda e: e.tensor_copy(out=cmb[:], in_=ident), R=[cm], W=[cmb])
    smallw = k.sb("smallw", [128, 48 + 16 + 12 + 4 + 14 + 28])
    o = 0
    adab_fm = smallw[:, o:o + 48]; k.dma(adab_fm, adab_fm_d[:, :], W=[smallw]); o += 48
    g12 = smallw[:, o:o + 16]; k.dma(g12, g12_d.rearrange("p a b -> p (a b)"), W=[smallw]); o += 16
    cw = smallw[:, o:o + 12]; k.dma(cw, convw_d.rearrange("p a b -> p (a b)"), W=[smallw]); o += 12
    cgn = smallw[:, o:o + 4]; k.dma(cgn, convgn_d[:, :], W=[smallw]); o += 4
    mu = smallw[:, o:o + 14]; k.dma(mu, mu_d[:, :], W=[smallw]); o += 14
    vecs = smallw[:, o:o + 28]; k.dma(vecs, vecs_d.rearrange("p a b -> p (a b)"), W=[smallw]); o += 28

    def vec(v, i):
        return vecs[:, v * 4 + i:v * 4 + i + 1]

    rw = k.sb("rw", [128, 8, 32]); k.dma(rw[:], rw_d.rearrange("(c p) n -> p c n", p=128), W=[rw])
    rb = k.sb("rb", [128, 32]); k.dma(rb[:], rb_d[:, :], W=[rb])
    cT = k.sb("cT", [128, 8 * NB]); k.dma(cT[:], cT_d.rearrange("p a b -> p (a b)"), W=[cT])

    small = k.sb("small", [128, 64])
    gtb = [[None, None] for _ in range(NB)]
    GS = [k.sb("GS", [128, 32]) for _ in range(NB)]
    nstat = [k.sb("nstat", [128, 4]) for _ in range(2)]
    xn = [k.sb("xn", [128, D])]
    junk = k.sb("junk", [128, D], BF)
    cact = k.sb("cact", [128, 8 * NB])
    csg = k.sb("csg", [128, 8 * NB])
    modfm = [k.sb("modfm", [128, 48]) for _ in range(NB)]
    winb = k.sb("winb", [128, 8, 3328], BF, scope=MX)
    woutb = k.sb("woutb", [128, 8, D], BF, scope=MX)
    rmask = k.sb("rmask", [128, GM], scope=MX)
    k.dma(rmask[:], rm_d[:, :], W=[rmask])
    lora = k.sb("lora", [128, 512], scope=MX); k.dma(lora[:], lora_d[:, :], W=[lora])
    gup = k.sb("gup", [128, 512], scope=MX); k.dma(gup[:], gup_d[:, :], W=[gup])
    gt1t = k.sb("gtb1", [128, D], scope=MX)
    selb = k.sb("selb", [NB, NB, 128], scope=P0)
    k.dma(selb[:], sel_d[:, :, :], W=[selb])
    stage = [k.sb("stage", [128, 2048], scope=P0) for _ in range(2)]
    sidx = {"n": 0}

    def next_stage():
        s = stage[sidx["n"] % 2]
        sidx["n"] += 1
        return s
    awbs = [k.sb("awb", [128, 8, 512], scope=P0) for _ in range(2)]
    modrow = k.sb("modrow", [NB, 4, D], scope=P0)
    adabr = k.sb("adabr", [NB, 4, D], scope=P0)
    g2row = k.sb("g2row", [NB, D], scope=P0)
    k.dma(g2row[:], g2row_d[:, :], W=[g2row])

    k.op("act", lambda e: e.activation(out=csg[:], in_=cT[:], func=AF.Sigmoid), R=[cT], W=[csg])
    k.op("dve", lambda e: e.tensor_tensor(out=cact[:], in0=cT[:], in1=csg[:], op=ALU.mult), R=[cT, csg], W=[cact])
    ps_fm = psum()
    reserved.add(banks.index(ps_fm))
    for blk in range(12):
        awb = awbs[blk % 2]
        for h in range(4):
            st = next_stage()
            k.dma(st[:, 0:1024].rearrange("p (c n) -> p c n", c=2),
                  adaw_d[h * 256:(h + 1) * 256, blk * 512:(blk + 1) * 512].rearrange("(c p) n -> p c n", p=128),
                  W=[st])
            k.op("act", lambda e, st=st, h=h, awb=awb: e.activation(
                out=awb[:, 2 * h:2 * h + 2, :], in_=st[:, 0:1024].rearrange("p (c n) -> p c n", c=2), func=AF.Copy),
                R=[st], W=[awb])
        for jj in range(4):
            j = blk * 4 + jj
            mm(ps_fm, ps_fm[:, j * NB:(j + 1) * NB],
               [(awb[:, kc, jj * 128:(jj + 1) * 128], cact[:, kc * NB:(kc + 1) * NB]) for kc in range(8)],
               R=[awb, cact])
        if blk in (4, 5, 6, 7, 8, 9, 10, 11):
            ps_r = psum()
            pairs = [(cact[:, kc * NB:(kc + 1) * NB], awb[:, kc, :]) for kc in range(8)]
            mm(ps_r, ps_r[0:NB, :], pairs, R=[awb, cact])
            which = {4: 0, 5: 0, 10: 1, 11: 1, 6: 2, 7: 2, 8: 3, 9: 3}[blk]
            half = blk % 2
            k.op("act", lambda e, ps_r=ps_r, which=which, half=half: e.activation(
                out=modrow[:, which, half * 512:(half + 1) * 512], in_=ps_r[0:NB, :], func=AF.Copy),
                R=[ps_r], W=[modrow])
    for b in range(NB):
        k.op("dve", lambda e, b=b: e.tensor_tensor(out=modfm[b][:], in0=ps_fm[:, b:48 * NB:NB], in1=adab_fm, op=ALU.add),
             R=[ps_fm, smallw], W=[modfm[b]])
    reserved.clear()
    for b in range(NB):
        for which, c0 in ((0, 2 * D), (1, 5 * D), (2, 3 * D), (3, 4 * D)):
            k.dma(adabr[b:b + 1, which, :], adab_row_d[0:1, c0:c0 + D], W=[adabr])
    k.op("dve", lambda e: e.tensor_tensor(out=modrow[:], in0=modrow[:], in1=adabr[:], op=ALU.add),
         R=[adabr], W=[modrow])
    k.op("dve", lambda e: e.scalar_tensor_tensor(out=modrow[:, 3, :], in0=modrow[:, 3, :], scalar=1.0, in1=g2row[:],
                                                 op0=ALU.add, op1=ALU.mult), R=[g2row], W=[modrow])
    k.dma(modscr_d[:, :, :], modrow[:, 0:4, :], R=[modrow])
    for b in range(NB):
        for n, (sc0, sh0) in enumerate(((8, 0), (32, 24))):
            k.op("dve", lambda e, b=b, n=n, sc0=sc0: e.scalar_tensor_tensor(
                out=GS[b][:, n * 16:n * 16 + 8], in0=modfm[b][:, sc0:sc0 + 8], scalar=1.0,
                in1=g12[:, n * 8:(n + 1) * 8], op0=ALU.add, op1=ALU.mult), R=[modfm[b], smallw], W=[GS[b]])
            k.op("dve", lambda e, b=b, n=n, sh0=sh0: e.tensor_copy(
                out=GS[b][:, n * 16 + 8:n * 16 + 16], in_=modfm[b][:, sh0:sh0 + 8]), R=[modfm[b]], W=[GS[b]])

    nctr = {"n": 0}

    def rstd_of(src_ap, srcbuf):
        i = nctr["n"] % 2
        nctr["n"] += 1
        st = nstat[i]
        k.op("act", lambda e: e.activation(out=junk[:], in_=src_ap, func=AF.Square, accum_out=st[:, 0:1]),
             R=[srcbuf], W=[junk, st])
        k.op("dve", lambda e: e.tensor_scalar(out=st[:, 1:2], in0=st[:, 0:1], scalar1=1.0 / D, scalar2=NORM_EPS,
                                              op0=ALU.mult, op1=ALU.add), R=[st], W=[st])
        k.op("act", lambda e: e.activation(out=st[:, 2:3], in_=st[:, 1:2], func=AF.Sqrt), R=[st], W=[st])
        k.op("dve", lambda e: e.reciprocal(out=st[:, 3:4], in_=st[:, 2:3]), R=[st], W=[st])
        return st, i

    def norm_T(src_ap, srcbuf, gs, goff, evac):
        st, i = rstd_of(src_ap, srcbuf)
        i = 0
        k.op("dve", lambda e: e.tensor_scalar(out=xn[i][:], in0=src_ap, scalar1=st[:, 3:4], scalar2=None, op0=ALU.mult),
             R=[srcbuf, st], W=[xn[i]])
        for hb in range(2):
            ps = psum()
            for c in range(4):
                kc = hb * 4 + c
                k.op("pe", lambda e, ps=ps, c=c, kc=kc: e.transpose(out=ps[:, c * 128:(c + 1) * 128],
                                                                     in_=xn[i][:, kc * 128:(kc + 1) * 128], identity=ident),
                     R=[xn[i], cm], W=[ps])
            for c in range(4):
                kc = hb * 4 + c
                evac(kc, ps[:, c * 128:(c + 1) * 128], gs[:, goff + kc:goff + kc + 1], gs[:, goff + 8 + kc:goff + 9 + kc], ps)

    for kc in range(8):
        for half in range(2):
            st = next_stage()
            k.dma(st[:, 0:1664], win_d[kc * 128:(kc + 1) * 128, half * 1664:(half + 1) * 1664], W=[st])
            k.op("act" if half else "dve", lambda e, st=st, kc=kc, half=half: (
                e.activation(out=winb[:, kc, half * 1664:(half + 1) * 1664], in_=st[:, 0:1664], func=AF.Copy) if half else
                e.tensor_copy(out=winb[:, kc, half * 1664:(half + 1) * 1664], in_=st[:, 0:1664])), R=[st], W=[winb])
    for kc in range(8):
        st = next_stage()
        k.dma(st[:, 0:D], wout_d[kc * 128:(kc + 1) * 128, :], W=[st])
        k.op("act", lambda e, st=st, kc=kc: e.activation(out=woutb[:, kc, :], in_=st[:, 0:D], func=AF.Copy), R=[st], W=[woutb])

    k.barrier()
    P0.close()
    def ksb(*a, **kw):
        return k.sb(*a, scope=MX, **kw)
    xgs = [ksb("xg", [128, TPG, D], nsub=TPG) for _ in range(2)]
    uT = ksb("uT", [128, 8, GM], BF)
    mixT = ksb("mixT", [128, 8, GM], BF, nsub=8)
    pbuf = [ksb("pbuf", [128, GM + 1]) for _ in range(14)]
    pl12, pl13 = ksb("pl12", [128, GM]), ksb("pl13", [128, GM])
    rkv = [[ksb("rkv", [128, GM]) for _ in range(3)] for _ in range(2)]
    ggs = [ksb("gg", [128, GM]) for _ in range(2)]
    bonuss = [ksb("bonus", [128, GM]) for _ in range(2)]
    PLs = [ksb("PL", [128, 2 * TPG]) for _ in range(2)]
    ucv = [ksb("ucv", [128, GM + 2]) for _ in range(4)]
    NTMP = 26
    tmps = [ksb("tmp", [128, GM]) for _ in range(NTMP)]
    Rtbs = [ksb("Rtb", [128, GM], BF) for _ in range(2)]
    TTts = [[ksb("TTt", [128, 512], BF) for _ in range(TPG)] for _ in range(2)]
    padss = [[[ksb("pads", [128, 3, 128], BF) for _ in range(TPG)] for _ in range(2)] for _ in range(2)]
    UTpad = [ksb("UTpad", [128, 128], BF) for _ in range(2)]
    Xs = [[ksb("Xs", [128, 2, 128], BF) for _ in range(2)] for _ in range(TPG)]
    XTs = [[ksb("XTs", [128, 2, 128], BF) for _ in range(2)] for _ in range(TPG)]
    Tms = [[ksb("Tms", [128, 2, 128], BF) for _ in range(2)] for _ in range(TPG)]
    MakT = [ksb("MakT", [128, 2, 128], BF) for _ in range(TPG)]
    MT = [ksb("MT", [128, 2, 128], BF) for _ in range(TPG)]
    Mbr = [ksb("Mbr", [128, 2, 128], BF) for _ in range(TPG)]
    Mkr = [ksb("Mkr", [128, 2, 128], BF) for _ in range(TPG)]
    Atbs = [ksb("Atb", [128, GM], BF) for _ in range(2)]
    Btbs = [ksb("Btb", [128, GM], BF) for _ in range(2)]
    Ktbs = [ksb("Ktb", [128, GM], BF) for _ in range(2)]
    Sdec = ksb("Sdec", [128, 128])
    cm2 = ksb("cm2", [128, 4, 256])
    k.dma(cm2[:], cm2_d[:, :, :], W=[cm2])
    mk2_su, mk2_sl, mk2_ui, mk2_id = (cm2[:, i_, :] for i_ in range(4))
    Ah = [ksb("Ah", [128, 128], BF) for _ in range(TPG)]
    WT2 = [ksb("WT2", [128, 128]) for _ in range(TPG)]
    STf = [ksb("STf", [128, 128]) for _ in range(4)]
    STb = [ksb("STb", [128, 128], BF) for _ in range(4)]

    for hh in range(2):
        for tb in range(TPG):
            for par_ in range(2):
                k.op("dve", lambda e: e.memset(padss[par_][hh][tb][:], 0.0), W=[padss[par_][hh][tb]])
        k.op("dve", lambda e, hh=hh: e.memset(UTpad[hh][:], 0.0), W=[UTpad[hh]])

    def proj(j):
        ps = psum()
        mm(ps, ps[:, 0:GM], [(winb[:, kc, j * 128:(j + 1) * 128], uT[:, kc, :]) for kc in range(8)], R=[winb, uT])
        return ps

    def bsum(src, srcbuf):
        ps = psum()
        mm(ps, ps[:, 0:GM], [(bones, src)], R=[cm, srcbuf])
        return ps

    def rsqrt_ps(ps, scale, eps, dst):
        k.op("dve", lambda e: e.tensor_scalar(out=dst[:], in0=ps[:, 0:GM], scalar1=scale, scalar2=eps, op0=ALU.mult, op1=ALU.add),
             R=[ps], W=[dst])
        k.op("act", lambda e: e.activation(out=dst[:], in_=dst[:], func=AF.Sqrt), R=[dst], W=[dst])
        k.op("dve", lambda e: e.reciprocal(out=dst[:], in_=dst[:]), R=[dst], W=[dst])

    def stage_U(b_, g_):
        xg_ = xgs[(b_ * NG + g_) % 2]
        for tt in range(TPG):
            k.dma(xg_[:, tt, :], x_d[b_, g_ * GM + tt * 128:g_ * GM + (tt + 1) * 128, :], W=[xg_.subs[tt]])

            def ev(kc, psap, sc, bi, ps, tt=tt):
                k.op("act", lambda e: e.activation(out=uT[:, kc, tt * 128:(tt + 1) * 128], in_=psap, func=AF.Identity,
                                                   scale=sc, bias=bi), R=[ps, GS[b_]], W=[uT])
            norm_T(xg_[:, tt, :], xg_.subs[tt], GS[b_], 0, ev)

    h1ctr = {"n": 0}
    for b in range(NB):
        k.dma(gt1t[:], modscr_d[b, 0, :].partition_broadcast(128), W=[gt1t])
        for q in range(14):
            k.op("dve", lambda e, q=q: e.memset(pbuf[q][:, 0:1], 0.0), W=[pbuf[q]])
        for ci in range(4):
            k.op("dve", lambda e, ci=ci: e.memset(ucv[ci][:, 0:2], 0.0), W=[ucv[ci]])
            k.op("dve", lambda e, ci=ci: e.memset(STf[ci][:], 0.0), W=[STf[ci]])
            k.op("dve", lambda e, ci=ci: e.memset(STb[ci][:], 0.0), W=[STb[ci]])
        for g in range(NG):
            t0 = g * GM
            xg = xgs[(b * NG + g) % 2]
            if b == 0 and g == 0:
                stage_U(0, 0)
            for ci in range(4):
                tB, tC, t1, yb, ysq, rr = tmps[6 * (ci % 2) + 3:6 * (ci % 2) + 9]
                ps = proj(ci)
                k.op("act", lambda e, ps=ps: e.activation(out=tB[:], in_=ps[:, 0:GM], func=AF.Copy), R=[ps], W=[tB])
                ps = proj(4 + ci)
                k.op("act", lambda e, ps=ps: e.activation(out=tC[:], in_=ps[:, 0:GM], func=AF.Copy), R=[ps], W=[tC])
                ps = proj(8 + ci)
                u = ucv[ci]
                k.op("dve", lambda e, ps=ps: e.tensor_tensor(out=u[:, 2:2 + GM], in0=tC[:], in1=ps[:, 0:GM], op=ALU.mult),
                     R=[tC, ps], W=[u])
                k.op("dve", lambda e: e.tensor_scalar(out=t1[:], in0=u[:, 0:GM], scalar1=cw[:, ci * 3:ci * 3 + 1], scalar2=None,
                                                      op0=ALU.mult), R=[u, smallw], W=[t1])
                for kk_ in (1, 2):
                    k.op("dve", lambda e, kk_=kk_: e.scalar_tensor_tensor(out=t1[:], in0=u[:, kk_:kk_ + GM],
                                                                           scalar=cw[:, ci * 3 + kk_:ci * 3 + kk_ + 1], in1=t1[:],
                                                                           op0=ALU.mult, op1=ALU.add), R=[u, smallw], W=[t1])
                k.op("dve", lambda e: e.tensor_tensor(out=yb[:], in0=tB[:], in1=t1[:], op=ALU.mult), R=[tB, t1], W=[yb])
                k.op("act", lambda e: e.activation(out=ysq[:], in_=yb[:], func=AF.Square), R=[yb], W=[ysq])
                psn = bsum(ysq[:], ysq)
                rsqrt_ps(psn, 1.0 / 64, NORM_EPS, rr)
                k.op("dve", lambda e: e.scalar_tensor_tensor(out=mixT[:, ci, :], in0=yb[:], scalar=cgn[:, ci:ci + 1], in1=rr[:],
                                                             op0=ALU.mult, op1=ALU.mult), R=[yb, rr, smallw], W=[mixT.subs[ci]])
                k.op("dve", lambda e: e.tensor_copy(out=u[:, 0:2], in_=u[:, GM:GM + 2]), R=[], W=[u])
            def lerp_tile(q, dst):
                ps = proj(12 + q)
                pb = pbuf[q]
                d_ = tmps[24 + (q % 2)]
                k.op("act", lambda e: e.activation(out=pb[:, 1:GM + 1], in_=ps[:, 0:GM], func=AF.Copy), R=[ps], W=[pb])
                k.op("dve", lambda e: e.tensor_tensor(out=d_[:], in0=pb[:, 0:GM], in1=pb[:, 1:GM + 1], op=ALU.subtract),
                     R=[pb], W=[d_])
                k.op("dve", lambda e: e.scalar_tensor_tensor(out=dst[:], in0=d_[:], scalar=mu[:, q:q + 1], in1=pb[:, 1:GM + 1],
                                                             op0=ALU.mult, op1=ALU.add), R=[d_, pb, smallw], W=[dst])
                k.op("dve", lambda e: e.tensor_copy(out=pb[:, 0:1], in_=pb[:, GM:GM + 1]), R=[], W=[pb])
            lerp_tile(12, pl12)
            lerp_tile(13, pl13)
            th, sgm = tmps[1], tmps[2]
            k.op("act", lambda e: e.activation(out=th[0:64, :], in_=pl12[0:64, :], func=AF.Tanh), R=[pl12], W=[th])
            k.op("act", lambda e: e.activation(out=sgm[:], in_=pl13[:], func=AF.Sigmoid), R=[pl13], W=[sgm])

            def tile_A1(i, par):
                (ld, av, kk, kksq, rinv, kkn, tq, kmod, prod, lcum, Pinc, Pexc, Pinv, At, Bt, Kt, Rt) = tmps[3:20]
                gg, bonus, PL = ggs[par], bonuss[par], PLs[par]
                Atb, Btb, Ktb, Rtb, TTt, pads = Atbs[par], Btbs[par], Ktbs[par], Rtbs[par], TTts[par], padss[par]
                rl, kl, vl = rkv[par]
                lerp_tile(i, rl)
                lerp_tile(4 + i, kl)
                lerp_tile(8 + i, vl)
                cs = slice(i * 128, (i + 1) * 128)
                ps = psum()
                mm(ps, ps[:, 0:GM], [(lora[0:64, cs], th[0:64, :])], R=[lora, th])
                k.op("act", lambda e, ps=ps: e.activation(out=ld[:], in_=ps[:, 0:GM], func=AF.Sigmoid, bias=vec(0, i), scale=1.0),
                     R=[ps, smallw], W=[ld])
                k.op("dve", lambda e: e.tensor_scalar(out=ld[:], in0=ld[:], scalar1=-0.6065306597126334, scalar2=None, op0=ALU.mult),
                     R=[], W=[ld])
                ps = psum()
                mm(ps, ps[:, 0:GM], [(lora[64:128, cs], pl12[64:128, :])], R=[lora, pl12])
                k.op("act", lambda e, ps=ps: e.activation(out=av[:], in_=ps[:, 0:GM], func=AF.Sigmoid, bias=vec(1, i), scale=1.0),
                     R=[ps, smallw], W=[av])
                ps = psum()
                mm(ps, ps[:, 0:GM], [(gup[:, cs], sgm[:])], R=[gup, sgm])
                k.op("act", lambda e, ps=ps: e.activation(out=gg[:], in_=ps[:, 0:GM], func=AF.Copy), R=[ps], W=[gg])
                k.op("dve", lambda e: e.tensor_scalar(out=kk[:], in0=kl[:], scalar1=vec(2, i), scalar2=None, op0=ALU.mult),
                     R=[kl, smallw], W=[kk])
                k.op("act", lambda e: e.activation(out=kksq[:], in_=kk[:], func=AF.Square), R=[kk], W=[kksq])
                psn = bsum(kksq[:], kksq)
                k.op("act", lambda e, psn=psn: e.activation(out=rinv[:], in_=psn[:, 0:GM], func=AF.Sqrt), R=[psn], W=[rinv])
                k.op("dve", lambda e: e.tensor_scalar(out=rinv[:], in0=rinv[:], scalar1=1e-12, scalar2=None, op0=ALU.max), R=[], W=[rinv])
                k.op("dve", lambda e: e.reciprocal(out=rinv[:], in_=rinv[:]), R=[], W=[rinv])
                k.op("dve", lambda e: e.tensor_tensor(out=kkn[:], in0=kk[:], in1=rinv[:], op=ALU.mult), R=[kk, rinv], W=[kkn])
                k.op("dve", lambda e: e.tensor_scalar(out=tq[:], in0=av[:], scalar1=-1.0, scalar2=vec(3, i), op0=ALU.add, op1=ALU.mult),
                     R=[av, smallw], W=[tq])
                k.op("dve", lambda e: e.scalar_tensor_tensor(out=kmod[:], in0=tq[:], scalar=1.0, in1=kl[:], op0=ALU.add, op1=ALU.mult),
                     R=[tq, kl], W=[kmod])
                k.op("dve", lambda e: e.scalar_tensor_tensor(out=prod[:], in0=rl[:], scalar=vec(4, i), in1=kmod[:], op0=ALU.mult,
                                                             op1=ALU.mult), R=[rl, kmod, smallw], W=[prod])
                psrk = bsum(prod[:], prod)
                k.op("dve", lambda e, psrk=psrk: e.tensor_tensor(out=bonus[:], in0=psrk[:, 0:GM], in1=vl[:], op=ALU.mult),
                     R=[psrk, vl], W=[bonus])
                k.op("dve", lambda e: e.tensor_tensor_scan(out=lcum[:], data0=rmask[:], data1=ld[:], initial=0.0, op0=ALU.mult,
                                                           op1=ALU.add), R=[rmask, ld], W=[lcum])
                k.op("act", lambda e: e.activation(out=Pinc[:], in_=lcum[:], func=AF.Exp), R=[lcum], W=[Pinc])
                k.op("dve", lambda e: e.tensor_tensor(out=Pexc[:], in0=lcum[:], in1=ld[:], op=ALU.subtract), R=[lcum, ld], W=[Pexc])
                k.op("act", lambda e: e.activation(out=Pexc[:], in_=Pexc[:], func=AF.Exp), R=[], W=[Pexc])
                k.op("act", lambda e: e.activation(out=Pinv[:], in_=lcum[:], func=AF.Exp, scale=-1.0), R=[lcum], W=[Pinv])
                k.op("dve", lambda e: e.scalar_tensor_tensor(out=At[:], in0=kkn[:], scalar=-1.0, in1=Pexc[:], op0=ALU.mult, op1=ALU.mult),
                     R=[kkn, Pexc], W=[At])
                k.op("dve", lambda e: e.tensor_tensor(out=Bt[:], in0=kkn[:], in1=av[:], op=ALU.mult), R=[kkn, av], W=[Bt])
                k.op("dve", lambda e: e.tensor_tensor(out=Bt[:], in0=Bt[:], in1=Pinv[:], op=ALU.mult), R=[Pinv], W=[Bt])
                k.op("dve", lambda e: e.tensor_tensor(out=Kt[:], in0=kmod[:], in1=Pinv[:], op=ALU.mult), R=[kmod, Pinv], W=[Kt])
                k.op("dve", lambda e: e.tensor_tensor(out=Rt[:], in0=rl[:], in1=Pinc[:], op=ALU.mult), R=[rl, Pinc], W=[Rt])
                k.op("act", lambda e: e.activation(out=Rtb[:], in_=Rt[:], func=AF.Copy), R=[Rt], W=[Rtb])
                for src_, dst_ in ((At, Atb), (Bt, Btb), (Kt, Ktb)):
                    k.op("pool", lambda e: e.tensor_copy(out=dst_[:], in_=src_[:]), R=[src_], W=[dst_])
                for tb in range(TPG):
                    cl = slice(tb * 128, (tb + 1) * 128)
                    ps = psum()
                    for n_, src in enumerate((At, Bt, Kt, vl)):
                        k.op("pe", lambda e: e.transpose(out=ps[:, n_ * 128:(n_ + 1) * 128], in_=src[:, cl], identity=ident),
                             R=[src, cm], W=[ps])
                    k.op("act", lambda e: e.activation(out=TTt[tb][:], in_=ps[:, :], func=AF.Copy), R=[ps], W=[TTt[tb]])
                    for hh in range(2):
                        k.op("dve" if hh else "act", lambda e: (e.tensor_copy(
                            out=pads[hh][tb][:, :, hh * 64:hh * 64 + 64],
                            in_=TTt[tb][:, 128:512].rearrange("p (a c) -> p a c", a=3)[:, :, hh * 64:hh * 64 + 64]) if hh else
                            e.activation(out=pads[hh][tb][:, :, hh * 64:hh * 64 + 64],
                                         in_=TTt[tb][:, 128:512].rearrange("p (a c) -> p a c", a=3)[:, :, hh * 64:hh * 64 + 64],
                                         func=AF.Copy)),
                            R=[TTt[tb]], W=[pads[hh][tb]])
                k.op("act", lambda e: e.activation(out=PL[:], in_=Pinc[:, 63:GM:64], func=AF.Copy), R=[Pinc], W=[PL])

            def tile_X(i, par):
                Osb, dd, dsq, rs = tmps[20:24]
                gg, bonus, PL = ggs[par], bonuss[par], PLs[par]
                Atb, Btb, Ktb, Rtb, TTt, pads = Atbs[par], Btbs[par], Ktbs[par], Rtbs[par], TTts[par], padss[par]
                for tb in range(TPG):
                    cl = slice(tb * 128, (tb + 1) * 128)
                    specs = [(Btb, Atb, mk2_su, Xs[tb][0]), (Atb, Btb, mk2_sl, XTs[tb][0]), (Atb, Ktb, mk2_sl, MakT[tb]),
                             (Btb, Rtb, mk2_ui, Mbr[tb]), (Ktb, Rtb, mk2_ui, Mkr[tb])]
                    gps = [psum() for _ in specs]
                    for hh in range(2):
                        pr = slice(hh * 64, hh * 64 + 64)
                        for (l, r, mask2, dst), ps in zip(specs, gps):
                            mm(ps, ps[:, hh * 128:(hh + 1) * 128], [(l[pr, cl], r[pr, cl])], R=[l, r])
                    for (l, r, mask2, dst), ps in zip(specs, gps):
                        k.op("dve", lambda e: e.tensor_tensor(out=dst[:].rearrange("p a b -> p (a b)"), in0=ps[:, 0:256], in1=mask2,
                                                              op=ALU.mult), R=[ps, cm2], W=[dst])
                    k.op("dve", lambda e: e.tensor_tensor(out=Tms[tb][0][:].rearrange("p a b -> p (a b)"),
                                                          in0=Xs[tb][0][:].rearrange("p a b -> p (a b)"), in1=mk2_id, op=ALU.add),
                         R=[Xs[tb][0], cm2], W=[Tms[tb][0]])
                cur = 0
                for lvl in range(1, 6):
                    nxt = 1 - cur
                    pss = {}
                    for tb in range(TPG):
                        if lvl < 5:
                            ps = psum()
                            for hh in range(2):
                                mm(ps, ps[:, hh * 128:(hh + 1) * 128], [(XTs[tb][cur][:, hh, :], Xs[tb][cur][:, hh, :])],
                                   R=[XTs[tb][cur], Xs[tb][cur]])
                            pss[(tb, 0)] = ps
                        ps = psum()
                        for hh in range(2):
                            mm(ps, ps[:, hh * 128:(hh + 1) * 128], [(Xs[tb][cur][:, hh, :], XTs[tb][cur][:, hh, :])],
                               R=[XTs[tb][cur], Xs[tb][cur]])
                        pss[(tb, 1)] = ps
                    for tb in range(TPG):
                        if lvl < 5:
                            ps = pss[(tb, 0)]
                            k.op("act", lambda e: e.activation(out=Xs[tb][nxt][:].rearrange("p a b -> p (a b)"), in_=ps[:, 0:256],
                                                               func=AF.Copy), R=[ps], W=[Xs[tb][nxt]])
                        ps = pss[(tb, 1)]
                        k.op("act", lambda e: e.activation(out=XTs[tb][nxt][:].rearrange("p a b -> p (a b)"), in_=ps[:, 0:256],
                                                           func=AF.Copy), R=[ps], W=[XTs[tb][nxt]])
                    for tb in range(TPG):
                        ps = psum()
                        for hh in range(2):
                            mm(ps, ps[:, hh * 128:(hh + 1) * 128], [(XTs[tb][nxt][:, hh, :], Tms[tb][cur][:, hh, :])],
                               R=[XTs[tb][nxt], Tms[tb][cur]])
                        pss[(tb, 2)] = ps
                    for tb in range(TPG):
                        ps = pss[(tb, 2)]
                        k.op("dve", lambda e: e.tensor_tensor(out=Tms[tb][nxt][:].rearrange("p a b -> p (a b)"), in0=ps[:, 0:256],
                                                              in1=Tms[tb][cur][:].rearrange("p a b -> p (a b)"), op=ALU.add),
                             R=[ps, Tms[tb][cur]], W=[Tms[tb][nxt]])
                    cur = nxt
                pA, pM = {}, {}
                for tb in range(TPG):
                    ps = psum()
                    for hh in range(2):
                        mm(ps, ps[:, hh * 128:(hh + 1) * 128], [(TTt[tb][:, 0:128], Tms[tb][cur][:, hh, :])], R=[TTt[tb], Tms[tb][cur]])
                    pA[tb] = ps
                    ps = psum()
                    for hh in range(2):
                        mm(ps, ps[:, hh * 128:(hh + 1) * 128], [(MakT[tb][:, hh, :], Tms[tb][cur][:, hh, :])], R=[MakT[tb], Tms[tb][cur]])
                    pM[tb] = ps
                for tb in range(TPG):
                    for hh in range(2):
                        pr = slice(hh * 64, hh * 64 + 64)
                        k.op("act", lambda e: e.activation(out=Ah[tb][pr, :], in_=pA[tb][pr, hh * 128:(hh + 1) * 128], func=AF.Copy),
                             R=[pA[tb]], W=[Ah[tb]])
                    k.op("act", lambda e: e.activation(out=MT[tb][:].rearrange("p a b -> p (a b)"), in_=pM[tb][:, 0:256], func=AF.Copy),
                         R=[pM[tb]], W=[MT[tb]])
                for tb in range(TPG):
                    ps = psum()
                    for hh in range(2):
                        mm(ps, ps[:, hh * 64:(hh + 1) * 64], [(MT[tb][:, hh, :], TTt[tb][:, 384 + hh * 64:384 + hh * 64 + 64])],
                           R=[MT[tb], TTt[tb]])
                    k.op("act", lambda e: e.activation(out=WT2[tb][:], in_=ps[:, 0:128], func=AF.Copy), R=[ps], W=[WT2[tb]])
                for tb in range(TPG):
                    for c in range(2):
                        ro = slice(64 * c, 64 * c + 64)
                        cc0 = tb * 128 + 64 * c
                        k.op("dve", lambda e: e.tensor_scalar(out=Sdec[:], in0=STf[i][:], scalar1=PL[:, 2 * tb + c:2 * tb + c + 1], scalar2=None,
                                                              op0=ALU.mult), R=[STf[i], PL], W=[Sdec])
                        ps = psum()
                        mm(ps, ps[:, 0:128], [(Ah[tb][:], STb[i][:])], R=[Ah[tb], STb[i]])
                        for hh in range(2):
                            hs = slice(hh * 64, hh * 64 + 64)
                            k.op("dve", lambda e: e.tensor_tensor(out=UTpad[hh][ro, hs], in0=ps[ro, hs], in1=WT2[tb][ro, hs], op=ALU.add),
                                 R=[ps, WT2[tb]], W=[UTpad[hh]])
                        pss_ = psum()
                        mm(pss_, pss_[:, 0:128],
                           [(pads[0][tb][ro, 0, :], UTpad[0][ro, :]), (pads[1][tb][ro, 0, :], UTpad[1][ro, :]),
                            (pads[0][tb][ro, 1, :], pads[0][tb][ro, 2, :]), (pads[1][tb][ro, 1, :], pads[1][tb][ro, 2, :])],
                           R=[pads[0][tb], pads[1][tb], UTpad[0], UTpad[1]])
                        k.op("dve", lambda e: e.scalar_tensor_tensor(out=STf[i][:], in0=pss_[:, 0:128], scalar=PL[:, 2 * tb + c:2 * tb + c + 1],
                                                                     in1=Sdec[:], op0=ALU.mult, op1=ALU.add),
                             R=[pss_, PL, Sdec], W=[STf[i]])
                        pso = psum()
                        mm(pso, pso[:, 0:64],
                           [(STb[i][:], Rtb[:, cc0:cc0 + 64]),
                            (UTpad[0][ro, :], Mbr[tb][ro, 0, ro]), (UTpad[1][ro, :], Mbr[tb][ro, 1, ro]),
                            (pads[0][tb][ro, 2, :], Mkr[tb][ro, 0, ro]), (pads[1][tb][ro, 2, :], Mkr[tb][ro, 1, ro])],
                           R=[STb[i], Rtb, UTpad[0], UTpad[1], Mbr[tb], Mkr[tb], pads[0][tb], pads[1][tb]])
                        k.op("act", lambda e: e.activation(out=STb[i][:], in_=STf[i][:], func=AF.Copy), R=[STf[i]], W=[STb[i]])
                        k.op("act", lambda e: e.activation(out=Osb[:, cc0:cc0 + 64], in_=pso[:, 0:64], func=AF.Copy), R=[pso], W=[Osb])
                psm = bsum(Osb[:], Osb)
                k.op("dve", lambda e, psm=psm: e.scalar_tensor_tensor(out=dd[:], in0=psm[:, 0:GM], scalar=-1.0 / 64, in1=Osb[:],
                                                                        op0=ALU.mult, op1=ALU.add), R=[psm, Osb], W=[dd])
                k.op("act", lambda e: e.activation(out=dsq[:], in_=dd[:], func=AF.Square), R=[dd], W=[dsq])
                psv = bsum(dsq[:], dsq)
                rsqrt_ps(psv, 1.0 / 64, GN_EPS, rs)
                k.op("dve", lambda e: e.tensor_tensor(out=dd[:], in0=dd[:], in1=rs[:], op=ALU.mult), R=[rs], W=[dd])
                k.op("dve", lambda e: e.tensor_scalar(out=dd[:], in0=dd[:], scalar1=vec(5, i), scalar2=vec(6, i), op0=ALU.mult, op1=ALU.add),
                     R=[smallw], W=[dd])
                k.op("dve", lambda e: e.tensor_tensor(out=dd[:], in0=dd[:], in1=bonus[:], op=ALU.add), R=[bonus], W=[dd])
                k.op("dve", lambda e: e.tensor_tensor(out=mixT[:, 4 + i, :], in0=dd[:], in1=gg[:], op=ALU.mult), R=[dd, gg],
                     W=[mixT.subs[4 + i]])
            bstate["pool"] = "Y"
            k.begin_rec(); tile_A1(0, 0); Y_ = k.end_rec()
            bstate["pool"] = None
            k.play_merged(Y_)
            for i in range(4):
                bstate["pool"] = "X"
                k.begin_rec(); tile_X(i, i % 2); X_ = k.end_rec()
                bstate["pool"] = "Y"
                k.begin_rec()
                if i < 3:
                    tile_A1(i + 1, (i + 1) % 2)
                else:
                    nxt_ = b * NG + g + 1
                    if nxt_ < NB * NG:
                        stage_U(nxt_ // NG, nxt_ % NG)
                Y_ = k.end_rec()
                bstate["pool"] = None
                if PMODE == 0:
                    k.play_merged(X_)
                    k.play_merged(Y_)
                else:
                    k.play_merged(X_, Y_)
            for tt in range(TPG):
                for half in range(2):
                    hsl = slice(half * 512, (half + 1) * 512)
                    ps = psum()
                    mm(ps, ps[:, :], [(mixT[:, c_, tt * 128:(tt + 1) * 128], woutb[:, c_, hsl]) for c_ in range(8)],
                       R=[woutb] + mixT.subs)
                    k.op("dve", lambda e, ps=ps, hsl=hsl: e.tensor_tensor(out=ps[:, :], in0=ps[:, :], in1=gt1t[:, hsl],
                                                                          op=ALU.mult), R=[gt1t], W=[ps])
                    k.op("dve", lambda e, ps=ps, hsl=hsl, tt=tt: e.tensor_tensor(out=xg[:, tt, hsl], in0=ps[:, :], in1=xg[:, tt, hsl],
                                                                                 op=ALU.add), R=[ps], W=[xg.subs[tt]])
                k.dma(h1_d[b, t0 + tt * 128:t0 + (tt + 1) * 128, :], xg[:, tt, :], R=[xg.subs[tt]], q="pool")

    if dbg == "h1":
        k.finish()
        return nc

    k.barrier()
    MX.close()
    PSX.close()
    del banks[6:]
    bbanks.extend(TT(nc.alloc_psum_tensor("psbb%d" % i, [128, 1024], BF)) for i in range(2))
    ME_P = ExitStack()
    I32 = mybir.dt.int32
    ltri_f, ones_f = cm[:, 5, :], cm[:, 6, :]

    def psb(*a, **kw):
        return k.sb(*a, scope=ME_P, **kw)
    cmb2 = psb("cmb2", [128, 2, 128], BF)
    k.op("dve", lambda e: e.tensor_copy(out=cmb2[:], in_=cm[:, 5:7, :]), R=[cm], W=[cmb2])
    lgs = psb("lgs", [128, NTC, 32])
    ranks = psb("ranks", [128, NTC, 32])
    m8s = psb("m8s", [128, NTC, 8])
    g4 = psb("g4", [128, NTC, 4])
    dest_f = psb("dest_f", [128, NTC * 4])
    dest_i = psb("dest_i", [128, NTC * 4], I32)
    cnt = psb("cnt", [128, 32])
    lay = psb("lay", [128, 5, 32])
    bs = psb("bs", [128, NBLK]); k.dma(bs[:], bs_d[:, :], W=[bs])
    iop = psb("iop", [128, 8]); k.dma(iop[:], iop_d[:, :], W=[iop])
    be = psb("be", [128, 2, NBLK])
    idxW_f = psb("idxW_f", [128, NBLK, 8])
    idxW_i = psb("idxW_i", [128, NBLK * 8], I32)
    idxW2_f = psb("idxW2_f", [128, NBLK, 8])
    idxW2_i = psb("idxW2_i", [128, NBLK * 8], I32)
    skp = psb("skp", [128, 2, NBLK])
    idxb_f = psb("idxb_f", [128, 2, NBLK])
    idxb_i = psb("idxb_i", [128, 2 * NBLK], I32)
    k.op("dve", lambda e: e.memset(cnt[:], 0.0), W=[cnt])
    k.op("dve", lambda e: e.memset(dest_f[:], 0.0), W=[dest_f])

    ME_R = ExitStack()

    def rsb(*a, **kw):
        return k.sb(*a, scope=ME_R, **kw)
    u2tm = rsb("u2tm", [128, NTC, D], BF, nsub=NTC)
    u2f = rsb("u2f", [128, 8, 128])
    h2 = [rsb("h2", [128, D]) for _ in range(2)]
    G2bc = [rsb("G2bc", [128, D]) for _ in range(NB)]
    SH2bc = [rsb("SH2bc", [128, D]) for _ in range(NB)]
    maskb = rsb("maskb", [128, 32], BF)
    rt_ = rsb("rt", [128, 64])
    for b in range(NB):
        k.dma(SH2bc[b][:], modscr_d[b, 2, :].partition_broadcast(128), W=[SH2bc[b]])
        k.dma(G2bc[b][:], modscr_d[b, 3, :].partition_broadcast(128), W=[G2bc[b]])
    for tile in range(NTC):
        b, tt = tile // NT, tile % NT
        ht = h2[tile % 2]
        k.dma(ht[:], h1_d[b, tt * 128:(tt + 1) * 128, :], W=[ht])

        def ev2(kc, psap, sc, bi, ps):
            k.op("act", lambda e: e.activation(out=u2f[:, kc, :], in_=psap, func=AF.Identity, scale=sc, bias=bi),
                 R=[ps, GS[b]], W=[u2f])
        norm_T(ht[:], ht, GS[b], 16, ev2)
        k.op("dve", lambda e: e.tensor_tensor(out=ht[:], in0=xn[0][:], in1=G2bc[b][:], op=ALU.mult), R=[xn[0], G2bc[b]], W=[ht])
        k.op("dve", lambda e: e.tensor_tensor(out=u2tm[:, tile, :], in0=ht[:], in1=SH2bc[b][:], op=ALU.add),
             R=[ht, SH2bc[b]], W=[u2tm.subs[tile]])
        ps = psum()
        mm(ps, ps[:, 0:32], [(u2f[:, kc, :], rw[:, kc, :]) for kc in range(8)], R=[u2f, rw])
        lg = lgs[:, tile, :]
        m8 = m8s[:, tile, :]
        k.op("dve", lambda e: e.tensor_tensor(out=lg, in0=ps[:, 0:32], in1=rb[:], op=ALU.add), R=[ps, rb], W=[lgs])
        k.op("dve", lambda e: e.max(out=m8, in_=lg), R=[lgs], W=[m8s])
        k.op("dve", lambda e: e.tensor_scalar(out=maskb[:], in0=lg, scalar1=m8s[:, tile, 3:4], scalar2=None, op0=ALU.is_ge),
             R=[lgs, m8s], W=[maskb])
        psr = psum()
        mm(psr, psr[:, 0:32], [(cmb2[:, 0, :], maskb[:])], R=[cmb2, maskb])
        k.op("dve", lambda e: e.tensor_tensor(out=ranks[:, tile, :], in0=psr[:, 0:32], in1=cnt[:], op=ALU.add),
             R=[psr, cnt], W=[ranks])
        psc = psum()
        mm(psc, psc[:, 0:32], [(cmb2[:, 1, :], maskb[:])], R=[cmb2, maskb])
        k.op("dve", lambda e: e.tensor_tensor(out=cnt[:], in0=psc[:, 0:32], in1=cnt[:], op=ALU.add), R=[psc], W=[cnt])
        negm, ssum, ex4 = rt_[:, 0:1], rt_[:, 1:2], rt_[:, 4:8]
        k.op("dve", lambda e: e.tensor_scalar(out=negm, in0=m8s[:, tile, 0:1], scalar1=-1.0, scalar2=None, op0=ALU.mult),
             R=[m8s], W=[rt_])
        k.op("act", lambda e: e.activation(out=ex4, in_=m8s[:, tile, 0:4], func=AF.Exp, bias=negm, scale=1.0), R=[m8s], W=[rt_])
        k.op("dve", lambda e: e.tensor_reduce(out=ssum, in_=ex4, axis=mybir.AxisListType.X, op=ALU.add), R=[], W=[rt_])
        k.op("dve", lambda e: e.reciprocal(out=ssum, in_=ssum), R=[], W=[rt_])
        k.op("dve", lambda e: e.tensor_scalar(out=g4[:, tile, :], in0=ex4, scalar1=ssum, scalar2=None, op0=ALU.mult),
             R=[rt_], W=[g4])
    nbt, padded, pend, pstart, ones32 = (lay[:, i, :] for i in range(5))
    k.op("dve", lambda e: e.memset(ones32, 1.0), W=[lay])
    k.op("dve", lambda e: e.tensor_scalar(out=nbt, in0=cnt[:], scalar1=0.0, scalar2=None, op0=ALU.is_gt), R=[cnt], W=[lay])
    for j in range(1, (NTC * 128) // BLK):
        k.op("dve", lambda e: e.scalar_tensor_tensor(out=nbt, in0=cnt[:], scalar=float(BLK * j), in1=nbt, op0=ALU.is_gt,
                                                     op1=ALU.add), R=[cnt], W=[lay])
    k.op("dve", lambda e: e.tensor_scalar(out=padded, in0=nbt, scalar1=float(BLK), scalar2=None, op0=ALU.mult), R=[], W=[lay])
    k.op("dve", lambda e: e.tensor_tensor_scan(out=pend, data0=ones32, data1=padded, initial=0.0, op0=ALU.mult, op1=ALU.add),
         R=[], W=[lay])
    k.op("dve", lambda e: e.tensor_tensor(out=pstart, in0=pend, in1=padded, op=ALU.subtract), R=[], W=[lay])
    k.op("dve", lambda e: e.memset(be[:, 0, :], 0.0), W=[be])
    for e_ in range(32):
        k.op("dve", lambda e: e.scalar_tensor_tensor(out=be[:, 0, :], in0=bs[:], scalar=lay[:, 2, e_:e_ + 1], in1=be[:, 0, :],
                                                     op0=ALU.is_ge, op1=ALU.add), R=[bs, lay], W=[be])
    k.op("dve", lambda e: e.tensor_scalar(out=be[:, 0, :], in0=be[:, 0, :], scalar1=float(E - 1), scalar2=None, op0=ALU.min),
         R=[], W=[be])
    k.op("dve", lambda e: e.tensor_scalar(out=be[:, 1, :], in0=be[:, 0, :], scalar1=float(D), scalar2=None, op0=ALU.mult),
         R=[], W=[be])
    for kc in range(8):
        k.op("dve", lambda e: e.tensor_scalar(out=idxW_f[:, :, kc], in0=be[:, 1, :], scalar1=iop[:, kc:kc + 1], scalar2=None,
                                              op0=ALU.add), R=[be, iop], W=[idxW_f])
    BIG = 1.0e6
    k.op("dve", lambda e: e.memset(skp[:], 0.0), W=[skp])
    k.op("dve", lambda e: e.tensor_tensor(out=skp[:, 0, 2:NBLK], in0=be[:, 0, 2:NBLK], in1=be[:, 0, 0:NBLK - 2], op=ALU.is_equal),
         R=[be], W=[skp])
    k.op("dve", lambda e: e.tensor_tensor(out=skp[:, 1, 1:NBLK], in0=be[:, 0, 1:NBLK], in1=be[:, 0, 0:NBLK - 1], op=ALU.is_equal),
         R=[be], W=[skp])
    k.op("dve", lambda e: e.tensor_scalar(out=skp[:], in0=skp[:], scalar1=BIG, scalar2=None, op0=ALU.mult), R=[], W=[skp])
    for kc in range(8):
        k.op("dve", lambda e: e.tensor_tensor(out=idxW2_f[:, :, kc], in0=idxW_f[:, :, kc], in1=skp[:, 1, :], op=ALU.add),
             R=[idxW_f, skp], W=[idxW2_f])
    k.op("dve", lambda e: e.tensor_copy(out=idxW2_i[:], in_=idxW2_f[:].rearrange("p a b -> p (a b)")), R=[idxW2_f], W=[idxW2_i])
    for kc in range(8):
        k.op("dve", lambda e: e.tensor_tensor(out=idxW_f[:, :, kc], in0=idxW_f[:, :, kc], in1=skp[:, 0, :], op=ALU.add),
             R=[skp], W=[idxW_f])
    k.op("dve", lambda e: e.tensor_copy(out=idxW_i[:], in_=idxW_f[:].rearrange("p a b -> p (a b)")), R=[idxW_f], W=[idxW_i])
    k.op("dve", lambda e: e.tensor_scalar(out=idxb_f[:, 0, :], in0=be[:, 0, :], scalar1=128.0, scalar2=iop[:, 0:1],
                                          op0=ALU.mult, op1=ALU.add), R=[be, iop], W=[idxb_f])
    k.op("dve", lambda e: e.tensor_copy(out=idxb_f[:, 1, :], in_=be[:, 0, :]), R=[be], W=[idxb_f])
    k.op("dve", lambda e: e.tensor_copy(out=idxb_i[:], in_=idxb_f[:].rearrange("p a b -> p (a b)")), R=[idxb_f], W=[idxb_i])
    for tile in range(NTC):
        k.op("dve", lambda e: e.tensor_tensor(out=ranks[:, tile, :], in0=ranks[:, tile, :], in1=pstart, op=ALU.add),
             R=[lay], W=[ranks])
        for kk_ in range(4):
            k.op("dve", lambda e: e.scalar_tensor_tensor(out=rt_[:, 32:64], in0=lgs[:, tile, :], scalar=m8s[:, tile, kk_:kk_ + 1],
                                                         in1=ranks[:, tile, :], op0=ALU.is_equal, op1=ALU.mult,
                                                         accum_out=dest_f[:, tile * 4 + kk_:tile * 4 + kk_ + 1]),
                 R=[lgs, m8s, ranks], W=[rt_, dest_f])
    k.op("dve", lambda e: e.tensor_copy(out=dest_i[:], in_=dest_f[:]), R=[dest_f], W=[dest_i])
    if dbg == "moe":
        dbg_d = nc.dram_tensor("dbg", [128, NTC * 4 + NTC * 4 + 32 + NBLK], F32, kind="ExternalOutput").ap()
        k.dma(dbg_d[:, 0:NTC * 4], dest_f[:], R=[dest_f])
        k.dma(dbg_d[:, NTC * 4:NTC * 8], g4[:].rearrange("p a b -> p (a b)"), R=[g4])
        k.dma(dbg_d[:, NTC * 8:NTC * 8 + 32], cnt[:], R=[cnt])
        k.dma(dbg_d[:, NTC * 8 + 32:NTC * 8 + 32 + NBLK], be[:, 0, :], R=[be])
    for tile in range(NTC):
        for kk_ in range(4):
            c_ = tile * 4 + kk_
            k.dma(xs_d[:, :], u2tm[:, tile, :], R=[u2tm.subs[tile], dest_i], q="pool", indirect=("scatter", dest_i[:, c_:c_ + 1]))
    k.barrier()
    ME_R.close()

    ME_X = ExitStack()

    def xsb(*a, **kw):
        return k.sb(*a, scope=ME_X, **kw)
    W1b = [xsb("W1b", [128, 8, 2 * D], BF, nsub=8) for _ in range(2)]
    W2b = xsb("W2b", [128, 8, D], BF, nsub=8)
    xrow = [xsb("xrow", [128, 4, D], BF) for _ in range(2)]
    xsT = [xsb("xsT", [128, 8, BLK], BF, nsub=8) for _ in range(2)]
    actT = [xsb("actT", [128, 8, BLK], BF, nsub=8) for _ in range(2)]
    et = [[xsb("et", [128, BLK]) for _ in range(4)] for _ in range(2)]
    yrow = [xsb("yrow", [128, D]) for _ in range(2)]
    b2bc = [xsb("b2bc", [128, D]) for _ in range(2)]
    b1blk = [xsb("b1blk", [128, 16]) for _ in range(2)]
    ectr = {"n": 0, "y": 0}
    def load_weights(blk):
        w1 = W1b[blk % 2]
        b1t, b2t = b1blk[blk % 2], b2bc[blk % 2]
        k.dma(b1t[:], b1_d[:, :], R=[idxb_i], W=[b1t], q="pool", indirect=("gather", idxb_i[:, blk:blk + 1]))
        k.dma(b2t[:], b2_d[:, :], R=[idxb_i], W=[b2t], q="pool", indirect=("gather", idxb_i[:, NBLK + blk:NBLK + blk + 1]))
        for kc in range(8):
            c_ = blk * 8 + kc
            k.dma(w1[:, kc, :], w1_d[:, :], R=[idxW_i], W=[w1.subs[kc]], q="pool",
                  indirect=("gather", idxW_i[:, c_:c_ + 1], E * D - 1))

    def load_w2(blk):
        for fc in range(8):
            c_ = blk * 8 + fc
            k.dma(W2b[:, fc, :], w2_d[:, :], R=[idxW2_i], W=[W2b.subs[fc]], q="pool",
                  indirect=("gather", idxW2_i[:, c_:c_ + 1], E * D - 1))

    def load_x(blk):
        k.dma(xrow[blk % 2][:], xs_d[blk * BLK:(blk + 1) * BLK, :].rearrange("(s p) d -> p s d", p=128), W=[xrow[blk % 2]])

    def transposes(blk):
        xT = xsT[blk % 2]
        xr = xrow[blk % 2]
        for kc in range(8):
            if kc % 2 == 0:
                pb = psum_bf()
            po = (kc % 2) * 512
            for s_ in range(4):
                k.op("pe", lambda e: e.transpose(out=pb[:, po + s_ * 128:po + (s_ + 1) * 128], in_=xr[:, s_, kc * 128:(kc + 1) * 128],
                                                 identity=cmb[:]), R=[xr, cmb], W=[pb])
            k.op("act", lambda e: e.activation(out=xT[:, kc, :], in_=pb[:, po:po + 512], func=AF.Copy), R=[pb], W=[xT.subs[kc]])

    load_weights(0)
    load_x(0)
    transposes(0)
    for blk in range(NBLK):
        w1 = W1b[blk % 2]
        b1t, b2t = b1blk[blk % 2], b2bc[blk % 2]
        load_w2(blk)
        if blk + 1 < NBLK:
            load_weights(blk + 1)
            load_x(blk + 1)
        xT = xsT[blk % 2]
        aT = actT[blk % 2]
        for fc in range(8):
            xg_, sg_, xl_, tt_ = et[ectr["n"] % 2]
            ectr["n"] += 1
            psg = psum()
            mm(psg, psg[:, :], [(w1[:, kc, fc * 128:(fc + 1) * 128], xT[:, kc, :]) for kc in range(8)], R=w1.subs + xT.subs)
            psl = psum()
            mm(psl, psl[:, :], [(w1[:, kc, D + fc * 128:D + (fc + 1) * 128], xT[:, kc, :]) for kc in range(8)], R=w1.subs + xT.subs)
            k.op("dve", lambda e: e.tensor_scalar(out=xg_[:], in0=psg[:, :], scalar1=b1t[:, fc:fc + 1], scalar2=7.0,
                                                  op0=ALU.add, op1=ALU.min), R=[psg, b1t], W=[xg_])
            k.op("act", lambda e: e.activation(out=sg_[:], in_=xg_[:], func=AF.Sigmoid, scale=1.702), R=[xg_], W=[sg_])
            k.op("dve", lambda e: e.tensor_scalar(out=xl_[:], in0=psl[:, :], scalar1=b1t[:, 8 + fc:9 + fc], scalar2=7.0,
                                                  op0=ALU.add, op1=ALU.min), R=[psl, b1t], W=[xl_])
            k.op("dve", lambda e: e.tensor_scalar(out=xl_[:], in0=xl_[:], scalar1=-7.0, scalar2=1.0, op0=ALU.max, op1=ALU.add),
                 R=[], W=[xl_])
            k.op("dve", lambda e: e.tensor_tensor(out=tt_[:], in0=xg_[:], in1=sg_[:], op=ALU.mult), R=[xg_, sg_], W=[tt_])
            k.op("dve", lambda e: e.tensor_tensor(out=aT[:, fc, :], in0=tt_[:], in1=xl_[:], op=ALU.mult), R=[tt_, xl_],
                 W=[aT.subs[fc]])
        if blk + 1 < NBLK:
            transposes(blk + 1)
        for s_ in range(4):
            yr = yrow[ectr["y"] % 2]
            ectr["y"] += 1
            for half in range(2):
                hsl = slice(half * 512, (half + 1) * 512)
                ps = psum()
                mm(ps, ps[:, :], [(aT[:, fc, s_ * 128:(s_ + 1) * 128], W2b[:, fc, hsl]) for fc in range(8)], R=aT.subs + W2b.subs)
                k.op("dve", lambda e: e.tensor_tensor(out=yr[:, hsl], in0=ps[:, :], in1=b2t[:, hsl], op=ALU.add), R=[ps, b2t], W=[yr])
            k.dma(yb_d[blk * BLK + s_ * 128:blk * BLK + (s_ + 1) * 128, :], yr[:], R=[yr])
    k.barrier()
    ME_X.close()

    ME_C = ExitStack()

    def csb(*a, **kw):
        return k.sb(*a, scope=ME_C, **kw)
    ygat = [[csb("ygat", [128, D]) for _ in range(4)] for _ in range(2)]
    h3 = [csb("h3", [128, D]) for _ in range(2)]
    fg = csb("fg", [128, D]); k.dma(fg[:], fg_d[:, :], W=[fg])
    for b in range(NB):
        gtb[b][1] = csb("gtb2", [128, D])
        k.dma(gtb[b][1][:], modscr_d[b, 1, :].partition_broadcast(128), W=[gtb[b][1]])
    for tile in range(NTC):
        b, tt = tile // NT, tile % NT
        ht = h3[tile % 2]
        yg = ygat[tile % 2]
        for kk_ in range(4):
            c_ = tile * 4 + kk_
            k.dma(yg[kk_][:], yb_d[:, :], R=[dest_i], W=[yg[kk_]], q="pool", indirect=("gather", dest_i[:, c_:c_ + 1]))
        k.dma(ht[:], h1_d[b, tt * 128:(tt + 1) * 128, :], W=[ht])
        k.op("dve", lambda e: e.tensor_scalar(out=yg[0][:], in0=yg[0][:], scalar1=g4[:, tile, 0:1], scalar2=None, op0=ALU.mult),
             R=[g4], W=[yg[0]])
        for kk_ in range(1, 4):
            k.op("dve", lambda e: e.scalar_tensor_tensor(out=yg[0][:], in0=yg[kk_][:], scalar=g4[:, tile, kk_:kk_ + 1], in1=yg[0][:],
                                                         op0=ALU.mult, op1=ALU.add), R=[yg[kk_], g4], W=[yg[0]])
        if dbg == "moe":
            k.dma(out_d[b, tt * 128:(tt + 1) * 128, :], yg[0][:], R=[yg[0]])
            continue
        k.op("dve", lambda e: e.tensor_tensor(out=yg[0][:], in0=yg[0][:], in1=gtb[b][1][:], op=ALU.mult), R=[gtb[b][1]], W=[yg[0]])
        k.op("dve", lambda e: e.tensor_tensor(out=ht[:], in0=ht[:], in1=yg[0][:], op=ALU.add), R=[yg[0]], W=[ht])
        st, i = rstd_of(ht[:], ht)
        k.op("dve", lambda e: e.scalar_tensor_tensor(out=ht[:], in0=ht[:], scalar=st[:, 3:4], in1=fg[:], op0=ALU.mult, op1=ALU.mult),
             R=[st, fg], W=[ht])
        k.dma(out_d[b, tt * 128:(tt + 1) * 128, :], ht[:], R=[ht])
    k.finish()
    return nc


def make_consts(NB, GM):
    idx = np.arange(128)
    same = (idx[:, None] // 64) == (idx[None, :] // 64)
    ident = np.eye(128, dtype=np.float32)
    m_su = (same & (idx[:, None] < idx[None, :])).astype(np.float32)
    m_sl = (same & (idx[:, None] > idx[None, :])).astype(np.float32)
    m_ui = (same & (idx[:, None] <= idx[None, :])).astype(np.float32)
    bones = same.astype(np.float32)
    ltri = (idx[:, None] < idx[None, :]).astype(np.float32)
    cm = np.ascontiguousarray(np.stack([ident, m_su, m_sl, m_ui, bones, ltri, np.ones((128, 128), np.float32)], axis=1))
    cm2 = np.ascontiguousarray(np.stack([np.concatenate([m, m], axis=1) for m in (m_su, m_sl, m_ui, ident)], axis=1))
    rm = np.ones((128, GM), np.float32)
    rm[:, ::64] = 0.0
    selb = np.zeros((NB, NB, 128), np.float32)
    for b in range(NB):
        selb[b, b, :] = 1.0
    return cm, rm, selb, np.ones((1, NB), np.float32), cm2


def fm(v, n):
    return np.ascontiguousarray(np.asarray(v, np.float32).reshape(n, 128).T)


def prep_shared(inp, NB, GM, E, T=2048):
    f = lambda a: np.asarray(a, np.float32)
    cm, rm, selb, ones, cm2 = make_consts(NB, GM)
    w1 = f(inp["exp_w1"])[0][:E]
    w1 = np.ascontiguousarray(np.concatenate([w1[:, :, 0::2], w1[:, :, 1::2]], axis=2))
    b1 = f(inp["exp_b1"])[0][:E]
    b1d = np.concatenate([b1[:, 0::2], b1[:, 1::2]], axis=1)
    b1_rows = np.ascontiguousarray(b1d.reshape(E, 16, 128).transpose(0, 2, 1).reshape(E * 128, 16))
    NBLK = (NB * T * 4) // 512 + 32
    blkstart = np.ascontiguousarray(np.broadcast_to((np.arange(NBLK, dtype=np.float32) * 512.0)[None, :], (128, NBLK)))
    iotaP = np.ascontiguousarray((np.arange(8, dtype=np.float32)[None, :] * 128.0 + np.arange(128, dtype=np.float32)[:, None]))
    vecs = np.stack([fm(f(inp[n])[0], 4) for n in
                     ("rwkv_w0", "rwkv_a0", "rwkv_k_k", "rwkv_k_a", "rwkv_r_k", "rwkv_ln_w", "rwkv_ln_b")], axis=1)
    sh = {
        "ada_w": np.ascontiguousarray(f(inp["ada_w"])[0]),
        "ada_b_fm": fm(f(inp["ada_b"])[0], 48),
        "ada_b_row": np.ascontiguousarray(f(inp["ada_b"])[0][None, :]),
        "g12_fm": np.ascontiguousarray(np.stack([fm(f(inp["norm1_g"])[0], 8), fm(f(inp["norm2_g"])[0], 8)], axis=1)),
        "w_in": np.ascontiguousarray(f(inp["w_in"])[0]),
        "conv_w_fm": np.ascontiguousarray(f(inp["conv_w"])[0].reshape(3, 4, 128).transpose(2, 1, 0)),
        "conv_gn_fm": fm(f(inp["conv_gn"])[0], 4),
        "mu_fm": fm(f(inp["rwkv_mu"])[0], 14),
        "vecs_fm": np.ascontiguousarray(vecs),
        "lora_up": np.ascontiguousarray(np.concatenate([f(inp["rwkv_w_up"])[0], f(inp["rwkv_a_up"])[0]], axis=0)),
        "g_up": np.ascontiguousarray(f(inp["rwkv_g_up"])[0]),
        "w_out": np.ascontiguousarray(f(inp["w_out"])[0]),
        "router_w": np.ascontiguousarray(f(inp["router_w"])[0][:, :32]),
        "router_b_bc": np.ascontiguousarray(np.broadcast_to(f(inp["router_b"])[0][None, :32], (128, 32))),
        "w1": w1.reshape(E * D, 2 * D),
        "b1_rows": b1_rows,
        "blkstart": blkstart, "iotaP": iotaP,
        "g2_row": np.ascontiguousarray(np.broadcast_to(f(inp["norm2_g"])[0][None, :], (NB, D))),
        "w2": np.ascontiguousarray(f(inp["exp_w2"])[0][:E]).reshape(E * D, D),
        "b2": np.ascontiguousarray(f(inp["exp_b2"])[0][:E]),
        "final_g_bc": np.ascontiguousarray(np.broadcast_to(f(inp["final_g"])[None, :], (128, D))),
        "cmats": cm, "cmats2": cm2, "resetmask": rm, "selb": selb, "ones_row": ones,
    }
    return sh


def core_inputs(sh, x, c, b0, NB):
    m = dict(sh)
    m["x"] = np.ascontiguousarray(x[b0:b0 + NB])
    cc = np.asarray(c[b0:b0 + NB], np.float32)
    m["cT"] = np.ascontiguousarray(cc.reshape(NB, 8, 128).transpose(2, 1, 0))
    return m


def kernel(**inputs):
    x = np.asarray(inputs["x"], np.float32)
    c = np.asarray(inputs["c"], np.float32)
    B, T, _ = x.shape
    n = 8
    NB = B // n
    E = 32
    GM = 256
    sh = prep_shared(inputs, NB, GM, E, T)
    nc = build(T, NB, E, GM)
    in_maps = [core_inputs(sh, x, c, i * NB, NB) for i in range(n)]
    res = run_bass_kernel_spmd(nc, in_maps, core_ids=list(range(n)))
    return np.concatenate([r["out"] for r in res.results], axis=0).astype(np.float32)
```

```python
import numpy as np
from contextlib import ExitStack
import concourse.bass as bass
import concourse.mybir as mybir
from concourse.bass_utils import run_bass_kernel_spmd

F32 = mybir.dt.float32
BF = mybir.dt.bfloat16
AF = mybir.ActivationFunctionType
ALU = mybir.AluOpType

D = 1024
NORM_EPS = 1e-5
GN_EPS = 64e-5
NDS = 12


class Buf:
    __slots__ = ("lw", "rd")

    def __init__(self):
        self.lw = None
        self.rd = {}


class TT:
    def __init__(self, t, nsub=0):
        self.t = t
        self.b = Buf()
        self.subs = [Buf() for _ in range(nsub)]

    def __getitem__(self, idx):
        return self.t[idx]


class _Rec:
    def __init__(self):
        self.call = None

    def __getattr__(self, name):
        def f(*a, **kw):
            self.call = (name, a, kw)
            return self
        return f


class KB:
    def __init__(self, nc):
        self.nc = nc
        self.eng = {"pe": nc.tensor, "act": nc.scalar, "dve": nc.vector, "pool": nc.gpsimd, "sp": nc.sync}
        self.sem = {n: nc.alloc_semaphore("s_" + n) for n in ["pe", "act", "dve", "pool"]}
        self.cnt = {n: 0 for n in ["pe", "act", "dve", "pool"]}
        self.waited = {}
        self.dsems = {q: [nc.alloc_semaphore("d%s%d" % (q, i)) for i in range(NDS)] for q in ("sp", "pool")}
        self.dtot = {q: [0] * NDS for q in ("sp", "pool")}
        self.dnext = {"sp": 0, "pool": 0}
        self.nid = 0
        self.bregs = {}
        self.rec = None
        self.last_tile = 2

    def _wait(self, eng, tok):
        if tok is None:
            return
        key, sem, val = tok
        if eng == "pe" and key == "pe":
            return
        if self.waited.get((eng, key), 0) >= val:
            return
        self.waited[(eng, key)] = val
        self.eng[eng].wait_ge(sem, val)

    def _deps(self, eng, R, W):
        for b in R:
            self._wait(eng, b.lw)
        for b in W:
            self._wait(eng, b.lw)
            for t in b.rd.values():
                self._wait(eng, t)

    def _post(self, tok, R, W):
        for b in R:
            b.rd[tok[0]] = tok
        for b in W:
            b.lw = tok
            b.rd = {}

    def op(self, eng, fn, R=(), W=()):
        R = [x.b if isinstance(x, TT) else x for x in R]
        W = [x.b if isinstance(x, TT) else x for x in W]
        if self.rec is not None:
            r = _Rec()
            fn(r)
            self.rec.append(("op", eng, r.call, R, W))
            return None
        if eng == "pe":
            tid = self._pe_tile(fn)
            if tid != self.last_tile and tid in (0, 1) and self.last_tile in (0, 1) and self.cnt["pe"] > 0:
                self.eng["pe"].wait_ge(self.sem["pe"], self.cnt["pe"])
            self.last_tile = tid
        self._deps(eng, R, W)
        ins = fn(self.eng[eng])
        self.cnt[eng] += 1
        ins.then_inc(self.sem[eng], 1)
        tok = (eng, self.sem[eng], self.cnt[eng])
        self._post(tok, R, W)
        return tok

    def dma(self, out, in_, R=(), W=(), q="sp", indirect=None):
        R = [x.b if isinstance(x, TT) else x for x in R]
        W = [x.b if isinstance(x, TT) else x for x in W]
        if self.rec is not None:
            self.rec.append(("dma", out, in_, R, W, q, indirect))
            return None
        i = self.dnext[q]
        self.dnext[q] = (i + 1) % NDS
        sem = self.dsems[q][i]
        key = "d%s%d" % (q, i)
        if self.dtot[q][i] > 0:
            self._wait(q, (key, sem, self.dtot[q][i]))
        self._deps(q, R, W)
        if indirect is None:
            ins = self.eng[q].dma_start(out=out, in_=in_)
        else:
            kind, idx = indirect[0], indirect[1]
            off = bass.IndirectOffsetOnAxis(ap=idx, axis=0)
            extra = {}
            if len(indirect) > 2:
                if indirect[2] not in self.bregs:
                    self.bregs[indirect[2]] = self.eng[q].to_reg(indirect[2])
                extra = dict(bounds_check=self.bregs[indirect[2]], oob_is_err=False)
            ins = self.eng[q].indirect_dma_start(out=out, out_offset=(off if kind == "scatter" else None), in_=in_,
                                                 in_offset=(off if kind == "gather" else None), **extra)
        self.dtot[q][i] += 16
        ins.then_inc(sem, 16)
        tok = (key, sem, self.dtot[q][i])
        self._post(tok, R, W)
        return tok

    def _pe_tile(self, fn):
        r = _Rec()
        fn(r)
        name, a, kw = r.call
        ap = kw.get("in_") if name == "transpose" else (kw.get("lhsT") if "lhsT" in kw else (a[1] if len(a) > 1 else None))
        if ap is None:
            return 2
        st, sz = ap.start_partition(), ap.partition_size()
        if sz > 64:
            return 2
        return 0 if st < 64 else 1

    def begin_rec(self):
        self.rec = []

    def end_rec(self):
        r, self.rec = self.rec, None
        return r

    def _play1(self, it):
        if it[0] == "op":
            _, eng, (name, a, kw), R, W = it
            self.op(eng, lambda e: getattr(e, name)(*a, **kw), R, W)
        else:
            _, out, in_, R, W, q, indirect = it
            self.dma(out, in_, R, W, q, indirect)

    def play_merged(self, X, Y=()):
        nx, ny = len(X), len(Y)
        ix = iy = 0
        def is_pe(it):
            return it[0] == "op" and it[1] == "pe"
        while ix < nx or iy < ny:
            if iy >= ny or (ix < nx and ix * ny <= iy * nx):
                self._play1(X[ix]); ix += 1
                while ix < nx and is_pe(X[ix - 1]) and is_pe(X[ix]):
                    self._play1(X[ix]); ix += 1
            else:
                self._play1(Y[iy]); iy += 1
                while iy < ny and is_pe(Y[iy - 1]) and is_pe(Y[iy]):
                    self._play1(Y[iy]); iy += 1

    def sb(self, name, shape, dt=F32, nsub=0, scope=None):
        self.nid += 1
        nm = "%s_%d" % (name, self.nid)
        if scope is None:
            return TT(self.nc.alloc_sbuf_tensor(nm, list(shape), dt), nsub)
        return TT(scope.enter_context(self.nc.sbuf_tensor(nm, list(shape), dt)), nsub)

    def barrier(self):
        names = ["pe", "act", "dve", "pool", "sp"]
        for e in names:
            for n in ["pe", "act", "dve", "pool"]:
                if n != e and self.cnt[n] > 0:
                    self._wait(e, (n, self.sem[n], self.cnt[n]))
            for q in ("sp", "pool"):
                for i in range(NDS):
                    if self.dtot[q][i] > 0:
                        self._wait(e, ("d%s%d" % (q, i), self.dsems[q][i], self.dtot[q][i]))

    def finish(self):
        for q in ("sp", "pool"):
            for i in range(NDS):
                if self.dtot[q][i] > 0:
                    self.eng["sp"].wait_ge(self.dsems[q][i], self.dtot[q][i])
        for n in ["pe", "act", "dve", "pool"]:
            if self.cnt[n] > 0:
                self.eng["sp"].wait_ge(self.sem[n], self.cnt[n])


KSTOP = 0
GVAR = 0
PMODE = 1


def build(T, NB, E, GM=256, dbg=None):
    nc = bass.Bass("TRN2", target_bir_lowering=False)
    k = KB(nc)
    NT = T // 128
    NG = T // GM
    TPG = GM // 128
    EG = min(512, T)

    def din(name, shape):
        return nc.dram_tensor(name, list(shape), F32, kind="ExternalInput").ap()

    x_d = din("x", [NB, T, D])
    cT_d = din("cT", [128, 8, NB])
    adaw_d = din("ada_w", [D, 6 * D])
    adab_fm_d = din("ada_b_fm", [128, 48])
    adab_row_d = din("ada_b_row", [1, 6 * D])
    g12_d = din("g12_fm", [128, 2, 8])
    win_d = din("w_in", [D, 3328])
    convw_d = din("conv_w_fm", [128, 4, 3])
    convgn_d = din("conv_gn_fm", [128, 4])
    mu_d = din("mu_fm", [128, 14])
    vecs_d = din("vecs_fm", [128, 7, 4])
    lora_d = din("lora_up", [128, 512])
    gup_d = din("g_up", [128, 512])
    wout_d = din("w_out", [D, D])
    rw_d = din("router_w", [D, 32])
    rb_d = din("router_b_bc", [128, 32])
    w1_d = din("w1", [E * D, 2 * D])
    b1_d = din("b1_rows", [E * 128, 16])
    w2_d = din("w2", [E * D, D])
    b2_d = din("b2", [E, D])
    NTC = NB * (T // 128)
    BLK = 512
    NBLK = (NTC * 128 * 4) // BLK + 32
    bs_d = din("blkstart", [128, NBLK])
    iop_d = din("iotaP", [128, 8])
    xs_d = nc.dram_tensor("xs_scr", [NBLK * BLK, D], BF, kind="Internal").ap()
    yb_d = nc.dram_tensor("yb_scr", [NBLK * BLK, D], F32, kind="Internal").ap()
    fg_d = din("final_g_bc", [128, D])
    cm_d = din("cmats", [128, 7, 128])
    rm_d = din("resetmask", [128, GM])
    cm2_d = din("cmats2", [128, 4, 256])
    sel_d = din("selb", [NB, NB, 128])
    ones_d = din("ones_row", [1, NB])
    g2row_d = din("g2_row", [NB, D])
    modscr_d = nc.dram_tensor("modscr", [NB, 4, D], F32, kind="Internal").ap()
    out_d = nc.dram_tensor("out", [NB, T, D], F32, kind="ExternalOutput").ap()
    h1_d = nc.dram_tensor("h1_scratch", [NB, T, D], F32, kind=("ExternalOutput" if dbg else "Internal")).ap()

    banks = [TT(nc.alloc_psum_tensor("psb%d" % i, [128, 512], F32)) for i in range(6)]
    PSX = ExitStack()
    banks += [TT(PSX.enter_context(nc.psum_tensor("psbx%d" % i, [128, 512], F32))) for i in range(2)]
    bbanks = []
    bstate = {"n": 0, "b": 0, "pool": None, "nx": 0, "ny": 0}

    reserved = set()

    def psum():
        if bstate["pool"] == "X":
            bstate["nx"] += 1
            return banks[bstate["nx"] % 6]
        if bstate["pool"] == "Y":
            bstate["ny"] += 1
            return banks[6 + bstate["ny"] % 2]
        while True:
            i = bstate["n"] % len(banks)
            bstate["n"] += 1
            if i not in reserved:
                return banks[i]

    def psum_bf():
        b = bbanks[bstate["b"] % 2]
        bstate["b"] += 1
        return b

    def mm(ps, out_ap, pairs, R):
        n = len(pairs)
        tok = None
        for i, (l, r) in enumerate(pairs):
            tok = k.op("pe", lambda e, l=l, r=r, i=i: e.matmul(out_ap, l, r, start=(i == 0), stop=(i == n - 1)),
                       R=R, W=[ps])
        return tok

    P0 = ExitStack()
    MX = ExitStack()
    ME = ExitStack()
    cm = k.sb("cm", [128, 7, 128])
    k.dma(cm[:], cm_d[:, :, :], W=[cm])
    ident, m_su, m_sl, m_ui, bones = (cm[:, i, :] for i in range(5))
    cmb = k.sb("cmb", [128, 128], BF)
    k.op("dve", lambda e: e.tensor_copy(out=cmb[:], in_=ident), R=[cm], W=[cmb])
    smallw = k.sb("smallw", [128, 48 + 16 + 12 + 4 + 14 + 28])
    o = 0
    adab_fm = smallw[:, o:o + 48]; k.dma(adab_fm, adab_fm_d[:, :], W=[smallw]); o += 48
    g12 = smallw[:, o:o + 16]; k.dma(g12, g12_d.rearrange("p a b -> p (a b)"), W=[smallw]); o += 16
    cw = smallw[:, o:o + 12]; k.dma(cw, convw_d.rearrange("p a b -> p (a b)"), W=[smallw]); o += 12
    cgn = smallw[:, o:o + 4]; k.dma(cgn, convgn_d[:, :], W=[smallw]); o += 4
    mu = smallw[:, o:o + 14]; k.dma(mu, mu_d[:, :], W=[smallw]); o += 14
    vecs = smallw[:, o:o + 28]; k.dma(vecs, vecs_d.rearrange("p a b -> p (a b)"), W=[smallw]); o += 28

    def vec(v, i):
        return vecs[:, v * 4 + i:v * 4 + i + 1]

    rw = k.sb("rw", [128, 8, 32]); k.dma(rw[:], rw_d.rearrange("(c p) n -> p c n", p=128), W=[rw])
    rb = k.sb("rb", [128, 32]); k.dma(rb[:], rb_d[:, :], W=[rb])
    cT = k.sb("cT", [128, 8 * NB]); k.dma(cT[:], cT_d.rearrange("p a b -> p (a b)"), W=[cT])

    small = k.sb("small", [128, 64])
    gtb = [[None, None] for _ in range(NB)]
    GS = [k.sb("GS", [128, 32]) for _ in range(NB)]
    nstat = [k.sb("nstat", [128, 4]) for _ in range(2)]
    xn = [k.sb("xn", [128, D])]
    junk = k.sb("junk", [128, D], BF)
    cact = k.sb("cact", [128, 8 * NB])
    csg = k.sb("csg", [128, 8 * NB])
    modfm = [k.sb("modfm", [128, 48]) for _ in range(NB)]
    winb = k.sb("winb", [128, 8, 3328], BF, scope=MX)
    woutb = k.sb("woutb", [128, 8, D], BF, scope=MX)
    rmask = k.sb("rmask", [128, GM], scope=MX)
    k.dma(rmask[:], rm_d[:, :], W=[rmask])
    lora = k.sb("lora", [128, 512], scope=MX); k.dma(lora[:], lora_d[:, :], W=[lora])
    gup = k.sb("gup", [128, 512], scope=MX); k.dma(gup[:], gup_d[:, :], W=[gup])
    gt1t = k.sb("gtb1", [128, D], scope=MX)
    selb = k.sb("selb", [NB, NB, 128], scope=P0)
    k.dma(selb[:], sel_d[:, :, :], W=[selb])
    stage = [k.sb("stage", [128, 2048], scope=P0) for _ in range(2)]
    sidx = {"n": 0}

    def next_stage():
        s = stage[sidx["n"] % 2]
        sidx["n"] += 1
        return s
    awbs = [k.sb("awb", [128, 8, 512], scope=P0) for _ in range(2)]
    modrow = k.sb("modrow", [NB, 4, D], scope=P0)
    adabr = k.sb("adabr", [NB, 4, D], scope=P0)
    g2row = k.sb("g2row", [NB, D], scope=P0)
    k.dma(g2row[:], g2row_d[:, :], W=[g2row])

    k.op("act", lambda e: e.activation(out=csg[:], in_=cT[:], func=AF.Sigmoid), R=[cT], W=[csg])
    k.op("dve", lambda e: e.tensor_tensor(out=cact[:], in0=cT[:], in1=csg[:], op=ALU.mult), R=[cT, csg], W=[cact])
    ps_fm = psum()
    reserved.add(banks.index(ps_fm))
    for blk in range(12):
        awb = awbs[blk % 2]
        for h in range(4):
            st = next_stage()
            k.dma(st[:, 0:1024].rearrange("p (c n) -> p c n", c=2),
                  adaw_d[h * 256:(h + 1) * 256, blk * 512:(blk + 1) * 512].rearrange("(c p) n -> p c n", p=128),
                  W=[st])
            k.op("act", lambda e, st=st, h=h, awb=awb: e.activation(
                out=awb[:, 2 * h:2 * h + 2, :], in_=st[:, 0:1024].rearrange("p (c n) -> p c n", c=2), func=AF.Copy),
                R=[st], W=[awb])
        for jj in range(4):
            j = blk * 4 + jj
            mm(ps_fm, ps_fm[:, j * NB:(j + 1) * NB],
               [(awb[:, kc, jj * 128:(jj + 1) * 128], cact[:, kc * NB:(kc + 1) * NB]) for kc in range(8)],
               R=[awb, cact])
        if blk in (4, 5, 6, 7, 8, 9, 10, 11):
            ps_r = psum()
            pairs = [(cact[:, kc * NB:(kc + 1) * NB], awb[:, kc, :]) for kc in range(8)]
            mm(ps_r, ps_r[0:NB, :], pairs, R=[awb, cact])
            which = {4: 0, 5: 0, 10: 1, 11: 1, 6: 2, 7: 2, 8: 3, 9: 3}[blk]
            half = blk % 2
            k.op("act", lambda e, ps_r=ps_r, which=which, half=half: e.activation(
                out=modrow[:, which, half * 512:(half + 1) * 512], in_=ps_r[0:NB, :], func=AF.Copy),
                R=[ps_r], W=[modrow])
    for b in range(NB):
        k.op("dve", lambda e, b=b: e.tensor_tensor(out=modfm[b][:], in0=ps_fm[:, b:48 * NB:NB], in1=adab_fm, op=ALU.add),
             R=[ps_fm, smallw], W=[modfm[b]])
    reserved.clear()
    for b in range(NB):
        for which, c0 in ((0, 2 * D), (1, 5 * D), (2, 3 * D), (3, 4 * D)):
            k.dma(adabr[b:b + 1, which, :], adab_row_d[0:1, c0:c0 + D], W=[adabr])
    k.op("dve", lambda e: e.tensor_tensor(out=modrow[:], in0=modrow[:], in1=adabr[:], op=ALU.add),
         R=[adabr], W=[modrow])
    k.op("dve", lambda e: e.scalar_tensor_tensor(out=modrow[:, 3, :], in0=modrow[:, 3, :], scalar=1.0, in1=g2row[:],
                                                 op0=ALU.add, op1=ALU.mult), R=[g2row], W=[modrow])
    k.dma(modscr_d[:, :, :], modrow[:, 0:4, :], R=[modrow])
    for b in range(NB):
        for n, (sc0, sh0) in enumerate(((8, 0), (32, 24))):
            k.op("dve", lambda e, b=b, n=n, sc0=sc0: e.scalar_tensor_tensor(
                out=GS[b][:, n * 16:n * 16 + 8], in0=modfm[b][:, sc0:sc0 + 8], scalar=1.0,
                in1=g12[:, n * 8:(n + 1) * 8], op0=ALU.add, op1=ALU.mult), R=[modfm[b], smallw], W=[GS[b]])
            k.op("dve", lambda e, b=b, n=n, sh0=sh0: e.tensor_copy(
                out=GS[b][:, n * 16 + 8:n * 16 + 16], in_=modfm[b][:, sh0:sh0 + 8]), R=[modfm[b]], W=[GS[b]])

    nctr = {"n": 0}

    def rstd_of(src_ap, srcbuf):
        i = nctr["n"] % 2
        nctr["n"] += 1
        st = nstat[i]
        k.op("act", lambda e: e.activation(out=junk[:], in_=src_ap, func=AF.Square, accum_out=st[:, 0:1]),
             R=[srcbuf], W=[junk, st])
        k.op("dve", lambda e: e.tensor_scalar(out=st[:, 1:2], in0=st[:, 0:1], scalar1=1.0 / D, scalar2=NORM_EPS,
                                              op0=ALU.mult, op1=ALU.add), R=[st], W=[st])
        k.op("act", lambda e: e.activation(out=st[:, 2:3], in_=st[:, 1:2], func=AF.Sqrt), R=[st], W=[st])
        k.op("dve", lambda e: e.reciprocal(out=st[:, 3:4], in_=st[:, 2:3]), R=[st], W=[st])
        return st, i

    def norm_T(src_ap, srcbuf, gs, goff, evac, xi=0):
        st, i = rstd_of(src_ap, srcbuf)
        i = xi
        k.op("dve", lambda e: e.tensor_scalar(out=xn[i][:], in0=src_ap, scalar1=st[:, 3:4], scalar2=None, op0=ALU.mult),
             R=[srcbuf, st], W=[xn[i]])
        for hb in range(2):
            ps = psum()
            for c in range(4):
                kc = hb * 4 + c
                k.op("pe", lambda e, ps=ps, c=c, kc=kc: e.transpose(out=ps[:, c * 128:(c + 1) * 128],
                                                                     in_=xn[i][:, kc * 128:(kc + 1) * 128], identity=ident),
                     R=[xn[i], cm], W=[ps])
            for c in range(4):
                kc = hb * 4 + c
                evac(kc, ps[:, c * 128:(c + 1) * 128], gs[:, goff + kc:goff + kc + 1], gs[:, goff + 8 + kc:goff + 9 + kc], ps)

    for kc in range(8):
        for half in range(2):
            st = next_stage()
            k.dma(st[:, 0:1664], win_d[kc * 128:(kc + 1) * 128, half * 1664:(half + 1) * 1664], W=[st])
            k.op("act" if half else "dve", lambda e, st=st, kc=kc, half=half: (
                e.activation(out=winb[:, kc, half * 1664:(half + 1) * 1664], in_=st[:, 0:1664], func=AF.Copy) if half else
                e.tensor_copy(out=winb[:, kc, half * 1664:(half + 1) * 1664], in_=st[:, 0:1664])), R=[st], W=[winb])
    for kc in range(8):
        st = next_stage()
        k.dma(st[:, 0:D], wout_d[kc * 128:(kc + 1) * 128, :], W=[st])
        k.op("act", lambda e, st=st, kc=kc: e.activation(out=woutb[:, kc, :], in_=st[:, 0:D], func=AF.Copy), R=[st], W=[woutb])

    k.barrier()
    P0.close()
    def ksb(*a, **kw):
        return k.sb(*a, scope=MX, **kw)
    xgs = [ksb("xg", [128, TPG, D], nsub=TPG) for _ in range(2)]
    uT = ksb("uT", [128, 8, GM], BF)
    mixT = ksb("mixT", [128, 8, GM], BF, nsub=8)
    pbuf = [ksb("pbuf", [128, GM + 1]) for _ in range(14)]
    pl12, pl13 = ksb("pl12", [128, GM]), ksb("pl13", [128, GM])
    rkv = [[ksb("rkv", [128, GM]) for _ in range(3)] for _ in range(2)]
    ggs = [ksb("gg", [128, GM]) for _ in range(2)]
    bonuss = [ksb("bonus", [128, GM]) for _ in range(2)]
    PLs = [ksb("PL", [128, 2 * TPG]) for _ in range(2)]
    ucv = [ksb("ucv", [128, GM + 2]) for _ in range(4)]
    NTMP = 26
    tmps = [ksb("tmp", [128, GM]) for _ in range(NTMP)]
    Rtbs = [ksb("Rtb", [128, GM], BF) for _ in range(2)]
    TTts = [[ksb("TTt", [128, 512], BF) for _ in range(TPG)] for _ in range(2)]
    padss = [[[ksb("pads", [128, 3, 128], BF) for _ in range(TPG)] for _ in range(2)] for _ in range(2)]
    UTpad = [ksb("UTpad", [128, 128], BF) for _ in range(2)]
    Xs = [[ksb("Xs", [128, 2, 128], BF) for _ in range(2)] for _ in range(TPG)]
    XTs = [[ksb("XTs", [128, 2, 128], BF) for _ in range(2)] for _ in range(TPG)]
    Tms = [[ksb("Tms", [128, 2, 128], BF) for _ in range(2)] for _ in range(TPG)]
    MakT = [ksb("MakT", [128, 2, 128], BF) for _ in range(TPG)]
    MT = [ksb("MT", [128, 2, 128], BF) for _ in range(TPG)]
    Mbr = [ksb("Mbr", [128, 2, 128], BF) for _ in range(TPG)]
    Mkr = [ksb("Mkr", [128, 2, 128], BF) for _ in range(TPG)]
    Atbs = [ksb("Atb", [128, GM], BF) for _ in range(2)]
    Btbs = [ksb("Btb", [128, GM], BF) for _ in range(2)]
    Ktbs = [ksb("Ktb", [128, GM], BF) for _ in range(2)]
    Sdec = ksb("Sdec", [128, 128])
    cm2 = ksb("cm2", [128, 4, 256])
    k.dma(cm2[:], cm2_d[:, :, :], W=[cm2])
    mk2_su, mk2_sl, mk2_ui, mk2_id = (cm2[:, i_, :] for i_ in range(4))
    Ah = [ksb("Ah", [128, 128], BF) for _ in range(TPG)]
    WT2 = [ksb("WT2", [128, 128]) for _ in range(TPG)]
    STf = [ksb("STf", [128, 128]) for _ in range(4)]
    STb = [ksb("STb", [128, 128], BF) for _ in range(4)]

    for hh in range(2):
        for tb in range(TPG):
            for par_ in range(2):
                k.op("dve", lambda e: e.memset(padss[par_][hh][tb][:], 0.0), W=[padss[par_][hh][tb]])
        k.op("dve", lambda e, hh=hh: e.memset(UTpad[hh][:], 0.0), W=[UTpad[hh]])

    def proj(j):
        ps = psum()
        mm(ps, ps[:, 0:GM], [(winb[:, kc, j * 128:(j + 1) * 128], uT[:, kc, :]) for kc in range(8)], R=[winb, uT])
        return ps

    def bsum(src, srcbuf):
        ps = psum()
        mm(ps, ps[:, 0:GM], [(bones, src)], R=[cm, srcbuf])
        return ps

    def rsqrt_ps(ps, scale, eps, dst):
        k.op("dve", lambda e: e.tensor_scalar(out=dst[:], in0=ps[:, 0:GM], scalar1=scale, scalar2=eps, op0=ALU.mult, op1=ALU.add),
             R=[ps], W=[dst])
        k.op("act", lambda e: e.activation(out=dst[:], in_=dst[:], func=AF.Sqrt), R=[dst], W=[dst])
        k.op("dve", lambda e: e.reciprocal(out=dst[:], in_=dst[:]), R=[dst], W=[dst])

    def stage_U(b_, g_):
        xg_ = xgs[(b_ * NG + g_) % 2]
        for tt in range(TPG):
            k.dma(xg_[:, tt, :], x_d[b_, g_ * GM + tt * 128:g_ * GM + (tt + 1) * 128, :], W=[xg_.subs[tt]])

            def ev(kc, psap, sc, bi, ps, tt=tt):
                k.op("act", lambda e: e.activation(out=uT[:, kc, tt * 128:(tt + 1) * 128], in_=psap, func=AF.Identity,
                                                   scale=sc, bias=bi), R=[ps, GS[b_]], W=[uT])
            norm_T(xg_[:, tt, :], xg_.subs[tt], GS[b_], 0, ev)

    h1ctr = {"n": 0}
    for b in range(NB):
        k.dma(gt1t[:], modscr_d[b, 0, :].partition_broadcast(128), W=[gt1t])
        for q in range(14):
            k.op("dve", lambda e, q=q: e.memset(pbuf[q][:, 0:1], 0.0), W=[pbuf[q]])
        for ci in range(4):
            k.op("dve", lambda e, ci=ci: e.memset(ucv[ci][:, 0:2], 0.0), W=[ucv[ci]])
            k.op("dve", lambda e, ci=ci: e.memset(STf[ci][:], 0.0), W=[STf[ci]])
            k.op("dve", lambda e, ci=ci: e.memset(STb[ci][:], 0.0), W=[STb[ci]])
        for g in range(NG):
            t0 = g * GM
            xg = xgs[(b * NG + g) % 2]
            if b == 0 and g == 0:
                stage_U(0, 0)
            for ci in range(4):
                tB, tC, t1, yb, ysq, rr = tmps[6 * (ci % 2) + 3:6 * (ci % 2) + 9]
                ps = proj(ci)
                k.op("act", lambda e, ps=ps: e.activation(out=tB[:], in_=ps[:, 0:GM], func=AF.Copy), R=[ps], W=[tB])
                ps = proj(4 + ci)
                k.op("act", lambda e, ps=ps: e.activation(out=tC[:], in_=ps[:, 0:GM], func=AF.Copy), R=[ps], W=[tC])
                ps = proj(8 + ci)
                u = ucv[ci]
                k.op("dve", lambda e, ps=ps: e.tensor_tensor(out=u[:, 2:2 + GM], in0=tC[:], in1=ps[:, 0:GM], op=ALU.mult),
                     R=[tC, ps], W=[u])
                k.op("dve", lambda e: e.tensor_scalar(out=t1[:], in0=u[:, 0:GM], scalar1=cw[:, ci * 3:ci * 3 + 1], scalar2=None,
                                                      op0=ALU.mult), R=[u, smallw], W=[t1])
                for kk_ in (1, 2):
                    k.op("dve", lambda e, kk_=kk_: e.scalar_tensor_tensor(out=t1[:], in0=u[:, kk_:kk_ + GM],
                                                                           scalar=cw[:, ci * 3 + kk_:ci * 3 + kk_ + 1], in1=t1[:],
                                                                           op0=ALU.mult, op1=ALU.add), R=[u, smallw], W=[t1])
                k.op("dve", lambda e: e.tensor_tensor(out=yb[:], in0=tB[:], in1=t1[:], op=ALU.mult), R=[tB, t1], W=[yb])
                k.op("act", lambda e: e.activation(out=ysq[:], in_=yb[:], func=AF.Square), R=[yb], W=[ysq])
                psn = bsum(ysq[:], ysq)
                rsqrt_ps(psn, 1.0 / 64, NORM_EPS, rr)
                k.op("dve", lambda e: e.scalar_tensor_tensor(out=mixT[:, ci, :], in0=yb[:], scalar=cgn[:, ci:ci + 1], in1=rr[:],
                                                             op0=ALU.mult, op1=ALU.mult), R=[yb, rr, smallw], W=[mixT.subs[ci]])
                k.op("dve", lambda e: e.tensor_copy(out=u[:, 0:2], in_=u[:, GM:GM + 2]), R=[], W=[u])
            def lerp_tile(q, dst):
                ps = proj(12 + q)
                pb = pbuf[q]
                d_ = tmps[24 + (q % 2)]
                k.op("act", lambda e: e.activation(out=pb[:, 1:GM + 1], in_=ps[:, 0:GM], func=AF.Copy), R=[ps], W=[pb])
                k.op("dve", lambda e: e.tensor_tensor(out=d_[:], in0=pb[:, 0:GM], in1=pb[:, 1:GM + 1], op=ALU.subtract),
                     R=[pb], W=[d_])
                k.op("dve", lambda e: e.scalar_tensor_tensor(out=dst[:], in0=d_[:], scalar=mu[:, q:q + 1], in1=pb[:, 1:GM + 1],
                                                             op0=ALU.mult, op1=ALU.add), R=[d_, pb, smallw], W=[dst])
                k.op("dve", lambda e: e.tensor_copy(out=pb[:, 0:1], in_=pb[:, GM:GM + 1]), R=[], W=[pb])
            lerp_tile(12, pl12)
            lerp_tile(13, pl13)
            th, sgm = tmps[1], tmps[2]
            k.op("act", lambda e: e.activation(out=th[0:64, :], in_=pl12[0:64, :], func=AF.Tanh), R=[pl12], W=[th])
            k.op("act", lambda e: e.activation(out=sgm[:], in_=pl13[:], func=AF.Sigmoid), R=[pl13], W=[sgm])

            def tile_A1(i, par):
                (ld, av, kk, kksq, rinv, kkn, tq, kmod, prod, lcum, Pinc, Pexc, Pinv, At, Bt, Kt, Rt) = tmps[3:20]
                gg, bonus, PL = ggs[par], bonuss[par], PLs[par]
                Atb, Btb, Ktb, Rtb, TTt, pads = Atbs[par], Btbs[par], Ktbs[par], Rtbs[par], TTts[par], padss[par]
                rl, kl, vl = rkv[par]
                lerp_tile(i, rl)
                lerp_tile(4 + i, kl)
                lerp_tile(8 + i, vl)
                cs = slice(i * 128, (i + 1) * 128)
                ps = psum()
                mm(ps, ps[:, 0:GM], [(lora[0:64, cs], th[0:64, :])], R=[lora, th])
                k.op("act", lambda e, ps=ps: e.activation(out=ld[:], in_=ps[:, 0:GM], func=AF.Sigmoid, bias=vec(0, i), scale=1.0),
                     R=[ps, smallw], W=[ld])
                k.op("dve", lambda e: e.tensor_scalar(out=ld[:], in0=ld[:], scalar1=-0.6065306597126334, scalar2=None, op0=ALU.mult),
                     R=[], W=[ld])
                ps = psum()
                mm(ps, ps[:, 0:GM], [(lora[64:128, cs], pl12[64:128, :])], R=[lora, pl12])
                k.op("act", lambda e, ps=ps: e.activation(out=av[:], in_=ps[:, 0:GM], func=AF.Sigmoid, bias=vec(1, i), scale=1.0),
                     R=[ps, smallw], W=[av])
                ps = psum()
                mm(ps, ps[:, 0:GM], [(gup[:, cs], sgm[:])], R=[gup, sgm])
                k.op("act", lambda e, ps=ps: e.activation(out=gg[:], in_=ps[:, 0:GM], func=AF.Copy), R=[ps], W=[gg])
                k.op("dve", lambda e: e.tensor_scalar(out=kk[:], in0=kl[:], scalar1=vec(2, i), scalar2=None, op0=ALU.mult),
                     R=[kl, smallw], W=[kk])
                k.op("act", lambda e: e.activation(out=kksq[:], in_=kk[:], func=AF.Square), R=[kk], W=[kksq])
                psn = bsum(kksq[:], kksq)
                k.op("act", lambda e, psn=psn: e.activation(out=rinv[:], in_=psn[:, 0:GM], func=AF.Sqrt), R=[psn], W=[rinv])
                k.op("dve", lambda e: e.tensor_scalar(out=rinv[:], in0=rinv[:], scalar1=1e-12, scalar2=None, op0=ALU.max), R=[], W=[rinv])
                k.op("dve", lambda e: e.reciprocal(out=rinv[:], in_=rinv[:]), R=[], W=[rinv])
                k.op("dve", lambda e: e.tensor_tensor(out=kkn[:], in0=kk[:], in1=rinv[:], op=ALU.mult), R=[kk, rinv], W=[kkn])
                k.op("dve", lambda e: e.tensor_scalar(out=tq[:], in0=av[:], scalar1=-1.0, scalar2=vec(3, i), op0=ALU.add, op1=ALU.mult),
                     R=[av, smallw], W=[tq])
                k.op("dve", lambda e: e.scalar_tensor_tensor(out=kmod[:], in0=tq[:], scalar=1.0, in1=kl[:], op0=ALU.add, op1=ALU.mult),
                     R=[tq, kl], W=[kmod])
                k.op("dve", lambda e: e.scalar_tensor_tensor(out=prod[:], in0=rl[:], scalar=vec(4, i), in1=kmod[:], op0=ALU.mult,
                                                             op1=ALU.mult), R=[rl, kmod, smallw], W=[prod])
                psrk = bsum(prod[:], prod)
                k.op("dve", lambda e, psrk=psrk: e.tensor_tensor(out=bonus[:], in0=psrk[:, 0:GM], in1=vl[:], op=ALU.mult),
                     R=[psrk, vl], W=[bonus])
                k.op("dve", lambda e: e.tensor_tensor_scan(out=lcum[:], data0=rmask[:], data1=ld[:], initial=0.0, op0=ALU.mult,
                                                           op1=ALU.add), R=[rmask, ld], W=[lcum])
                k.op("act", lambda e: e.activation(out=Pinc[:], in_=lcum[:], func=AF.Exp), R=[lcum], W=[Pinc])
                k.op("dve", lambda e: e.tensor_tensor(out=Pexc[:], in0=lcum[:], in1=ld[:], op=ALU.subtract), R=[lcum, ld], W=[Pexc])
                k.op("act", lambda e: e.activation(out=Pexc[:], in_=Pexc[:], func=AF.Exp), R=[], W=[Pexc])
                k.op("act", lambda e: e.activation(out=Pinv[:], in_=lcum[:], func=AF.Exp, scale=-1.0), R=[lcum], W=[Pinv])
                k.op("dve", lambda e: e.scalar_tensor_tensor(out=At[:], in0=kkn[:], scalar=-1.0, in1=Pexc[:], op0=ALU.mult, op1=ALU.mult),
                     R=[kkn, Pexc], W=[At])
                k.op("dve", lambda e: e.tensor_tensor(out=Bt[:], in0=kkn[:], in1=av[:], op=ALU.mult), R=[kkn, av], W=[Bt])
                k.op("dve", lambda e: e.tensor_tensor(out=Bt[:], in0=Bt[:], in1=Pinv[:], op=ALU.mult), R=[Pinv], W=[Bt])
                k.op("dve", lambda e: e.tensor_tensor(out=Kt[:], in0=kmod[:], in1=Pinv[:], op=ALU.mult), R=[kmod, Pinv], W=[Kt])
                k.op("dve", lambda e: e.tensor_tensor(out=Rt[:], in0=rl[:], in1=Pinc[:], op=ALU.mult), R=[rl, Pinc], W=[Rt])
                k.op("act", lambda e: e.activation(out=Rtb[:], in_=Rt[:], func=AF.Copy), R=[Rt], W=[Rtb])
                for src_, dst_ in ((At, Atb), (Bt, Btb), (Kt, Ktb)):
                    k.op("pool", lambda e: e.tensor_copy(out=dst_[:], in_=src_[:]), R=[src_], W=[dst_])
                for tb in range(TPG):
                    cl = slice(tb * 128, (tb + 1) * 128)
                    ps = psum()
                    for n_, src in enumerate((At, Bt, Kt, vl)):
                        k.op("pe", lambda e: e.transpose(out=ps[:, n_ * 128:(n_ + 1) * 128], in_=src[:, cl], identity=ident),
                             R=[src, cm], W=[ps])
                    k.op("act", lambda e: e.activation(out=TTt[tb][:], in_=ps[:, :], func=AF.Copy), R=[ps], W=[TTt[tb]])
                    for hh in range(2):
                        k.op("dve" if hh else "act", lambda e: (e.tensor_copy(
                            out=pads[hh][tb][:, :, hh * 64:hh * 64 + 64],
                            in_=TTt[tb][:, 128:512].rearrange("p (a c) -> p a c", a=3)[:, :, hh * 64:hh * 64 + 64]) if hh else
                            e.activation(out=pads[hh][tb][:, :, hh * 64:hh * 64 + 64],
                                         in_=TTt[tb][:, 128:512].rearrange("p (a c) -> p a c", a=3)[:, :, hh * 64:hh * 64 + 64],
                                         func=AF.Copy)),
                            R=[TTt[tb]], W=[pads[hh][tb]])
                k.op("act", lambda e: e.activation(out=PL[:], in_=Pinc[:, 63:GM:64], func=AF.Copy), R=[Pinc], W=[PL])

            def tile_X(i, par):
                Osb, dd, dsq, rs = tmps[20:24]
                gg, bonus, PL = ggs[par], bonuss[par], PLs[par]
                Atb, Btb, Ktb, Rtb, TTt, pads = Atbs[par], Btbs[par], Ktbs[par], Rtbs[par], TTts[par], padss[par]
                for tb in range(TPG):
                    cl = slice(tb * 128, (tb + 1) * 128)
                    specs = [(Btb, Atb, mk2_su, Xs[tb][0]), (Atb, Btb, mk2_sl, XTs[tb][0]), (Atb, Ktb, mk2_sl, MakT[tb]),
                             (Btb, Rtb, mk2_ui, Mbr[tb]), (Ktb, Rtb, mk2_ui, Mkr[tb])]
                    gps = [psum() for _ in specs]
                    for hh in range(2):
                        pr = slice(hh * 64, hh * 64 + 64)
                        for (l, r, mask2, dst), ps in zip(specs, gps):
                            mm(ps, ps[:, hh * 128:(hh + 1) * 128], [(l[pr, cl], r[pr, cl])], R=[l, r])
                    for (l, r, mask2, dst), ps in zip(specs, gps):
                        k.op("dve", lambda e: e.tensor_tensor(out=dst[:].rearrange("p a b -> p (a b)"), in0=ps[:, 0:256], in1=mask2,
                                                              op=ALU.mult), R=[ps, cm2], W=[dst])
                    k.op("dve", lambda e: e.tensor_tensor(out=Tms[tb][0][:].rearrange("p a b -> p (a b)"),
                                                          in0=Xs[tb][0][:].rearrange("p a b -> p (a b)"), in1=mk2_id, op=ALU.add),
                         R=[Xs[tb][0], cm2], W=[Tms[tb][0]])
                cur = 0
                for lvl in range(1, 6):
                    nxt = 1 - cur
                    pss = {}
                    for tb in range(TPG):
                        if lvl < 5:
                            ps = psum()
                            for hh in range(2):
                                mm(ps, ps[:, hh * 128:(hh + 1) * 128], [(XTs[tb][cur][:, hh, :], Xs[tb][cur][:, hh, :])],
                                   R=[XTs[tb][cur], Xs[tb][cur]])
                            pss[(tb, 0)] = ps
                        ps = psum()
                        for hh in range(2):
                            mm(ps, ps[:, hh * 128:(hh + 1) * 128], [(Xs[tb][cur][:, hh, :], XTs[tb][cur][:, hh, :])],
                               R=[XTs[tb][cur], Xs[tb][cur]])
                        pss[(tb, 1)] = ps
                    for tb in range(TPG):
                        if lvl < 5:
                            ps = pss[(tb, 0)]
                            k.op("act", lambda e: e.activation(out=Xs[tb][nxt][:].rearrange("p a b -> p (a b)"), in_=ps[:, 0:256],
                                                               func=AF.Copy), R=[ps], W=[Xs[tb][nxt]])
                        ps = pss[(tb, 1)]
                        k.op("act", lambda e: e.activation(out=XTs[tb][nxt][:].rearrange("p a b -> p (a b)"), in_=ps[:, 0:256],
                                                           func=AF.Copy), R=[ps], W=[XTs[tb][nxt]])
                    for tb in range(TPG):
                        ps = psum()
                        for hh in range(2):
                            mm(ps, ps[:, hh * 128:(hh + 1) * 128], [(XTs[tb][nxt][:, hh, :], Tms[tb][cur][:, hh, :])],
                               R=[XTs[tb][nxt], Tms[tb][cur]])
                        pss[(tb, 2)] = ps
                    for tb in range(TPG):
                        ps = pss[(tb, 2)]
                        k.op("dve", lambda e: e.tensor_tensor(out=Tms[tb][nxt][:].rearrange("p a b -> p (a b)"), in0=ps[:, 0:256],
                                                              in1=Tms[tb][cur][:].rearrange("p a b -> p (a b)"), op=ALU.add),
                             R=[ps, Tms[tb][cur]], W=[Tms[tb][nxt]])
                    cur = nxt
                pA, pM = {}, {}
                for tb in range(TPG):
                    ps = psum()
                    for hh in range(2):
                        mm(ps, ps[:, hh * 128:(hh + 1) * 128], [(TTt[tb][:, 0:128], Tms[tb][cur][:, hh, :])], R=[TTt[tb], Tms[tb][cur]])
                    pA[tb] = ps
                    ps = psum()
                    for hh in range(2):
                        mm(ps, ps[:, hh * 128:(hh + 1) * 128], [(MakT[tb][:, hh, :], Tms[tb][cur][:, hh, :])], R=[MakT[tb], Tms[tb][cur]])
                    pM[tb] = ps
                for tb in range(TPG):
                    for hh in range(2):
                        pr = slice(hh * 64, hh * 64 + 64)
                        k.op("act", lambda e: e.activation(out=Ah[tb][pr, :], in_=pA[tb][pr, hh * 128:(hh + 1) * 128], func=AF.Copy),
                             R=[pA[tb]], W=[Ah[tb]])
                    k.op("act", lambda e: e.activation(out=MT[tb][:].rearrange("p a b -> p (a b)"), in_=pM[tb][:, 0:256], func=AF.Copy),
                         R=[pM[tb]], W=[MT[tb]])
                for tb in range(TPG):
                    ps = psum()
                    for hh in range(2):
                        mm(ps, ps[:, hh * 64:(hh + 1) * 64], [(MT[tb][:, hh, :], TTt[tb][:, 384 + hh * 64:384 + hh * 64 + 64])],
                           R=[MT[tb], TTt[tb]])
                    k.op("act", lambda e: e.activation(out=WT2[tb][:], in_=ps[:, 0:128], func=AF.Copy), R=[ps], W=[WT2[tb]])
                for tb in range(TPG):
                    for c in range(2):
                        ro = slice(64 * c, 64 * c + 64)
                        cc0 = tb * 128 + 64 * c
                        k.op("dve", lambda e: e.tensor_scalar(out=Sdec[:], in0=STf[i][:], scalar1=PL[:, 2 * tb + c:2 * tb + c + 1], scalar2=None,
                                                              op0=ALU.mult), R=[STf[i], PL], W=[Sdec])
                        ps = psum()
                        mm(ps, ps[:, 0:128], [(Ah[tb][:], STb[i][:])], R=[Ah[tb], STb[i]])
                        for hh in range(2):
                            hs = slice(hh * 64, hh * 64 + 64)
                            k.op("dve", lambda e: e.tensor_tensor(out=UTpad[hh][ro, hs], in0=ps[ro, hs], in1=WT2[tb][ro, hs], op=ALU.add),
                                 R=[ps, WT2[tb]], W=[UTpad[hh]])
                        pss_ = psum()
                        mm(pss_, pss_[:, 0:128],
                           [(pads[0][tb][ro, 0, :], UTpad[0][ro, :]), (pads[1][tb][ro, 0, :], UTpad[1][ro, :]),
                            (pads[0][tb][ro, 1, :], pads[0][tb][ro, 2, :]), (pads[1][tb][ro, 1, :], pads[1][tb][ro, 2, :])],
                           R=[pads[0][tb], pads[1][tb], UTpad[0], UTpad[1]])
                        k.op("dve", lambda e: e.scalar_tensor_tensor(out=STf[i][:], in0=pss_[:, 0:128], scalar=PL[:, 2 * tb + c:2 * tb + c + 1],
                                                                     in1=Sdec[:], op0=ALU.mult, op1=ALU.add),
                             R=[pss_, PL, Sdec], W=[STf[i]])
                        pso = psum()
                        mm(pso, pso[:, 0:64],
                           [(STb[i][:], Rtb[:, cc0:cc0 + 64]),
                            (UTpad[0][ro, :], Mbr[tb][ro, 0, ro]), (UTpad[1][ro, :], Mbr[tb][ro, 1, ro]),
                            (pads[0][tb][ro, 2, :], Mkr[tb][ro, 0, ro]), (pads[1][tb][ro, 2, :], Mkr[tb][ro, 1, ro])],
                           R=[STb[i], Rtb, UTpad[0], UTpad[1], Mbr[tb], Mkr[tb], pads[0][tb], pads[1][tb]])
                        k.op("act", lambda e: e.activation(out=STb[i][:], in_=STf[i][:], func=AF.Copy), R=[STf[i]], W=[STb[i]])
                        k.op("act", lambda e: e.activation(out=Osb[:, cc0:cc0 + 64], in_=pso[:, 0:64], func=AF.Copy), R=[pso], W=[Osb])
                psm = bsum(Osb[:], Osb)
                k.op("dve", lambda e, psm=psm: e.scalar_tensor_tensor(out=dd[:], in0=psm[:, 0:GM], scalar=-1.0 / 64, in1=Osb[:],
                                                                        op0=ALU.mult, op1=ALU.add), R=[psm, Osb], W=[dd])
                k.op("act", lambda e: e.activation(out=dsq[:], in_=dd[:], func=AF.Square), R=[dd], W=[dsq])
                psv = bsum(dsq[:], dsq)
                rsqrt_ps(psv, 1.0 / 64, GN_EPS, rs)
                k.op("dve", lambda e: e.tensor_tensor(out=dd[:], in0=dd[:], in1=rs[:], op=ALU.mult), R=[rs], W=[dd])
                k.op("dve", lambda e: e.tensor_scalar(out=dd[:], in0=dd[:], scalar1=vec(5, i), scalar2=vec(6, i), op0=ALU.mult, op1=ALU.add),
                     R=[smallw], W=[dd])
                k.op("dve", lambda e: e.tensor_tensor(out=dd[:], in0=dd[:], in1=bonus[:], op=ALU.add), R=[bonus], W=[dd])
                k.op("dve", lambda e: e.tensor_tensor(out=mixT[:, 4 + i, :], in0=dd[:], in1=gg[:], op=ALU.mult), R=[dd, gg],
                     W=[mixT.subs[4 + i]])
            bstate["pool"] = "Y"
            k.begin_rec(); tile_A1(0, 0); Y_ = k.end_rec()
            bstate["pool"] = None
            k.play_merged(Y_)
            for i in range(4):
                bstate["pool"] = "X"
                k.begin_rec(); tile_X(i, i % 2); X_ = k.end_rec()
                bstate["pool"] = "Y"
                k.begin_rec()
                if i < 3:
                    tile_A1(i + 1, (i + 1) % 2)
                else:
                    nxt_ = b * NG + g + 1
                    if nxt_ < NB * NG:
                        stage_U(nxt_ // NG, nxt_ % NG)
                Y_ = k.end_rec()
                bstate["pool"] = None
                if PMODE == 0:
                    k.play_merged(X_)
                    k.play_merged(Y_)
                else:
                    k.play_merged(X_, Y_)
            for tt in range(TPG):
                for half in range(2):
                    hsl = slice(half * 512, (half + 1) * 512)
                    ps = psum()
                    mm(ps, ps[:, :], [(mixT[:, c_, tt * 128:(tt + 1) * 128], woutb[:, c_, hsl]) for c_ in range(8)],
                       R=[woutb] + mixT.subs)
                    k.op("dve", lambda e, ps=ps, hsl=hsl: e.tensor_tensor(out=ps[:, :], in0=ps[:, :], in1=gt1t[:, hsl],
                                                                          op=ALU.mult), R=[gt1t], W=[ps])
                    k.op("dve", lambda e, ps=ps, hsl=hsl, tt=tt: e.tensor_tensor(out=xg[:, tt, hsl], in0=ps[:, :], in1=xg[:, tt, hsl],
                                                                                 op=ALU.add), R=[ps], W=[xg.subs[tt]])
                k.dma(h1_d[b, t0 + tt * 128:t0 + (tt + 1) * 128, :], xg[:, tt, :], R=[xg.subs[tt]], q="pool")

    if dbg == "h1":
        k.finish()
        return nc

    k.barrier()
    MX.close()
    PSX.close()
    del banks[6:]
    bbanks.extend(TT(nc.alloc_psum_tensor("psbb%d" % i, [128, 1024], BF)) for i in range(2))
    ME_P = ExitStack()
    I32 = mybir.dt.int32
    ltri_f, ones_f = cm[:, 5, :], cm[:, 6, :]

    def psb(*a, **kw):
        return k.sb(*a, scope=ME_P, **kw)
    cmb2 = psb("cmb2", [128, 2, 128], BF)
    k.op("dve", lambda e: e.tensor_copy(out=cmb2[:], in_=cm[:, 5:7, :]), R=[cm], W=[cmb2])
    lgs = psb("lgs", [128, NTC, 32])
    ranks = psb("ranks", [128, NTC, 32])
    m8s = psb("m8s", [128, NTC, 8])
    g4 = psb("g4", [128, NTC, 4])
    dest_f = psb("dest_f", [128, NTC * 4])
    dest_i = psb("dest_i", [128, NTC * 4], I32)
    cnt = psb("cnt", [128, 32])
    lay = psb("lay", [128, 5, 32])
    bs = psb("bs", [128, NBLK]); k.dma(bs[:], bs_d[:, :], W=[bs])
    iop = psb("iop", [128, 8]); k.dma(iop[:], iop_d[:, :], W=[iop])
    be = psb("be", [128, 2, NBLK])
    idxW_f = psb("idxW_f", [128, NBLK, 8])
    idxW_i = psb("idxW_i", [128, NBLK * 8], I32)
    idxW2_f = psb("idxW2_f", [128, NBLK, 8])
    idxW2_i = psb("idxW2_i", [128, NBLK * 8], I32)
    skp = psb("skp", [128, 2, NBLK])
    idxb_f = psb("idxb_f", [128, 2, NBLK])
    idxb_i = psb("idxb_i", [128, 2 * NBLK], I32)
    k.op("dve", lambda e: e.memset(cnt[:], 0.0), W=[cnt])
    k.op("dve", lambda e: e.memset(dest_f[:], 0.0), W=[dest_f])

    ME_R = ExitStack()

    def rsb(*a, **kw):
        return k.sb(*a, scope=ME_R, **kw)
    u2tm = rsb("u2tm", [128, NTC, D], BF, nsub=NTC)
    u2fs = [rsb("u2f", [128, 8, 128]) for _ in range(2)]
    xn.append(rsb("xn2", [128, D]))
    h2 = [rsb("h2", [128, D]) for _ in range(2)]
    G2bc = [rsb("G2bc", [128, D]) for _ in range(NB)]
    SH2bc = [rsb("SH2bc", [128, D]) for _ in range(NB)]
    maskbs = [rsb("maskb", [128, 32], BF) for _ in range(2)]
    rts = [rsb("rt", [128, 64]) for _ in range(2)]
    for b in range(NB):
        k.dma(SH2bc[b][:], modscr_d[b, 2, :].partition_broadcast(128), W=[SH2bc[b]])
        k.dma(G2bc[b][:], modscr_d[b, 3, :].partition_broadcast(128), W=[G2bc[b]])
    for tile in range(NTC):
        b, tt = tile // NT, tile % NT
        ht = h2[tile % 2]
        u2f, maskb, rt_ = u2fs[tile % 2], maskbs[tile % 2], rts[tile % 2]
        k.dma(ht[:], h1_d[b, tt * 128:(tt + 1) * 128, :], W=[ht])

        def ev2(kc, psap, sc, bi, ps):
            k.op("act", lambda e: e.activation(out=u2f[:, kc, :], in_=psap, func=AF.Identity, scale=sc, bias=bi),
                 R=[ps, GS[b]], W=[u2f])
        norm_T(ht[:], ht, GS[b], 16, ev2, xi=tile % 2)
        k.op("dve", lambda e: e.tensor_tensor(out=ht[:], in0=xn[tile % 2][:], in1=G2bc[b][:], op=ALU.mult),
             R=[xn[tile % 2], G2bc[b]], W=[ht])
        k.op("dve", lambda e: e.tensor_tensor(out=u2tm[:, tile, :], in0=ht[:], in1=SH2bc[b][:], op=ALU.add),
             R=[ht, SH2bc[b]], W=[u2tm.subs[tile]])
        ps = psum()
        mm(ps, ps[:, 0:32], [(u2f[:, kc, :], rw[:, kc, :]) for kc in range(8)], R=[u2f, rw])
        lg = lgs[:, tile, :]
        m8 = m8s[:, tile, :]
        k.op("dve", lambda e: e.tensor_tensor(out=lg, in0=ps[:, 0:32], in1=rb[:], op=ALU.add), R=[ps, rb], W=[lgs])
        k.op("dve", lambda e: e.max(out=m8, in_=lg), R=[lgs], W=[m8s])
        k.op("dve", lambda e: e.tensor_scalar(out=maskb[:], in0=lg, scalar1=m8s[:, tile, 3:4], scalar2=None, op0=ALU.is_ge),
             R=[lgs, m8s], W=[maskb])
        psr = psum()
        mm(psr, psr[:, 0:32], [(cmb2[:, 0, :], maskb[:])], R=[cmb2, maskb])
        k.op("dve", lambda e: e.tensor_tensor(out=ranks[:, tile, :], in0=psr[:, 0:32], in1=cnt[:], op=ALU.add),
             R=[psr, cnt], W=[ranks])
        psc = psum()
        mm(psc, psc[:, 0:32], [(cmb2[:, 1, :], maskb[:])], R=[cmb2, maskb])
        k.op("dve", lambda e: e.tensor_tensor(out=cnt[:], in0=psc[:, 0:32], in1=cnt[:], op=ALU.add), R=[psc], W=[cnt])
        negm, ssum, ex4 = rt_[:, 0:1], rt_[:, 1:2], rt_[:, 4:8]
        k.op("dve", lambda e: e.tensor_scalar(out=negm, in0=m8s[:, tile, 0:1], scalar1=-1.0, scalar2=None, op0=ALU.mult),
             R=[m8s], W=[rt_])
        k.op("act", lambda e: e.activation(out=ex4, in_=m8s[:, tile, 0:4], func=AF.Exp, bias=negm, scale=1.0), R=[m8s], W=[rt_])
        k.op("dve", lambda e: e.tensor_reduce(out=ssum, in_=ex4, axis=mybir.AxisListType.X, op=ALU.add), R=[], W=[rt_])
        k.op("dve", lambda e: e.reciprocal(out=ssum, in_=ssum), R=[], W=[rt_])
        k.op("dve", lambda e: e.tensor_scalar(out=g4[:, tile, :], in0=ex4, scalar1=ssum, scalar2=None, op0=ALU.mult),
             R=[rt_], W=[g4])
    nbt, padded, pend, pstart, ones32 = (lay[:, i, :] for i in range(5))
    k.op("dve", lambda e: e.memset(ones32, 1.0), W=[lay])
    k.op("dve", lambda e: e.tensor_scalar(out=nbt, in0=cnt[:], scalar1=0.0, scalar2=None, op0=ALU.is_gt), R=[cnt], W=[lay])
    for j in range(1, (NTC * 128) // BLK):
        k.op("dve", lambda e: e.scalar_tensor_tensor(out=nbt, in0=cnt[:], scalar=float(BLK * j), in1=nbt, op0=ALU.is_gt,
                                                     op1=ALU.add), R=[cnt], W=[lay])
    k.op("dve", lambda e: e.tensor_scalar(out=padded, in0=nbt, scalar1=float(BLK), scalar2=None, op0=ALU.mult), R=[], W=[lay])
    k.op("dve", lambda e: e.tensor_tensor_scan(out=pend, data0=ones32, data1=padded, initial=0.0, op0=ALU.mult, op1=ALU.add),
         R=[], W=[lay])
    k.op("dve", lambda e: e.tensor_tensor(out=pstart, in0=pend, in1=padded, op=ALU.subtract), R=[], W=[lay])
    k.op("dve", lambda e: e.memset(be[:, 0, :], 0.0), W=[be])
    for e_ in range(32):
        k.op("dve", lambda e: e.scalar_tensor_tensor(out=be[:, 0, :], in0=bs[:], scalar=lay[:, 2, e_:e_ + 1], in1=be[:, 0, :],
                                                     op0=ALU.is_ge, op1=ALU.add), R=[bs, lay], W=[be])
    k.op("dve", lambda e: e.tensor_scalar(out=be[:, 0, :], in0=be[:, 0, :], scalar1=float(E - 1), scalar2=None, op0=ALU.min),
         R=[], W=[be])
    k.op("dve", lambda e: e.tensor_scalar(out=be[:, 1, :], in0=be[:, 0, :], scalar1=float(D), scalar2=None, op0=ALU.mult),
         R=[], W=[be])
    for kc in range(8):
        k.op("dve", lambda e: e.tensor_scalar(out=idxW_f[:, :, kc], in0=be[:, 1, :], scalar1=iop[:, kc:kc + 1], scalar2=None,
                                              op0=ALU.add), R=[be, iop], W=[idxW_f])
    BIG = 1.0e6
    k.op("dve", lambda e: e.memset(skp[:], 0.0), W=[skp])
    k.op("dve", lambda e: e.tensor_tensor(out=skp[:, 0, 2:NBLK], in0=be[:, 0, 2:NBLK], in1=be[:, 0, 0:NBLK - 2], op=ALU.is_equal),
         R=[be], W=[skp])
    k.op("dve", lambda e: e.tensor_tensor(out=skp[:, 1, 1:NBLK], in0=be[:, 0, 1:NBLK], in1=be[:, 0, 0:NBLK - 1], op=ALU.is_equal),
         R=[be], W=[skp])
    k.op("dve", lambda e: e.tensor_scalar(out=skp[:], in0=skp[:], scalar1=BIG, scalar2=None, op0=ALU.mult), R=[], W=[skp])
    for kc in range(8):
        k.op("dve", lambda e: e.tensor_tensor(out=idxW2_f[:, :, kc], in0=idxW_f[:, :, kc], in1=skp[:, 1, :], op=ALU.add),
             R=[idxW_f, skp], W=[idxW2_f])
    k.op("dve", lambda e: e.tensor_copy(out=idxW2_i[:], in_=idxW2_f[:].rearrange("p a b -> p (a b)")), R=[idxW2_f], W=[idxW2_i])
    for kc in range(8):
        k.op("dve", lambda e: e.tensor_tensor(out=idxW_f[:, :, kc], in0=idxW_f[:, :, kc], in1=skp[:, 0, :], op=ALU.add),
             R=[skp], W=[idxW_f])
    k.op("dve", lambda e: e.tensor_copy(out=idxW_i[:], in_=idxW_f[:].rearrange("p a b -> p (a b)")), R=[idxW_f], W=[idxW_i])
    k.op("dve", lambda e: e.tensor_scalar(out=idxb_f[:, 0, :], in0=be[:, 0, :], scalar1=128.0, scalar2=iop[:, 0:1],
                                          op0=ALU.mult, op1=ALU.add), R=[be, iop], W=[idxb_f])
    k.op("dve", lambda e: e.tensor_copy(out=idxb_f[:, 1, :], in_=be[:, 0, :]), R=[be], W=[idxb_f])
    k.op("dve", lambda e: e.tensor_copy(out=idxb_i[:], in_=idxb_f[:].rearrange("p a b -> p (a b)")), R=[idxb_f], W=[idxb_i])
    rt_ = rts[0]
    for tile in range(NTC):
        k.op("dve", lambda e: e.tensor_tensor(out=ranks[:, tile, :], in0=ranks[:, tile, :], in1=pstart, op=ALU.add),
             R=[lay], W=[ranks])
        for kk_ in range(4):
            k.op("dve", lambda e: e.scalar_tensor_tensor(out=rt_[:, 32:64], in0=lgs[:, tile, :], scalar=m8s[:, tile, kk_:kk_ + 1],
                                                         in1=ranks[:, tile, :], op0=ALU.is_equal, op1=ALU.mult,
                                                         accum_out=dest_f[:, tile * 4 + kk_:tile * 4 + kk_ + 1]),
                 R=[lgs, m8s, ranks], W=[rt_, dest_f])
    k.op("dve", lambda e: e.tensor_copy(out=dest_i[:], in_=dest_f[:]), R=[dest_f], W=[dest_i])
    if dbg == "moe":
        dbg_d = nc.dram_tensor("dbg", [128, NTC * 4 + NTC * 4 + 32 + NBLK], F32, kind="ExternalOutput").ap()
        k.dma(dbg_d[:, 0:NTC * 4], dest_f[:], R=[dest_f])
        k.dma(dbg_d[:, NTC * 4:NTC * 8], g4[:].rearrange("p a b -> p (a b)"), R=[g4])
        k.dma(dbg_d[:, NTC * 8:NTC * 8 + 32], cnt[:], R=[cnt])
        k.dma(dbg_d[:, NTC * 8 + 32:NTC * 8 + 32 + NBLK], be[:, 0, :], R=[be])
    for tile in range(NTC):
        for kk_ in range(4):
            c_ = tile * 4 + kk_
            k.dma(xs_d[:, :], u2tm[:, tile, :], R=[u2tm.subs[tile], dest_i], q="pool", indirect=("scatter", dest_i[:, c_:c_ + 1]))
    k.barrier()
    xn.pop()
    ME_R.close()

    ME_X = ExitStack()

    def xsb(*a, **kw):
        return k.sb(*a, scope=ME_X, **kw)
    W1b = [xsb("W1b", [128, 8, 2 * D], BF, nsub=8) for _ in range(2)]
    W2b = xsb("W2b", [128, 8, D], BF, nsub=8)
    xrow = [xsb("xrow", [128, 4, D], BF) for _ in range(2)]
    xsT = [xsb("xsT", [128, 8, BLK], BF, nsub=8) for _ in range(2)]
    actT = [xsb("actT", [128, 8, BLK], BF, nsub=8) for _ in range(2)]
    et = [[xsb("et", [128, BLK]) for _ in range(4)] for _ in range(2)]
    yrow = [xsb("yrow", [128, D]) for _ in range(2)]
    b2bc = [xsb("b2bc", [128, D]) for _ in range(2)]
    b1blk = [xsb("b1blk", [128, 16]) for _ in range(2)]
    ectr = {"n": 0, "y": 0}
    def load_weights(blk):
        w1 = W1b[blk % 2]
        b1t, b2t = b1blk[blk % 2], b2bc[blk % 2]
        k.dma(b1t[:], b1_d[:, :], R=[idxb_i], W=[b1t], q="pool", indirect=("gather", idxb_i[:, blk:blk + 1]))
        k.dma(b2t[:], b2_d[:, :], R=[idxb_i], W=[b2t], q="pool", indirect=("gather", idxb_i[:, NBLK + blk:NBLK + blk + 1]))
        for kc in range(8):
            c_ = blk * 8 + kc
            k.dma(w1[:, kc, :], w1_d[:, :], R=[idxW_i], W=[w1.subs[kc]], q="pool",
                  indirect=("gather", idxW_i[:, c_:c_ + 1], E * D - 1))

    def load_w2(blk):
        for fc in range(8):
            c_ = blk * 8 + fc
            k.dma(W2b[:, fc, :], w2_d[:, :], R=[idxW2_i], W=[W2b.subs[fc]], q="pool",
                  indirect=("gather", idxW2_i[:, c_:c_ + 1], E * D - 1))

    def load_x(blk):
        k.dma(xrow[blk % 2][:], xs_d[blk * BLK:(blk + 1) * BLK, :].rearrange("(s p) d -> p s d", p=128), W=[xrow[blk % 2]])

    def transposes(blk):
        xT = xsT[blk % 2]
        xr = xrow[blk % 2]
        for kc in range(8):
            if kc % 2 == 0:
                pb = psum_bf()
            po = (kc % 2) * 512
            for s_ in range(4):
                k.op("pe", lambda e: e.transpose(out=pb[:, po + s_ * 128:po + (s_ + 1) * 128], in_=xr[:, s_, kc * 128:(kc + 1) * 128],
                                                 identity=cmb[:]), R=[xr, cmb], W=[pb])
            k.op("act", lambda e: e.activation(out=xT[:, kc, :], in_=pb[:, po:po + 512], func=AF.Copy), R=[pb], W=[xT.subs[kc]])

    load_weights(0)
    load_x(0)
    transposes(0)
    for blk in range(NBLK):
        w1 = W1b[blk % 2]
        b1t, b2t = b1blk[blk % 2], b2bc[blk % 2]
        load_w2(blk)
        if blk + 1 < NBLK:
            load_weights(blk + 1)
            load_x(blk + 1)
        xT = xsT[blk % 2]
        aT = actT[blk % 2]
        for fc in range(8):
            xg_, sg_, xl_, tt_ = et[ectr["n"] % 2]
            ectr["n"] += 1
            psg = psum()
            mm(psg, psg[:, :], [(w1[:, kc, fc * 128:(fc + 1) * 128], xT[:, kc, :]) for kc in range(8)], R=w1.subs + xT.subs)
            psl = psum()
            mm(psl, psl[:, :], [(w1[:, kc, D + fc * 128:D + (fc + 1) * 128], xT[:, kc, :]) for kc in range(8)], R=w1.subs + xT.subs)
            k.op("dve", lambda e: e.tensor_scalar(out=xg_[:], in0=psg[:, :], scalar1=b1t[:, fc:fc + 1], scalar2=7.0,
                                                  op0=ALU.add, op1=ALU.min), R=[psg, b1t], W=[xg_])
            k.op("act", lambda e: e.activation(out=sg_[:], in_=xg_[:], func=AF.Sigmoid, scale=1.702), R=[xg_], W=[sg_])
            k.op("dve", lambda e: e.tensor_scalar(out=xl_[:], in0=psl[:, :], scalar1=b1t[:, 8 + fc:9 + fc], scalar2=7.0,
                                                  op0=ALU.add, op1=ALU.min), R=[psl, b1t], W=[xl_])
            k.op("dve", lambda e: e.tensor_scalar(out=xl_[:], in0=xl_[:], scalar1=-7.0, scalar2=1.0, op0=ALU.max, op1=ALU.add),
                 R=[], W=[xl_])
            k.op("dve", lambda e: e.tensor_tensor(out=tt_[:], in0=xg_[:], in1=sg_[:], op=ALU.mult), R=[xg_, sg_], W=[tt_])
            k.op("dve", lambda e: e.tensor_tensor(out=aT[:, fc, :], in0=tt_[:], in1=xl_[:], op=ALU.mult), R=[tt_, xl_],
                 W=[aT.subs[fc]])
        if blk + 1 < NBLK:
            transposes(blk + 1)
        for s_ in range(4):
            yr = yrow[ectr["y"] % 2]
            ectr["y"] += 1
            for half in range(2):
                hsl = slice(half * 512, (half + 1) * 512)
                ps = psum()
                mm(ps, ps[:, :], [(aT[:, fc, s_ * 128:(s_ + 1) * 128], W2b[:, fc, hsl]) for fc in range(8)], R=aT.subs + W2b.subs)
                k.op("dve", lambda e: e.tensor_tensor(out=yr[:, hsl], in0=ps[:, :], in1=b2t[:, hsl], op=ALU.add), R=[ps, b2t], W=[yr])
            k.dma(yb_d[blk * BLK + s_ * 128:blk * BLK + (s_ + 1) * 128, :], yr[:], R=[yr])
    k.barrier()
    ME_X.close()

    ME_C = ExitStack()

    def csb(*a, **kw):
        return k.sb(*a, scope=ME_C, **kw)
    ygat = [[csb("ygat", [128, D]) for _ in range(4)] for _ in range(2)]
    h3 = [csb("h3", [128, D]) for _ in range(2)]
    fg = csb("fg", [128, D]); k.dma(fg[:], fg_d[:, :], W=[fg])
    for b in range(NB):
        gtb[b][1] = csb("gtb2", [128, D])
        k.dma(gtb[b][1][:], modscr_d[b, 1, :].partition_broadcast(128), W=[gtb[b][1]])
    for tile in range(NTC):
        b, tt = tile // NT, tile % NT
        ht = h3[tile % 2]
        yg = ygat[tile % 2]
        for kk_ in range(4):
            c_ = tile * 4 + kk_
            k.dma(yg[kk_][:], yb_d[:, :], R=[dest_i], W=[yg[kk_]], q="pool", indirect=("gather", dest_i[:, c_:c_ + 1]))
        k.dma(ht[:], h1_d[b, tt * 128:(tt + 1) * 128, :], W=[ht])
        k.op("dve", lambda e: e.tensor_scalar(out=yg[0][:], in0=yg[0][:], scalar1=g4[:, tile, 0:1], scalar2=None, op0=ALU.mult),
             R=[g4], W=[yg[0]])
        for kk_ in range(1, 4):
            k.op("dve", lambda e: e.scalar_tensor_tensor(out=yg[0][:], in0=yg[kk_][:], scalar=g4[:, tile, kk_:kk_ + 1], in1=yg[0][:],
                                                         op0=ALU.mult, op1=ALU.add), R=[yg[kk_], g4], W=[yg[0]])
        if dbg == "moe":
            k.dma(out_d[b, tt * 128:(tt + 1) * 128, :], yg[0][:], R=[yg[0]])
            continue
        k.op("dve", lambda e: e.tensor_tensor(out=yg[0][:], in0=yg[0][:], in1=gtb[b][1][:], op=ALU.mult), R=[gtb[b][1]], W=[yg[0]])
        k.op("dve", lambda e: e.tensor_tensor(out=ht[:], in0=ht[:], in1=yg[0][:], op=ALU.add), R=[yg[0]], W=[ht])
        st, i = rstd_of(ht[:], ht)
        k.op("dve", lambda e: e.scalar_tensor_tensor(out=ht[:], in0=ht[:], scalar=st[:, 3:4], in1=fg[:], op0=ALU.mult, op1=ALU.mult),
             R=[st, fg], W=[ht])
        k.dma(out_d[b, tt * 128:(tt + 1) * 128, :], ht[:], R=[ht])
    k.finish()
    return nc


def make_consts(NB, GM):
    idx = np.arange(128)
    same = (idx[:, None] // 64) == (idx[None, :] // 64)
    ident = np.eye(128, dtype=np.float32)
    m_su = (same & (idx[:, None] < idx[None, :])).astype(np.float32)
    m_sl = (same & (idx[:, None] > idx[None, :])).astype(np.float32)
    m_ui = (same & (idx[:, None] <= idx[None, :])).astype(np.float32)
    bones = same.astype(np.float32)
    ltri = (idx[:, None] < idx[None, :]).astype(np.float32)
    cm = np.ascontiguousarray(np.stack([ident, m_su, m_sl, m_ui, bones, ltri, np.ones((128, 128), np.float32)], axis=1))
    cm2 = np.ascontiguousarray(np.stack([np.concatenate([m, m], axis=1) for m in (m_su, m_sl, m_ui, ident)], axis=1))
    rm = np.ones((128, GM), np.float32)
    rm[:, ::64] = 0.0
    selb = np.zeros((NB, NB, 128), np.float32)
    for b in range(NB):
        selb[b, b, :] = 1.0
    return cm, rm, selb, np.ones((1, NB), np.float32), cm2


def fm(v, n):
    return np.ascontiguousarray(np.asarray(v, np.float32).reshape(n, 128).T)


def prep_shared(inp, NB, GM, E, T=2048):
    f = lambda a: np.asarray(a, np.float32)
    cm, rm, selb, ones, cm2 = make_consts(NB, GM)
    w1 = f(inp["exp_w1"])[0][:E]
    w1 = np.ascontiguousarray(np.concatenate([w1[:, :, 0::2], w1[:, :, 1::2]], axis=2))
    b1 = f(inp["exp_b1"])[0][:E]
    b1d = np.concatenate([b1[:, 0::2], b1[:, 1::2]], axis=1)
    b1_rows = np.ascontiguousarray(b1d.reshape(E, 16, 128).transpose(0, 2, 1).reshape(E * 128, 16))
    NBLK = (NB * T * 4) // 512 + 32
    blkstart = np.ascontiguousarray(np.broadcast_to((np.arange(NBLK, dtype=np.float32) * 512.0)[None, :], (128, NBLK)))
    iotaP = np.ascontiguousarray((np.arange(8, dtype=np.float32)[None, :] * 128.0 + np.arange(128, dtype=np.float32)[:, None]))
    vecs = np.stack([fm(f(inp[n])[0], 4) for n in
                     ("rwkv_w0", "rwkv_a0", "rwkv_k_k", "rwkv_k_a", "rwkv_r_k", "rwkv_ln_w", "rwkv_ln_b")], axis=1)
    sh = {
        "ada_w": np.ascontiguousarray(f(inp["ada_w"])[0]),
        "ada_b_fm": fm(f(inp["ada_b"])[0], 48),
        "ada_b_row": np.ascontiguousarray(f(inp["ada_b"])[0][None, :]),
        "g12_fm": np.ascontiguousarray(np.stack([fm(f(inp["norm1_g"])[0], 8), fm(f(inp["norm2_g"])[0], 8)], axis=1)),
        "w_in": np.ascontiguousarray(f(inp["w_in"])[0]),
        "conv_w_fm": np.ascontiguousarray(f(inp["conv_w"])[0].reshape(3, 4, 128).transpose(2, 1, 0)),
        "conv_gn_fm": fm(f(inp["conv_gn"])[0], 4),
        "mu_fm": fm(f(inp["rwkv_mu"])[0], 14),
        "vecs_fm": np.ascontiguousarray(vecs),
        "lora_up": np.ascontiguousarray(np.concatenate([f(inp["rwkv_w_up"])[0], f(inp["rwkv_a_up"])[0]], axis=0)),
        "g_up": np.ascontiguousarray(f(inp["rwkv_g_up"])[0]),
        "w_out": np.ascontiguousarray(f(inp["w_out"])[0]),
        "router_w": np.ascontiguousarray(f(inp["router_w"])[0][:, :32]),
        "router_b_bc": np.ascontiguousarray(np.broadcast_to(f(inp["router_b"])[0][None, :32], (128, 32))),
        "w1": w1.reshape(E * D, 2 * D),
        "b1_rows": b1_rows,
        "blkstart": blkstart, "iotaP": iotaP,
        "g2_row": np.ascontiguousarray(np.broadcast_to(f(inp["norm2_g"])[0][None, :], (NB, D))),
        "w2": np.ascontiguousarray(f(inp["exp_w2"])[0][:E]).reshape(E * D, D),
        "b2": np.ascontiguousarray(f(inp["exp_b2"])[0][:E]),
        "final_g_bc": np.ascontiguousarray(np.broadcast_to(f(inp["final_g"])[None, :], (128, D))),
        "cmats": cm, "cmats2": cm2, "resetmask": rm, "selb": selb, "ones_row": ones,
    }
    return sh


def core_inputs(sh, x, c, b0, NB):
    m = dict(sh)
    m["x"] = np.ascontiguousarray(x[b0:b0 + NB])
    cc = np.asarray(c[b0:b0 + NB], np.float32)
    m["cT"] = np.ascontiguousarray(cc.reshape(NB, 8, 128).transpose(2, 1, 0))
    return m


def kernel(**inputs):
    x = np.asarray(inputs["x"], np.float32)
    c = np.asarray(inputs["c"], np.float32)
    B, T, _ = x.shape
    n = 8
    NB = B // n
    E = 32
    GM = 256
    sh = prep_shared(inputs, NB, GM, E, T)
    nc = build(T, NB, E, GM)
    in_maps = [core_inputs(sh, x, c, i * NB, NB) for i in range(n)]
    res = run_bass_kernel_spmd(nc, in_maps, core_ids=list(range(n)))
    return np.concatenate([r["out"] for r in res.results], axis=0).astype(np.float32)
```

```python
import numpy as np
from contextlib import ExitStack
import concourse.bass as bass
import concourse.mybir as mybir
from concourse.bass_utils import run_bass_kernel_spmd

F32 = mybir.dt.float32
BF = mybir.dt.bfloat16
AF = mybir.ActivationFunctionType
ALU = mybir.AluOpType

D = 1024
NORM_EPS = 1e-5
GN_EPS = 64e-5
NDS = 12


class Buf:
    __slots__ = ("lw", "rd")

    def __init__(self):
        self.lw = None
        self.rd = {}


class TT:
    def __init__(self, t, nsub=0):
        self.t = t
        self.b = Buf()
        self.subs = [Buf() for _ in range(nsub)]

    def __getitem__(self, idx):
        return self.t[idx]


class _Rec:
    def __init__(self):
        self.call = None

    def __getattr__(self, name):
        def f(*a, **kw):
            self.call = (name, a, kw)
            return self
        return f


class KB:
    def __init__(self, nc):
        self.nc = nc
        self.eng = {"pe": nc.tensor, "act": nc.scalar, "dve": nc.vector, "pool": nc.gpsimd, "sp": nc.sync}
        self.sem = {n: nc.alloc_semaphore("s_" + n) for n in ["pe", "act", "dve", "pool"]}
        self.cnt = {n: 0 for n in ["pe", "act", "dve", "pool"]}
        self.waited = {}
        self.dsems = {q: [nc.alloc_semaphore("d%s%d" % (q, i)) for i in range(NDS)] for q in ("sp", "pool")}
        self.dtot = {q: [0] * NDS for q in ("sp", "pool")}
        self.dnext = {"sp": 0, "pool": 0}
        self.nid = 0
        self.bregs = {}
        self.rec = None
        self.last_tile = 2

    def _wait(self, eng, tok):
        if tok is None:
            return
        key, sem, val = tok
        if eng == "pe" and key == "pe":
            return
        if self.waited.get((eng, key), 0) >= val:
            return
        self.waited[(eng, key)] = val
        self.eng[eng].wait_ge(sem, val)

    def _deps(self, eng, R, W):
        for b in R:
            self._wait(eng, b.lw)
        for b in W:
            self._wait(eng, b.lw)
            for t in b.rd.values():
                self._wait(eng, t)

    def _post(self, tok, R, W):
        for b in R:
            b.rd[tok[0]] = tok
        for b in W:
            b.lw = tok
            b.rd = {}

    def op(self, eng, fn, R=(), W=()):
        R = [x.b if isinstance(x, TT) else x for x in R]
        W = [x.b if isinstance(x, TT) else x for x in W]
        if self.rec is not None:
            r = _Rec()
            fn(r)
            self.rec.append(("op", eng, r.call, R, W))
            return None
        if eng == "pe":
            tid = self._pe_tile(fn)
            if tid != self.last_tile and tid in (0, 1) and self.last_tile in (0, 1) and self.cnt["pe"] > 0:
                self.eng["pe"].wait_ge(self.sem["pe"], self.cnt["pe"])
            self.last_tile = tid
        self._deps(eng, R, W)
        ins = fn(self.eng[eng])
        self.cnt[eng] += 1
        ins.then_inc(self.sem[eng], 1)
        tok = (eng, self.sem[eng], self.cnt[eng])
        self._post(tok, R, W)
        return tok

    def dma(self, out, in_, R=(), W=(), q="sp", indirect=None):
        R = [x.b if isinstance(x, TT) else x for x in R]
        W = [x.b if isinstance(x, TT) else x for x in W]
        if self.rec is not None:
            self.rec.append(("dma", out, in_, R, W, q, indirect))
            return None
        i = self.dnext[q]
        self.dnext[q] = (i + 1) % NDS
        sem = self.dsems[q][i]
        key = "d%s%d" % (q, i)
        if self.dtot[q][i] > 0:
            self._wait(q, (key, sem, self.dtot[q][i]))
        self._deps(q, R, W)
        if indirect is None:
            ins = self.eng[q].dma_start(out=out, in_=in_)
        else:
            kind, idx = indirect[0], indirect[1]
            off = bass.IndirectOffsetOnAxis(ap=idx, axis=0)
            extra = {}
            if len(indirect) > 2:
                if indirect[2] not in self.bregs:
                    self.bregs[indirect[2]] = self.eng[q].to_reg(indirect[2])
                extra = dict(bounds_check=self.bregs[indirect[2]], oob_is_err=False)
            ins = self.eng[q].indirect_dma_start(out=out, out_offset=(off if kind == "scatter" else None), in_=in_,
                                                 in_offset=(off if kind == "gather" else None), **extra)
        self.dtot[q][i] += 16
        ins.then_inc(sem, 16)
        tok = (key, sem, self.dtot[q][i])
        self._post(tok, R, W)
        return tok

    def _pe_tile(self, fn):
        r = _Rec()
        fn(r)
        name, a, kw = r.call
        ap = kw.get("in_") if name == "transpose" else (kw.get("lhsT") if "lhsT" in kw else (a[1] if len(a) > 1 else None))
        if ap is None:
            return 2
        st, sz = ap.start_partition(), ap.partition_size()
        if sz > 64:
            return 2
        return 0 if st < 64 else 1

    def begin_rec(self):
        self.rec = []

    def end_rec(self):
        r, self.rec = self.rec, None
        return r

    def _play1(self, it):
        if it[0] == "op":
            _, eng, (name, a, kw), R, W = it
            self.op(eng, lambda e: getattr(e, name)(*a, **kw), R, W)
        else:
            _, out, in_, R, W, q, indirect = it
            self.dma(out, in_, R, W, q, indirect)

    def play_merged(self, X, Y=()):
        nx, ny = len(X), len(Y)
        ix = iy = 0
        def is_pe(it):
            return it[0] == "op" and it[1] == "pe"
        while ix < nx or iy < ny:
            if iy >= ny or (ix < nx and ix * ny <= iy * nx):
                self._play1(X[ix]); ix += 1
                while ix < nx and is_pe(X[ix - 1]) and is_pe(X[ix]):
                    self._play1(X[ix]); ix += 1
            else:
                self._play1(Y[iy]); iy += 1
                while iy < ny and is_pe(Y[iy - 1]) and is_pe(Y[iy]):
                    self._play1(Y[iy]); iy += 1

    def sb(self, name, shape, dt=F32, nsub=0, scope=None):
        self.nid += 1
        nm = "%s_%d" % (name, self.nid)
        if scope is None:
            return TT(self.nc.alloc_sbuf_tensor(nm, list(shape), dt), nsub)
        return TT(scope.enter_context(self.nc.sbuf_tensor(nm, list(shape), dt)), nsub)

    def barrier(self):
        names = ["pe", "act", "dve", "pool", "sp"]
        for e in names:
            for n in ["pe", "act", "dve", "pool"]:
                if n != e and self.cnt[n] > 0:
                    self._wait(e, (n, self.sem[n], self.cnt[n]))
            for q in ("sp", "pool"):
                for i in range(NDS):
                    if self.dtot[q][i] > 0:
                        self._wait(e, ("d%s%d" % (q, i), self.dsems[q][i], self.dtot[q][i]))

    def finish(self):
        for q in ("sp", "pool"):
            for i in range(NDS):
                if self.dtot[q][i] > 0:
                    self.eng["sp"].wait_ge(self.dsems[q][i], self.dtot[q][i])
        for n in ["pe", "act", "dve", "pool"]:
            if self.cnt[n] > 0:
                self.eng["sp"].wait_ge(self.sem[n], self.cnt[n])


KSTOP = 0
GVAR = 0
PMODE = 1


def build(T, NB, E, GM=256, dbg=None):
    nc = bass.Bass("TRN2", target_bir_lowering=False)
    k = KB(nc)
    NT = T // 128
    NG = T // GM
    TPG = GM // 128
    EG = min(512, T)

    def din(name, shape):
        return nc.dram_tensor(name, list(shape), F32, kind="ExternalInput").ap()

    x_d = din("x", [NB, T, D])
    cT_d = din("cT", [128, 8, NB])
    adaw_d = din("ada_w", [D, 6 * D])
    adab_fm_d = din("ada_b_fm", [128, 48])
    adab_row_d = din("ada_b_row", [1, 6 * D])
    g12_d = din("g12_fm", [128, 2, 8])
    win_d = din("w_in", [D, 3328])
    convw_d = din("conv_w_fm", [128, 4, 3])
    convgn_d = din("conv_gn_fm", [128, 4])
    mu_d = din("mu_fm", [128, 14])
    vecs_d = din("vecs_fm", [128, 7, 4])
    lora_d = din("lora_up", [128, 512])
    gup_d = din("g_up", [128, 512])
    wout_d = din("w_out", [D, D])
    rw_d = din("router_w", [D, 32])
    rb_d = din("router_b_bc", [128, 32])
    w1_d = din("w1", [E * D, 2 * D])
    b1_d = din("b1_rows", [E * 128, 16])
    w2_d = din("w2", [E * D, D])
    b2_d = din("b2", [E, D])
    NTC = NB * (T // 128)
    BLK = 512
    NBLK = (NTC * 128 * 4) // BLK + 32
    bs_d = din("blkstart", [128, NBLK])
    iop_d = din("iotaP", [128, 8])
    xs_d = nc.dram_tensor("xs_scr", [NBLK * BLK, D], BF, kind="Internal").ap()
    yb_d = nc.dram_tensor("yb_scr", [NBLK * BLK, D], F32, kind="Internal").ap()
    fg_d = din("final_g_bc", [128, D])
    cm_d = din("cmats", [128, 7, 128])
    rm_d = din("resetmask", [128, GM])
    cm2_d = din("cmats2", [128, 4, 256])
    sel_d = din("selb", [NB, NB, 128])
    ones_d = din("ones_row", [1, NB])
    g2row_d = din("g2_row", [NB, D])
    modscr_d = nc.dram_tensor("modscr", [NB, 4, D], F32, kind="Internal").ap()
    out_d = nc.dram_tensor("out", [NB, T, D], F32, kind="ExternalOutput").ap()
    h1_d = nc.dram_tensor("h1_scratch", [NB, T, D], F32, kind=("ExternalOutput" if dbg else "Internal")).ap()

    banks = [TT(nc.alloc_psum_tensor("psb%d" % i, [128, 512], F32)) for i in range(6)]
    PSX = ExitStack()
    banks += [TT(PSX.enter_context(nc.psum_tensor("psbx%d" % i, [128, 512], F32))) for i in range(2)]
    bbanks = []
    bstate = {"n": 0, "b": 0, "pool": None, "nx": 0, "ny": 0}

    reserved = set()

    def psum():
        if bstate["pool"] == "X":
            bstate["nx"] += 1
            return banks[bstate["nx"] % 6]
        if bstate["pool"] == "Y":
            bstate["ny"] += 1
            return banks[6 + bstate["ny"] % 2]
        while True:
            i = bstate["n"] % len(banks)
            bstate["n"] += 1
            if i not in reserved:
                return banks[i]

    def psum_bf():
        b = bbanks[bstate["b"] % 2]
        bstate["b"] += 1
        return b

    def mm(ps, out_ap, pairs, R):
        n = len(pairs)
        tok = None
        for i, (l, r) in enumerate(pairs):
            tok = k.op("pe", lambda e, l=l, r=r, i=i: e.matmul(out_ap, l, r, start=(i == 0), stop=(i == n - 1)),
                       R=R, W=[ps])
        return tok

    P0 = ExitStack()
    MX = ExitStack()
    ME = ExitStack()
    cm = k.sb("cm", [128, 7, 128])
    k.dma(cm[:], cm_d[:, :, :], W=[cm])
    ident, m_su, m_sl, m_ui, bones = (cm[:, i, :] for i in range(5))
    cmb = k.sb("cmb", [128, 128], BF)
    k.op("dve", lambda e: e.tensor_copy(out=cmb[:], in_=ident), R=[cm], W=[cmb])
    smallw = k.sb("smallw", [128, 48 + 16 + 12 + 4 + 14 + 28])
    o = 0
    adab_fm = smallw[:, o:o + 48]; k.dma(adab_fm, adab_fm_d[:, :], W=[smallw]); o += 48
    g12 = smallw[:, o:o + 16]; k.dma(g12, g12_d.rearrange("p a b -> p (a b)"), W=[smallw]); o += 16
    cw = smallw[:, o:o + 12]; k.dma(cw, convw_d.rearrange("p a b -> p (a b)"), W=[smallw]); o += 12
    cgn = smallw[:, o:o + 4]; k.dma(cgn, convgn_d[:, :], W=[smallw]); o += 4
    mu = smallw[:, o:o + 14]; k.dma(mu, mu_d[:, :], W=[smallw]); o += 14
    vecs = smallw[:, o:o + 28]; k.dma(vecs, vecs_d.rearrange("p a b -> p (a b)"), W=[smallw]); o += 28

    def vec(v, i):
        return vecs[:, v * 4 + i:v * 4 + i + 1]

    rw = k.sb("rw", [128, 8, 32]); k.dma(rw[:], rw_d.rearrange("(c p) n -> p c n", p=128), W=[rw])
    rb = k.sb("rb", [128, 32]); k.dma(rb[:], rb_d[:, :], W=[rb])
    cT = k.sb("cT", [128, 8 * NB]); k.dma(cT[:], cT_d.rearrange("p a b -> p (a b)"), W=[cT])

    small = k.sb("small", [128, 64])
    gtb = [[None, None] for _ in range(NB)]
    GS = [k.sb("GS", [128, 32]) for _ in range(NB)]
    nstat = [k.sb("nstat", [128, 4]) for _ in range(2)]
    xn = [k.sb("xn", [128, D])]
    junk = k.sb("junk", [128, D], BF)
    cact = k.sb("cact", [128, 8 * NB])
    csg = k.sb("csg", [128, 8 * NB])
    modfm = [k.sb("modfm", [128, 48]) for _ in range(NB)]
    winb = k.sb("winb", [128, 8, 3328], BF, scope=MX)
    woutb = k.sb("woutb", [128, 8, D], BF, scope=MX)
    rmask = k.sb("rmask", [128, GM], scope=MX)
    k.dma(rmask[:], rm_d[:, :], W=[rmask])
    lora = k.sb("lora", [128, 512], scope=MX); k.dma(lora[:], lora_d[:, :], W=[lora])
    gup = k.sb("gup", [128, 512], scope=MX); k.dma(gup[:], gup_d[:, :], W=[gup])
    gt1t = k.sb("gtb1", [128, D], scope=MX)
    selb = k.sb("selb", [NB, NB, 128], scope=P0)
    k.dma(selb[:], sel_d[:, :, :], W=[selb])
    stage = [k.sb("stage", [128, 2048], scope=P0) for _ in range(2)]
    sidx = {"n": 0}

    def next_stage():
        s = stage[sidx["n"] % 2]
        sidx["n"] += 1
        return s
    awbs = [k.sb("awb", [128, 8, 512], scope=P0) for _ in range(2)]
    modrow = k.sb("modrow", [NB, 4, D], scope=P0)
    adabr = k.sb("adabr", [NB, 4, D], scope=P0)
    g2row = k.sb("g2row", [NB, D], scope=P0)
    k.dma(g2row[:], g2row_d[:, :], W=[g2row])

    k.op("act", lambda e: e.activation(out=csg[:], in_=cT[:], func=AF.Sigmoid), R=[cT], W=[csg])
    k.op("dve", lambda e: e.tensor_tensor(out=cact[:], in0=cT[:], in1=csg[:], op=ALU.mult), R=[cT, csg], W=[cact])
    ps_fm = psum()
    reserved.add(banks.index(ps_fm))
    for blk in range(12):
        awb = awbs[blk % 2]
        for h in range(4):
            st = next_stage()
            k.dma(st[:, 0:1024].rearrange("p (c n) -> p c n", c=2),
                  adaw_d[h * 256:(h + 1) * 256, blk * 512:(blk + 1) * 512].rearrange("(c p) n -> p c n", p=128),
                  W=[st])
            k.op("act", lambda e, st=st, h=h, awb=awb: e.activation(
                out=awb[:, 2 * h:2 * h + 2, :], in_=st[:, 0:1024].rearrange("p (c n) -> p c n", c=2), func=AF.Copy),
                R=[st], W=[awb])
        for jj in range(4):
            j = blk * 4 + jj
            mm(ps_fm, ps_fm[:, j * NB:(j + 1) * NB],
               [(awb[:, kc, jj * 128:(jj + 1) * 128], cact[:, kc * NB:(kc + 1) * NB]) for kc in range(8)],
               R=[awb, cact])
        if blk in (4, 5, 6, 7, 8, 9, 10, 11):
            ps_r = psum()
            pairs = [(cact[:, kc * NB:(kc + 1) * NB], awb[:, kc, :]) for kc in range(8)]
            mm(ps_r, ps_r[0:NB, :], pairs, R=[awb, cact])
            which = {4: 0, 5: 0, 10: 1, 11: 1, 6: 2, 7: 2, 8: 3, 9: 3}[blk]
            half = blk % 2
            k.op("act", lambda e, ps_r=ps_r, which=which, half=half: e.activation(
                out=modrow[:, which, half * 512:(half + 1) * 512], in_=ps_r[0:NB, :], func=AF.Copy),
                R=[ps_r], W=[modrow])
    for b in range(NB):
        k.op("dve", lambda e, b=b: e.tensor_tensor(out=modfm[b][:], in0=ps_fm[:, b:48 * NB:NB], in1=adab_fm, op=ALU.add),
             R=[ps_fm, smallw], W=[modfm[b]])
    reserved.clear()
    for b in range(NB):
        for which, c0 in ((0, 2 * D), (1, 5 * D), (2, 3 * D), (3, 4 * D)):
            k.dma(adabr[b:b + 1, which, :], adab_row_d[0:1, c0:c0 + D], W=[adabr])
    k.op("dve", lambda e: e.tensor_tensor(out=modrow[:], in0=modrow[:], in1=adabr[:], op=ALU.add),
         R=[adabr], W=[modrow])
    k.op("dve", lambda e: e.scalar_tensor_tensor(out=modrow[:, 3, :], in0=modrow[:, 3, :], scalar=1.0, in1=g2row[:],
                                                 op0=ALU.add, op1=ALU.mult), R=[g2row], W=[modrow])
    k.dma(modscr_d[:, :, :], modrow[:, 0:4, :], R=[modrow])
    for b in range(NB):
        for n, (sc0, sh0) in enumerate(((8, 0), (32, 24))):
            k.op("dve", lambda e, b=b, n=n, sc0=sc0: e.scalar_tensor_tensor(
                out=GS[b][:, n * 16:n * 16 + 8], in0=modfm[b][:, sc0:sc0 + 8], scalar=1.0,
                in1=g12[:, n * 8:(n + 1) * 8], op0=ALU.add, op1=ALU.mult), R=[modfm[b], smallw], W=[GS[b]])
            k.op("dve", lambda e, b=b, n=n, sh0=sh0: e.tensor_copy(
                out=GS[b][:, n * 16 + 8:n * 16 + 16], in_=modfm[b][:, sh0:sh0 + 8]), R=[modfm[b]], W=[GS[b]])

    nctr = {"n": 0}

    def rstd_of(src_ap, srcbuf):
        i = nctr["n"] % 2
        nctr["n"] += 1
        st = nstat[i]
        k.op("act", lambda e: e.activation(out=junk[:], in_=src_ap, func=AF.Square, accum_out=st[:, 0:1]),
             R=[srcbuf], W=[junk, st])
        k.op("dve", lambda e: e.tensor_scalar(out=st[:, 1:2], in0=st[:, 0:1], scalar1=1.0 / D, scalar2=NORM_EPS,
                                              op0=ALU.mult, op1=ALU.add), R=[st], W=[st])
        k.op("act", lambda e: e.activation(out=st[:, 2:3], in_=st[:, 1:2], func=AF.Sqrt), R=[st], W=[st])
        k.op("dve", lambda e: e.reciprocal(out=st[:, 3:4], in_=st[:, 2:3]), R=[st], W=[st])
        return st, i

    def norm_T(src_ap, srcbuf, gs, goff, evac, xi=0):
        st, i = rstd_of(src_ap, srcbuf)
        i = xi
        k.op("dve", lambda e: e.tensor_scalar(out=xn[i][:], in0=src_ap, scalar1=st[:, 3:4], scalar2=None, op0=ALU.mult),
             R=[srcbuf, st], W=[xn[i]])
        for hb in range(2):
            ps = psum()
            for c in range(4):
                kc = hb * 4 + c
                k.op("pe", lambda e, ps=ps, c=c, kc=kc: e.transpose(out=ps[:, c * 128:(c + 1) * 128],
                                                                     in_=xn[i][:, kc * 128:(kc + 1) * 128], identity=ident),
                     R=[xn[i], cm], W=[ps])
            for c in range(4):
                kc = hb * 4 + c
                evac(kc, ps[:, c * 128:(c + 1) * 128], gs[:, goff + kc:goff + kc + 1], gs[:, goff + 8 + kc:goff + 9 + kc], ps)

    for kc in range(8):
        for half in range(2):
            st = next_stage()
            k.dma(st[:, 0:1664], win_d[kc * 128:(kc + 1) * 128, half * 1664:(half + 1) * 1664], W=[st])
            k.op("act" if half else "dve", lambda e, st=st, kc=kc, half=half: (
                e.activation(out=winb[:, kc, half * 1664:(half + 1) * 1664], in_=st[:, 0:1664], func=AF.Copy) if half else
                e.tensor_copy(out=winb[:, kc, half * 1664:(half + 1) * 1664], in_=st[:, 0:1664])), R=[st], W=[winb])
    for kc in range(8):
        st = next_stage()
        k.dma(st[:, 0:D], wout_d[kc * 128:(kc + 1) * 128, :], W=[st])
        k.op("act", lambda e, st=st, kc=kc: e.activation(out=woutb[:, kc, :], in_=st[:, 0:D], func=AF.Copy), R=[st], W=[woutb])

    k.barrier()
    P0.close()
    def ksb(*a, **kw):
        return k.sb(*a, scope=MX, **kw)
    xgs = [ksb("xg", [128, TPG, D], nsub=TPG) for _ in range(2)]
    uT = ksb("uT", [128, 8, GM], BF)
    mixT = ksb("mixT", [128, 8, GM], BF, nsub=8)
    pbuf = [ksb("pbuf", [128, GM + 1]) for _ in range(14)]
    pl12, pl13 = ksb("pl12", [128, GM]), ksb("pl13", [128, GM])
    rkv = [[ksb("rkv", [128, GM]) for _ in range(3)] for _ in range(2)]
    ggs = [ksb("gg", [128, GM]) for _ in range(2)]
    bonuss = [ksb("bonus", [128, GM]) for _ in range(2)]
    PLs = [ksb("PL", [128, 2 * TPG]) for _ in range(2)]
    ucv = [ksb("ucv", [128, GM + 2]) for _ in range(4)]
    NTMP = 26
    tmps = [ksb("tmp", [128, GM]) for _ in range(NTMP)]
    Rtbs = [ksb("Rtb", [128, GM], BF) for _ in range(2)]
    TTts = [[ksb("TTt", [128, 512], BF) for _ in range(TPG)] for _ in range(2)]
    padss = [[[ksb("pads", [128, 3, 128], BF) for _ in range(TPG)] for _ in range(2)] for _ in range(2)]
    UTpad = [ksb("UTpad", [128, 128], BF) for _ in range(2)]
    Xs = [[ksb("Xs", [128, 2, 128], BF) for _ in range(2)] for _ in range(TPG)]
    XTs = [[ksb("XTs", [128, 2, 128], BF) for _ in range(2)] for _ in range(TPG)]
    Tms = [[ksb("Tms", [128, 2, 128], BF) for _ in range(2)] for _ in range(TPG)]
    MakT = [ksb("MakT", [128, 2, 128], BF) for _ in range(TPG)]
    MT = [ksb("MT", [128, 2, 128], BF) for _ in range(TPG)]
    Mbr = [ksb("Mbr", [128, 2, 128], BF) for _ in range(TPG)]
    Mkr = [ksb("Mkr", [128, 2, 128], BF) for _ in range(TPG)]
    Atbs = [ksb("Atb", [128, GM], BF) for _ in range(2)]
    Btbs = [ksb("Btb", [128, GM], BF) for _ in range(2)]
    Ktbs = [ksb("Ktb", [128, GM], BF) for _ in range(2)]
    Sdec = ksb("Sdec", [128, 128])
    cm2 = ksb("cm2", [128, 4, 256])
    k.dma(cm2[:], cm2_d[:, :, :], W=[cm2])
    mk2_su, mk2_sl, mk2_ui, mk2_id = (cm2[:, i_, :] for i_ in range(4))
    Ah = [ksb("Ah", [128, 128], BF) for _ in range(TPG)]
    WT2 = [ksb("WT2", [128, 128]) for _ in range(TPG)]
    STf = [ksb("STf", [128, 128]) for _ in range(4)]
    STb = [ksb("STb", [128, 128], BF) for _ in range(4)]

    for hh in range(2):
        for tb in range(TPG):
            for par_ in range(2):
                k.op("dve", lambda e: e.memset(padss[par_][hh][tb][:], 0.0), W=[padss[par_][hh][tb]])
        k.op("dve", lambda e, hh=hh: e.memset(UTpad[hh][:], 0.0), W=[UTpad[hh]])

    def proj(j):
        ps = psum()
        mm(ps, ps[:, 0:GM], [(winb[:, kc, j * 128:(j + 1) * 128], uT[:, kc, :]) for kc in range(8)], R=[winb, uT])
        return ps

    def bsum(src, srcbuf):
        ps = psum()
        mm(ps, ps[:, 0:GM], [(bones, src)], R=[cm, srcbuf])
        return ps

    def rsqrt_ps(ps, scale, eps, dst):
        k.op("dve", lambda e: e.tensor_scalar(out=dst[:], in0=ps[:, 0:GM], scalar1=scale, scalar2=eps, op0=ALU.mult, op1=ALU.add),
             R=[ps], W=[dst])
        k.op("act", lambda e: e.activation(out=dst[:], in_=dst[:], func=AF.Sqrt), R=[dst], W=[dst])
        k.op("dve", lambda e: e.reciprocal(out=dst[:], in_=dst[:]), R=[dst], W=[dst])

    def stage_U(b_, g_):
        xg_ = xgs[(b_ * NG + g_) % 2]
        for tt in range(TPG):
            k.dma(xg_[:, tt, :], x_d[b_, g_ * GM + tt * 128:g_ * GM + (tt + 1) * 128, :], W=[xg_.subs[tt]])

            def ev(kc, psap, sc, bi, ps, tt=tt):
                k.op("act", lambda e: e.activation(out=uT[:, kc, tt * 128:(tt + 1) * 128], in_=psap, func=AF.Identity,
                                                   scale=sc, bias=bi), R=[ps, GS[b_]], W=[uT])
            norm_T(xg_[:, tt, :], xg_.subs[tt], GS[b_], 0, ev)

    h1ctr = {"n": 0}
    for b in range(NB):
        k.dma(gt1t[:], modscr_d[b, 0, :].partition_broadcast(128), W=[gt1t])
        for q in range(14):
            k.op("dve", lambda e, q=q: e.memset(pbuf[q][:, 0:1], 0.0), W=[pbuf[q]])
        for ci in range(4):
            k.op("dve", lambda e, ci=ci: e.memset(ucv[ci][:, 0:2], 0.0), W=[ucv[ci]])
            k.op("dve", lambda e, ci=ci: e.memset(STf[ci][:], 0.0), W=[STf[ci]])
            k.op("dve", lambda e, ci=ci: e.memset(STb[ci][:], 0.0), W=[STb[ci]])
        for g in range(NG):
            t0 = g * GM
            xg = xgs[(b * NG + g) % 2]
            if b == 0 and g == 0:
                stage_U(0, 0)
            for ci in range(4):
                tB, tC, t1, yb, ysq, rr = tmps[6 * (ci % 2) + 3:6 * (ci % 2) + 9]
                ps = proj(ci)
                k.op("act", lambda e, ps=ps: e.activation(out=tB[:], in_=ps[:, 0:GM], func=AF.Copy), R=[ps], W=[tB])
                ps = proj(4 + ci)
                k.op("act", lambda e, ps=ps: e.activation(out=tC[:], in_=ps[:, 0:GM], func=AF.Copy), R=[ps], W=[tC])
                ps = proj(8 + ci)
                u = ucv[ci]
                k.op("dve", lambda e, ps=ps: e.tensor_tensor(out=u[:, 2:2 + GM], in0=tC[:], in1=ps[:, 0:GM], op=ALU.mult),
                     R=[tC, ps], W=[u])
                k.op("dve", lambda e: e.tensor_scalar(out=t1[:], in0=u[:, 0:GM], scalar1=cw[:, ci * 3:ci * 3 + 1], scalar2=None,
                                                      op0=ALU.mult), R=[u, smallw], W=[t1])
                for kk_ in (1, 2):
                    k.op("dve", lambda e, kk_=kk_: e.scalar_tensor_tensor(out=t1[:], in0=u[:, kk_:kk_ + GM],
                                                                           scalar=cw[:, ci * 3 + kk_:ci * 3 + kk_ + 1], in1=t1[:],
                                                                           op0=ALU.mult, op1=ALU.add), R=[u, smallw], W=[t1])
                k.op("dve", lambda e: e.tensor_tensor(out=yb[:], in0=tB[:], in1=t1[:], op=ALU.mult), R=[tB, t1], W=[yb])
                k.op("act", lambda e: e.activation(out=ysq[:], in_=yb[:], func=AF.Square), R=[yb], W=[ysq])
                psn = bsum(ysq[:], ysq)
                rsqrt_ps(psn, 1.0 / 64, NORM_EPS, rr)
                k.op("dve", lambda e: e.scalar_tensor_tensor(out=mixT[:, ci, :], in0=yb[:], scalar=cgn[:, ci:ci + 1], in1=rr[:],
                                                             op0=ALU.mult, op1=ALU.mult), R=[yb, rr, smallw], W=[mixT.subs[ci]])
                k.op("dve", lambda e: e.tensor_copy(out=u[:, 0:2], in_=u[:, GM:GM + 2]), R=[], W=[u])
            def lerp_tile(q, dst):
                ps = proj(12 + q)
                pb = pbuf[q]
                d_ = tmps[24 + (q % 2)]
                k.op("act", lambda e: e.activation(out=pb[:, 1:GM + 1], in_=ps[:, 0:GM], func=AF.Copy), R=[ps], W=[pb])
                k.op("dve", lambda e: e.tensor_tensor(out=d_[:], in0=pb[:, 0:GM], in1=pb[:, 1:GM + 1], op=ALU.subtract),
                     R=[pb], W=[d_])
                k.op("dve", lambda e: e.scalar_tensor_tensor(out=dst[:], in0=d_[:], scalar=mu[:, q:q + 1], in1=pb[:, 1:GM + 1],
                                                             op0=ALU.mult, op1=ALU.add), R=[d_, pb, smallw], W=[dst])
                k.op("dve", lambda e: e.tensor_copy(out=pb[:, 0:1], in_=pb[:, GM:GM + 1]), R=[], W=[pb])
            lerp_tile(12, pl12)
            lerp_tile(13, pl13)
            th, sgm = tmps[1], tmps[2]
            k.op("act", lambda e: e.activation(out=th[0:64, :], in_=pl12[0:64, :], func=AF.Tanh), R=[pl12], W=[th])
            k.op("act", lambda e: e.activation(out=sgm[:], in_=pl13[:], func=AF.Sigmoid), R=[pl13], W=[sgm])

            def tile_A1(i, par):
                (ld, av, kk, kksq, rinv, kkn, tq, kmod, prod, lcum, Pinc, Pexc, Pinv, At, Bt, Kt, Rt) = tmps[3:20]
                gg, bonus, PL = ggs[par], bonuss[par], PLs[par]
                Atb, Btb, Ktb, Rtb, TTt, pads = Atbs[par], Btbs[par], Ktbs[par], Rtbs[par], TTts[par], padss[par]
                rl, kl, vl = rkv[par]
                lerp_tile(i, rl)
                lerp_tile(4 + i, kl)
                lerp_tile(8 + i, vl)
                cs = slice(i * 128, (i + 1) * 128)
                ps = psum()
                mm(ps, ps[:, 0:GM], [(lora[0:64, cs], th[0:64, :])], R=[lora, th])
                k.op("act", lambda e, ps=ps: e.activation(out=ld[:], in_=ps[:, 0:GM], func=AF.Sigmoid, bias=vec(0, i), scale=1.0),
                     R=[ps, smallw], W=[ld])
                k.op("dve", lambda e: e.tensor_scalar(out=ld[:], in0=ld[:], scalar1=-0.6065306597126334, scalar2=None, op0=ALU.mult),
                     R=[], W=[ld])
                ps = psum()
                mm(ps, ps[:, 0:GM], [(lora[64:128, cs], pl12[64:128, :])], R=[lora, pl12])
                k.op("act", lambda e, ps=ps: e.activation(out=av[:], in_=ps[:, 0:GM], func=AF.Sigmoid, bias=vec(1, i), scale=1.0),
                     R=[ps, smallw], W=[av])
                ps = psum()
                mm(ps, ps[:, 0:GM], [(gup[:, cs], sgm[:])], R=[gup, sgm])
                k.op("act", lambda e, ps=ps: e.activation(out=gg[:], in_=ps[:, 0:GM], func=AF.Copy), R=[ps], W=[gg])
                k.op("dve", lambda e: e.tensor_scalar(out=kk[:], in0=kl[:], scalar1=vec(2, i), scalar2=None, op0=ALU.mult),
                     R=[kl, smallw], W=[kk])
                k.op("act", lambda e: e.activation(out=kksq[:], in_=kk[:], func=AF.Square), R=[kk], W=[kksq])
                psn = bsum(kksq[:], kksq)
                k.op("act", lambda e, psn=psn: e.activation(out=rinv[:], in_=psn[:, 0:GM], func=AF.Sqrt), R=[psn], W=[rinv])
                k.op("dve", lambda e: e.tensor_scalar(out=rinv[:], in0=rinv[:], scalar1=1e-12, scalar2=None, op0=ALU.max), R=[], W=[rinv])
                k.op("dve", lambda e: e.reciprocal(out=rinv[:], in_=rinv[:]), R=[], W=[rinv])
                k.op("dve", lambda e: e.tensor_tensor(out=kkn[:], in0=kk[:], in1=rinv[:], op=ALU.mult), R=[kk, rinv], W=[kkn])
                k.op("dve", lambda e: e.tensor_scalar(out=tq[:], in0=av[:], scalar1=-1.0, scalar2=vec(3, i), op0=ALU.add, op1=ALU.mult),
                     R=[av, smallw], W=[tq])
                k.op("dve", lambda e: e.scalar_tensor_tensor(out=kmod[:], in0=tq[:], scalar=1.0, in1=kl[:], op0=ALU.add, op1=ALU.mult),
                     R=[tq, kl], W=[kmod])
                k.op("dve", lambda e: e.scalar_tensor_tensor(out=prod[:], in0=rl[:], scalar=vec(4, i), in1=kmod[:], op0=ALU.mult,
                                                             op1=ALU.mult), R=[rl, kmod, smallw], W=[prod])
                psrk = bsum(prod[:], prod)
                k.op("dve", lambda e, psrk=psrk: e.tensor_tensor(out=bonus[:], in0=psrk[:, 0:GM], in1=vl[:], op=ALU.mult),
                     R=[psrk, vl], W=[bonus])
                k.op("dve", lambda e: e.tensor_tensor_scan(out=lcum[:], data0=rmask[:], data1=ld[:], initial=0.0, op0=ALU.mult,
                                                           op1=ALU.add), R=[rmask, ld], W=[lcum])
                k.op("act", lambda e: e.activation(out=Pinc[:], in_=lcum[:], func=AF.Exp), R=[lcum], W=[Pinc])
                k.op("dve", lambda e: e.tensor_tensor(out=Pexc[:], in0=lcum[:], in1=ld[:], op=ALU.subtract), R=[lcum, ld], W=[Pexc])
                k.op("act", lambda e: e.activation(out=Pexc[:], in_=Pexc[:], func=AF.Exp), R=[], W=[Pexc])
                k.op("act", lambda e: e.activation(out=Pinv[:], in_=lcum[:], func=AF.Exp, scale=-1.0), R=[lcum], W=[Pinv])
                k.op("dve", lambda e: e.scalar_tensor_tensor(out=At[:], in0=kkn[:], scalar=-1.0, in1=Pexc[:], op0=ALU.mult, op1=ALU.mult),
                     R=[kkn, Pexc], W=[At])
                k.op("dve", lambda e: e.tensor_tensor(out=Bt[:], in0=kkn[:], in1=av[:], op=ALU.mult), R=[kkn, av], W=[Bt])
                k.op("dve", lambda e: e.tensor_tensor(out=Bt[:], in0=Bt[:], in1=Pinv[:], op=ALU.mult), R=[Pinv], W=[Bt])
                k.op("dve", lambda e: e.tensor_tensor(out=Kt[:], in0=kmod[:], in1=Pinv[:], op=ALU.mult), R=[kmod, Pinv], W=[Kt])
                k.op("dve", lambda e: e.tensor_tensor(out=Rt[:], in0=rl[:], in1=Pinc[:], op=ALU.mult), R=[rl, Pinc], W=[Rt])
                k.op("act", lambda e: e.activation(out=Rtb[:], in_=Rt[:], func=AF.Copy), R=[Rt], W=[Rtb])
                for src_, dst_ in ((At, Atb), (Bt, Btb), (Kt, Ktb)):
                    k.op("pool", lambda e: e.tensor_copy(out=dst_[:], in_=src_[:]), R=[src_], W=[dst_])
                for tb in range(TPG):
                    cl = slice(tb * 128, (tb + 1) * 128)
                    ps = psum()
                    for n_, src in enumerate((At, Bt, Kt, vl)):
                        k.op("pe", lambda e: e.transpose(out=ps[:, n_ * 128:(n_ + 1) * 128], in_=src[:, cl], identity=ident),
                             R=[src, cm], W=[ps])
                    k.op("act", lambda e: e.activation(out=TTt[tb][:], in_=ps[:, :], func=AF.Copy), R=[ps], W=[TTt[tb]])
                    for hh in range(2):
                        k.op("dve" if hh else "act", lambda e: (e.tensor_copy(
                            out=pads[hh][tb][:, :, hh * 64:hh * 64 + 64],
                            in_=TTt[tb][:, 128:512].rearrange("p (a c) -> p a c", a=3)[:, :, hh * 64:hh * 64 + 64]) if hh else
                            e.activation(out=pads[hh][tb][:, :, hh * 64:hh * 64 + 64],
                                         in_=TTt[tb][:, 128:512].rearrange("p (a c) -> p a c", a=3)[:, :, hh * 64:hh * 64 + 64],
                                         func=AF.Copy)),
                            R=[TTt[tb]], W=[pads[hh][tb]])
                k.op("act", lambda e: e.activation(out=PL[:], in_=Pinc[:, 63:GM:64], func=AF.Copy), R=[Pinc], W=[PL])

            def tile_X(i, par):
                Osb, dd, dsq, rs = tmps[20:24]
                gg, bonus, PL = ggs[par], bonuss[par], PLs[par]
                Atb, Btb, Ktb, Rtb, TTt, pads = Atbs[par], Btbs[par], Ktbs[par], Rtbs[par], TTts[par], padss[par]
                for tb in range(TPG):
                    cl = slice(tb * 128, (tb + 1) * 128)
                    specs = [(Btb, Atb, mk2_su, Xs[tb][0]), (Atb, Btb, mk2_sl, XTs[tb][0]), (Atb, Ktb, mk2_sl, MakT[tb]),
                             (Btb, Rtb, mk2_ui, Mbr[tb]), (Ktb, Rtb, mk2_ui, Mkr[tb])]
                    gps = [psum() for _ in specs]
                    for hh in range(2):
                        pr = slice(hh * 64, hh * 64 + 64)
                        for (l, r, mask2, dst), ps in zip(specs, gps):
                            mm(ps, ps[:, hh * 128:(hh + 1) * 128], [(l[pr, cl], r[pr, cl])], R=[l, r])
                    for (l, r, mask2, dst), ps in zip(specs, gps):
                        k.op("dve", lambda e: e.tensor_tensor(out=dst[:].rearrange("p a b -> p (a b)"), in0=ps[:, 0:256], in1=mask2,
                                                              op=ALU.mult), R=[ps, cm2], W=[dst])
                    k.op("dve", lambda e: e.tensor_tensor(out=Tms[tb][0][:].rearrange("p a b -> p (a b)"),
                                                          in0=Xs[tb][0][:].rearrange("p a b -> p (a b)"), in1=mk2_id, op=ALU.add),
                         R=[Xs[tb][0], cm2], W=[Tms[tb][0]])
                cur = 0
                for lvl in range(1, 6):
                    nxt = 1 - cur
                    pss = {}
                    for tb in range(TPG):
                        if lvl < 5:
                            ps = psum()
                            for hh in range(2):
                                mm(ps, ps[:, hh * 128:(hh + 1) * 128], [(XTs[tb][cur][:, hh, :], Xs[tb][cur][:, hh, :])],
                                   R=[XTs[tb][cur], Xs[tb][cur]])
                            pss[(tb, 0)] = ps
                        ps = psum()
                        for hh in range(2):
                            mm(ps, ps[:, hh * 128:(hh + 1) * 128], [(Xs[tb][cur][:, hh, :], XTs[tb][cur][:, hh, :])],
                               R=[XTs[tb][cur], Xs[tb][cur]])
                        pss[(tb, 1)] = ps
                    for tb in range(TPG):
                        if lvl < 5:
                            ps = pss[(tb, 0)]
                            k.op("act", lambda e: e.activation(out=Xs[tb][nxt][:].rearrange("p a b -> p (a b)"), in_=ps[:, 0:256],
                                                               func=AF.Copy), R=[ps], W=[Xs[tb][nxt]])
                        ps = pss[(tb, 1)]
                        k.op("act", lambda e: e.activation(out=XTs[tb][nxt][:].rearrange("p a b -> p (a b)"), in_=ps[:, 0:256],
                                                           func=AF.Copy), R=[ps], W=[XTs[tb][nxt]])
                    for tb in range(TPG):
                        ps = psum()
                        for hh in range(2):
                            mm(ps, ps[:, hh * 128:(hh + 1) * 128], [(XTs[tb][nxt][:, hh, :], Tms[tb][cur][:, hh, :])],
                               R=[XTs[tb][nxt], Tms[tb][cur]])
                        pss[(tb, 2)] = ps
                    for tb in range(TPG):
                        ps = pss[(tb, 2)]
                        k.op("dve", lambda e: e.tensor_tensor(out=Tms[tb][nxt][:].rearrange("p a b -> p (a b)"), in0=ps[:, 0:256],
                                                              in1=Tms[tb][cur][:].rearrange("p a b -> p (a b)"), op=ALU.add),
                             R=[ps, Tms[tb][cur]], W=[Tms[tb][nxt]])
                    cur = nxt
                pA, pM = {}, {}
                for tb in range(TPG):
                    ps = psum()
                    for hh in range(2):
                        mm(ps, ps[:, hh * 128:(hh + 1) * 128], [(TTt[tb][:, 0:128], Tms[tb][cur][:, hh, :])], R=[TTt[tb], Tms[tb][cur]])
                    pA[tb] = ps
                    ps = psum()
                    for hh in range(2):
                        mm(ps, ps[:, hh * 128:(hh + 1) * 128], [(MakT[tb][:, hh, :], Tms[tb][cur][:, hh, :])], R=[MakT[tb], Tms[tb][cur]])
                    pM[tb] = ps
                for tb in range(TPG):
                    for hh in range(2):
                        pr = slice(hh * 64, hh * 64 + 64)
                        k.op("act", lambda e: e.activation(out=Ah[tb][pr, :], in_=pA[tb][pr, hh * 128:(hh + 1) * 128], func=AF.Copy),
                             R=[pA[tb]], W=[Ah[tb]])
                    k.op("act", lambda e: e.activation(out=MT[tb][:].rearrange("p a b -> p (a b)"), in_=pM[tb][:, 0:256], func=AF.Copy),
                         R=[pM[tb]], W=[MT[tb]])
                for tb in range(TPG):
                    ps = psum()
                    for hh in range(2):
                        mm(ps, ps[:, hh * 64:(hh + 1) * 64], [(MT[tb][:, hh, :], TTt[tb][:, 384 + hh * 64:384 + hh * 64 + 64])],
                           R=[MT[tb], TTt[tb]])
                    k.op("act", lambda e: e.activation(out=WT2[tb][:], in_=ps[:, 0:128], func=AF.Copy), R=[ps], W=[WT2[tb]])
                for tb in range(TPG):
                    for c in range(2):
                        ro = slice(64 * c, 64 * c + 64)
                        cc0 = tb * 128 + 64 * c
                        k.op("dve", lambda e: e.tensor_scalar(out=Sdec[:], in0=STf[i][:], scalar1=PL[:, 2 * tb + c:2 * tb + c + 1], scalar2=None,
                                                              op0=ALU.mult), R=[STf[i], PL], W=[Sdec])
                        ps = psum()
                        mm(ps, ps[:, 0:128], [(Ah[tb][:], STb[i][:])], R=[Ah[tb], STb[i]])
                        for hh in range(2):
                            hs = slice(hh * 64, hh * 64 + 64)
                            k.op("dve", lambda e: e.tensor_tensor(out=UTpad[hh][ro, hs], in0=ps[ro, hs], in1=WT2[tb][ro, hs], op=ALU.add),
                                 R=[ps, WT2[tb]], W=[UTpad[hh]])
                        pss_ = psum()
                        mm(pss_, pss_[:, 0:128],
                           [(pads[0][tb][ro, 0, :], UTpad[0][ro, :]), (pads[1][tb][ro, 0, :], UTpad[1][ro, :]),
                            (pads[0][tb][ro, 1, :], pads[0][tb][ro, 2, :]), (pads[1][tb][ro, 1, :], pads[1][tb][ro, 2, :])],
                           R=[pads[0][tb], pads[1][tb], UTpad[0], UTpad[1]])
                        k.op("dve", lambda e: e.scalar_tensor_tensor(out=STf[i][:], in0=pss_[:, 0:128], scalar=PL[:, 2 * tb + c:2 * tb + c + 1],
                                                                     in1=Sdec[:], op0=ALU.mult, op1=ALU.add),
                             R=[pss_, PL, Sdec], W=[STf[i]])
                        pso = psum()
                        mm(pso, pso[:, 0:64],
                           [(STb[i][:], Rtb[:, cc0:cc0 + 64]),
                            (UTpad[0][ro, :], Mbr[tb][ro, 0, ro]), (UTpad[1][ro, :], Mbr[tb][ro, 1, ro]),
                            (pads[0][tb][ro, 2, :], Mkr[tb][ro, 0, ro]), (pads[1][tb][ro, 2, :], Mkr[tb][ro, 1, ro])],
                           R=[STb[i], Rtb, UTpad[0], UTpad[1], Mbr[tb], Mkr[tb], pads[0][tb], pads[1][tb]])
                        k.op("act", lambda e: e.activation(out=STb[i][:], in_=STf[i][:], func=AF.Copy), R=[STf[i]], W=[STb[i]])
                        k.op("act", lambda e: e.activation(out=Osb[:, cc0:cc0 + 64], in_=pso[:, 0:64], func=AF.Copy), R=[pso], W=[Osb])
                psm = bsum(Osb[:], Osb)
                k.op("dve", lambda e, psm=psm: e.scalar_tensor_tensor(out=dd[:], in0=psm[:, 0:GM], scalar=-1.0 / 64, in1=Osb[:],
                                                                        op0=ALU.mult, op1=ALU.add), R=[psm, Osb], W=[dd])
                k.op("act", lambda e: e.activation(out=dsq[:], in_=dd[:], func=AF.Square), R=[dd], W=[dsq])
                psv = bsum(dsq[:], dsq)
                rsqrt_ps(psv, 1.0 / 64, GN_EPS, rs)
                k.op("dve", lambda e: e.tensor_tensor(out=dd[:], in0=dd[:], in1=rs[:], op=ALU.mult), R=[rs], W=[dd])
                k.op("dve", lambda e: e.tensor_scalar(out=dd[:], in0=dd[:], scalar1=vec(5, i), scalar2=vec(6, i), op0=ALU.mult, op1=ALU.add),
                     R=[smallw], W=[dd])
                k.op("dve", lambda e: e.tensor_tensor(out=dd[:], in0=dd[:], in1=bonus[:], op=ALU.add), R=[bonus], W=[dd])
                k.op("dve", lambda e: e.tensor_tensor(out=mixT[:, 4 + i, :], in0=dd[:], in1=gg[:], op=ALU.mult), R=[dd, gg],
                     W=[mixT.subs[4 + i]])
            bstate["pool"] = "Y"
            k.begin_rec(); tile_A1(0, 0); Y_ = k.end_rec()
            bstate["pool"] = None
            k.play_merged(Y_)
            for i in range(4):
                bstate["pool"] = "X"
                k.begin_rec(); tile_X(i, i % 2); X_ = k.end_rec()
                bstate["pool"] = "Y"
                k.begin_rec()
                if i < 3:
                    tile_A1(i + 1, (i + 1) % 2)
                else:
                    nxt_ = b * NG + g + 1
                    if nxt_ < NB * NG:
                        stage_U(nxt_ // NG, nxt_ % NG)
                Y_ = k.end_rec()
                bstate["pool"] = None
                if PMODE == 0:
                    k.play_merged(X_)
                    k.play_merged(Y_)
                else:
                    k.play_merged(X_, Y_)
            for tt in range(TPG):
                for half in range(2):
                    hsl = slice(half * 512, (half + 1) * 512)
                    ps = psum()
                    mm(ps, ps[:, :], [(mixT[:, c_, tt * 128:(tt + 1) * 128], woutb[:, c_, hsl]) for c_ in range(8)],
                       R=[woutb] + mixT.subs)
                    k.op("dve", lambda e, ps=ps, hsl=hsl: e.tensor_tensor(out=ps[:, :], in0=ps[:, :], in1=gt1t[:, hsl],
                                                                          op=ALU.mult), R=[gt1t], W=[ps])
                    k.op("dve", lambda e, ps=ps, hsl=hsl, tt=tt: e.tensor_tensor(out=xg[:, tt, hsl], in0=ps[:, :], in1=xg[:, tt, hsl],
                                                                                 op=ALU.add), R=[ps], W=[xg.subs[tt]])
                k.dma(h1_d[b, t0 + tt * 128:t0 + (tt + 1) * 128, :], xg[:, tt, :], R=[xg.subs[tt]], q="pool")

    if dbg == "h1":
        k.finish()
        return nc

    k.barrier()
    MX.close()
    PSX.close()
    del banks[6:]
    bbanks.extend(TT(nc.alloc_psum_tensor("psbb%d" % i, [128, 1024], BF)) for i in range(2))
    ME_P = ExitStack()
    I32 = mybir.dt.int32
    ltri_f, ones_f = cm[:, 5, :], cm[:, 6, :]

    def psb(*a, **kw):
        return k.sb(*a, scope=ME_P, **kw)
    cmb2 = psb("cmb2", [128, 2, 128], BF)
    k.op("dve", lambda e: e.tensor_copy(out=cmb2[:], in_=cm[:, 5:7, :]), R=[cm], W=[cmb2])
    lgs = psb("lgs", [128, NTC, 32])
    ranks = psb("ranks", [128, NTC, 32])
    m8s = psb("m8s", [128, NTC, 8])
    g4 = psb("g4", [128, NTC, 4])
    dest_f = psb("dest_f", [128, NTC * 4])
    dest_i = psb("dest_i", [128, NTC * 4], I32)
    cnt = psb("cnt", [128, 32])
    lay = psb("lay", [128, 5, 32])
    bs = psb("bs", [128, NBLK]); k.dma(bs[:], bs_d[:, :], W=[bs])
    iop = psb("iop", [128, 8]); k.dma(iop[:], iop_d[:, :], W=[iop])
    be = psb("be", [128, 2, NBLK])
    idxW_f = psb("idxW_f", [128, NBLK, 8])
    idxW_i = psb("idxW_i", [128, NBLK * 8], I32)
    idxW2_f = psb("idxW2_f", [128, NBLK, 8])
    idxW2_i = psb("idxW2_i", [128, NBLK * 8], I32)
    skp = psb("skp", [128, 2, NBLK])
    idxb_f = psb("idxb_f", [128, 2, NBLK])
    idxb_i = psb("idxb_i", [128, 2 * NBLK], I32)
    k.op("dve", lambda e: e.memset(cnt[:], 0.0), W=[cnt])
    k.op("dve", lambda e: e.memset(dest_f[:], 0.0), W=[dest_f])

    ME_R = ExitStack()

    def rsb(*a, **kw):
        return k.sb(*a, scope=ME_R, **kw)
    u2tm = rsb("u2tm", [128, NTC, D], BF, nsub=NTC)
    u2fs = [rsb("u2f", [128, 8, 128]) for _ in range(2)]
    xn.append(rsb("xn2", [128, D]))
    h2 = [rsb("h2", [128, D]) for _ in range(2)]
    G2bc = [rsb("G2bc", [128, D]) for _ in range(NB)]
    SH2bc = [rsb("SH2bc", [128, D]) for _ in range(NB)]
    maskbs = [rsb("maskb", [128, 32], BF) for _ in range(2)]
    rts = [rsb("rt", [128, 64]) for _ in range(2)]
    for b in range(NB):
        k.dma(SH2bc[b][:], modscr_d[b, 2, :].partition_broadcast(128), W=[SH2bc[b]])
        k.dma(G2bc[b][:], modscr_d[b, 3, :].partition_broadcast(128), W=[G2bc[b]])
    def router_A(tile):
        b, tt = tile // NT, tile % NT
        ht = h2[tile % 2]
        u2f, maskb, rt_ = u2fs[tile % 2], maskbs[tile % 2], rts[tile % 2]
        k.dma(ht[:], h1_d[b, tt * 128:(tt + 1) * 128, :], W=[ht])

        def ev2(kc, psap, sc, bi, ps):
            k.op("act", lambda e: e.activation(out=u2f[:, kc, :], in_=psap, func=AF.Identity, scale=sc, bias=bi),
                 R=[ps, GS[b]], W=[u2f])
        norm_T(ht[:], ht, GS[b], 16, ev2, xi=tile % 2)
        k.op("dve", lambda e: e.tensor_tensor(out=ht[:], in0=xn[tile % 2][:], in1=G2bc[b][:], op=ALU.mult),
             R=[xn[tile % 2], G2bc[b]], W=[ht])
        k.op("dve", lambda e: e.tensor_tensor(out=u2tm[:, tile, :], in0=ht[:], in1=SH2bc[b][:], op=ALU.add),
             R=[ht, SH2bc[b]], W=[u2tm.subs[tile]])
        ps = psum()
        mm(ps, ps[:, 0:32], [(u2f[:, kc, :], rw[:, kc, :]) for kc in range(8)], R=[u2f, rw])
        lg = lgs[:, tile, :]
        m8 = m8s[:, tile, :]
        k.op("dve", lambda e: e.tensor_tensor(out=lg, in0=ps[:, 0:32], in1=rb[:], op=ALU.add), R=[ps, rb], W=[lgs])
        k.op("dve", lambda e: e.max(out=m8, in_=lg), R=[lgs], W=[m8s])
        k.op("dve", lambda e: e.tensor_scalar(out=maskb[:], in0=lg, scalar1=m8s[:, tile, 3:4], scalar2=None, op0=ALU.is_ge),
             R=[lgs, m8s], W=[maskb])

    def router_B(tile):
        b, tt = tile // NT, tile % NT
        maskb, rt_ = maskbs[tile % 2], rts[tile % 2]
        psr = psum()
        mm(psr, psr[:, 0:32], [(cmb2[:, 0, :], maskb[:])], R=[cmb2, maskb])
        k.op("dve", lambda e: e.tensor_tensor(out=ranks[:, tile, :], in0=psr[:, 0:32], in1=cnt[:], op=ALU.add),
             R=[psr, cnt], W=[ranks])
        psc = psum()
        mm(psc, psc[:, 0:32], [(cmb2[:, 1, :], maskb[:])], R=[cmb2, maskb])
        k.op("dve", lambda e: e.tensor_tensor(out=cnt[:], in0=psc[:, 0:32], in1=cnt[:], op=ALU.add), R=[psc], W=[cnt])
        negm, ssum, ex4 = rt_[:, 0:1], rt_[:, 1:2], rt_[:, 4:8]
        k.op("dve", lambda e: e.tensor_scalar(out=negm, in0=m8s[:, tile, 0:1], scalar1=-1.0, scalar2=None, op0=ALU.mult),
             R=[m8s], W=[rt_])
        k.op("act", lambda e: e.activation(out=ex4, in_=m8s[:, tile, 0:4], func=AF.Exp, bias=negm, scale=1.0), R=[m8s], W=[rt_])
        k.op("dve", lambda e: e.tensor_reduce(out=ssum, in_=ex4, axis=mybir.AxisListType.X, op=ALU.add), R=[], W=[rt_])
        k.op("dve", lambda e: e.reciprocal(out=ssum, in_=ssum), R=[], W=[rt_])
        k.op("dve", lambda e: e.tensor_scalar(out=g4[:, tile, :], in0=ex4, scalar1=ssum, scalar2=None, op0=ALU.mult),
             R=[rt_], W=[g4])
    for t2 in range(0, NTC, 2):
        recs = []
        for tile in range(t2, min(t2 + 2, NTC)):
            k.begin_rec(); router_A(tile); recs.append(k.end_rec())
        k.play_merged(*recs)
        for tile in range(t2, min(t2 + 2, NTC)):
            router_B(tile)
    nbt, padded, pend, pstart, ones32 = (lay[:, i, :] for i in range(5))
    k.op("dve", lambda e: e.memset(ones32, 1.0), W=[lay])
    k.op("dve", lambda e: e.tensor_scalar(out=nbt, in0=cnt[:], scalar1=0.0, scalar2=None, op0=ALU.is_gt), R=[cnt], W=[lay])
    for j in range(1, (NTC * 128) // BLK):
        k.op("dve", lambda e: e.scalar_tensor_tensor(out=nbt, in0=cnt[:], scalar=float(BLK * j), in1=nbt, op0=ALU.is_gt,
                                                     op1=ALU.add), R=[cnt], W=[lay])
    k.op("dve", lambda e: e.tensor_scalar(out=padded, in0=nbt, scalar1=float(BLK), scalar2=None, op0=ALU.mult), R=[], W=[lay])
    k.op("dve", lambda e: e.tensor_tensor_scan(out=pend, data0=ones32, data1=padded, initial=0.0, op0=ALU.mult, op1=ALU.add),
         R=[], W=[lay])
    k.op("dve", lambda e: e.tensor_tensor(out=pstart, in0=pend, in1=padded, op=ALU.subtract), R=[], W=[lay])
    k.op("dve", lambda e: e.memset(be[:, 0, :], 0.0), W=[be])
    for e_ in range(32):
        k.op("dve", lambda e: e.scalar_tensor_tensor(out=be[:, 0, :], in0=bs[:], scalar=lay[:, 2, e_:e_ + 1], in1=be[:, 0, :],
                                                     op0=ALU.is_ge, op1=ALU.add), R=[bs, lay], W=[be])
    k.op("dve", lambda e: e.tensor_scalar(out=be[:, 0, :], in0=be[:, 0, :], scalar1=float(E - 1), scalar2=None, op0=ALU.min),
         R=[], W=[be])
    k.op("dve", lambda e: e.tensor_scalar(out=be[:, 1, :], in0=be[:, 0, :], scalar1=float(D), scalar2=None, op0=ALU.mult),
         R=[], W=[be])
    for kc in range(8):
        k.op("dve", lambda e: e.tensor_scalar(out=idxW_f[:, :, kc], in0=be[:, 1, :], scalar1=iop[:, kc:kc + 1], scalar2=None,
                                              op0=ALU.add), R=[be, iop], W=[idxW_f])
    BIG = 1.0e6
    k.op("dve", lambda e: e.memset(skp[:], 0.0), W=[skp])
    k.op("dve", lambda e: e.tensor_tensor(out=skp[:, 0, 2:NBLK], in0=be[:, 0, 2:NBLK], in1=be[:, 0, 0:NBLK - 2], op=ALU.is_equal),
         R=[be], W=[skp])
    k.op("dve", lambda e: e.tensor_tensor(out=skp[:, 1, 1:NBLK], in0=be[:, 0, 1:NBLK], in1=be[:, 0, 0:NBLK - 1], op=ALU.is_equal),
         R=[be], W=[skp])
    k.op("dve", lambda e: e.tensor_scalar(out=skp[:], in0=skp[:], scalar1=BIG, scalar2=None, op0=ALU.mult), R=[], W=[skp])
    for kc in range(8):
        k.op("dve", lambda e: e.tensor_tensor(out=idxW2_f[:, :, kc], in0=idxW_f[:, :, kc], in1=skp[:, 1, :], op=ALU.add),
             R=[idxW_f, skp], W=[idxW2_f])
    k.op("dve", lambda e: e.tensor_copy(out=idxW2_i[:], in_=idxW2_f[:].rearrange("p a b -> p (a b)")), R=[idxW2_f], W=[idxW2_i])
    for kc in range(8):
        k.op("dve", lambda e: e.tensor_tensor(out=idxW_f[:, :, kc], in0=idxW_f[:, :, kc], in1=skp[:, 0, :], op=ALU.add),
             R=[skp], W=[idxW_f])
    k.op("dve", lambda e: e.tensor_copy(out=idxW_i[:], in_=idxW_f[:].rearrange("p a b -> p (a b)")), R=[idxW_f], W=[idxW_i])
    k.op("dve", lambda e: e.tensor_scalar(out=idxb_f[:, 0, :], in0=be[:, 0, :], scalar1=128.0, scalar2=iop[:, 0:1],
                                          op0=ALU.mult, op1=ALU.add), R=[be, iop], W=[idxb_f])
    k.op("dve", lambda e: e.tensor_copy(out=idxb_f[:, 1, :], in_=be[:, 0, :]), R=[be], W=[idxb_f])
    k.op("dve", lambda e: e.tensor_copy(out=idxb_i[:], in_=idxb_f[:].rearrange("p a b -> p (a b)")), R=[idxb_f], W=[idxb_i])
    rt_ = rts[0]
    for tile in range(NTC):
        k.op("dve", lambda e: e.tensor_tensor(out=ranks[:, tile, :], in0=ranks[:, tile, :], in1=pstart, op=ALU.add),
             R=[lay], W=[ranks])
        for kk_ in range(4):
            k.op("dve", lambda e: e.scalar_tensor_tensor(out=rt_[:, 32:64], in0=lgs[:, tile, :], scalar=m8s[:, tile, kk_:kk_ + 1],
                                                         in1=ranks[:, tile, :], op0=ALU.is_equal, op1=ALU.mult,
                                                         accum_out=dest_f[:, tile * 4 + kk_:tile * 4 + kk_ + 1]),
                 R=[lgs, m8s, ranks], W=[rt_, dest_f])
    k.op("dve", lambda e: e.tensor_copy(out=dest_i[:], in_=dest_f[:]), R=[dest_f], W=[dest_i])
    if dbg == "moe":
        dbg_d = nc.dram_tensor("dbg", [128, NTC * 4 + NTC * 4 + 32 + NBLK], F32, kind="ExternalOutput").ap()
        k.dma(dbg_d[:, 0:NTC * 4], dest_f[:], R=[dest_f])
        k.dma(dbg_d[:, NTC * 4:NTC * 8], g4[:].rearrange("p a b -> p (a b)"), R=[g4])
        k.dma(dbg_d[:, NTC * 8:NTC * 8 + 32], cnt[:], R=[cnt])
        k.dma(dbg_d[:, NTC * 8 + 32:NTC * 8 + 32 + NBLK], be[:, 0, :], R=[be])
    for tile in range(NTC):
        for kk_ in range(4):
            c_ = tile * 4 + kk_
            k.dma(xs_d[:, :], u2tm[:, tile, :], R=[u2tm.subs[tile], dest_i], q="pool", indirect=("scatter", dest_i[:, c_:c_ + 1]))
    k.barrier()
    xn.pop()
    ME_R.close()

    ME_X = ExitStack()

    def xsb(*a, **kw):
        return k.sb(*a, scope=ME_X, **kw)
    W1b = [xsb("W1b", [128, 8, 2 * D], BF, nsub=8) for _ in range(2)]
    W2b = xsb("W2b", [128, 8, D], BF, nsub=8)
    xrow = [xsb("xrow", [128, 4, D], BF) for _ in range(2)]
    xsT = [xsb("xsT", [128, 8, BLK], BF, nsub=8) for _ in range(2)]
    actT = [xsb("actT", [128, 8, BLK], BF, nsub=8) for _ in range(2)]
    et = [[xsb("et", [128, BLK]) for _ in range(4)] for _ in range(2)]
    yrow = [xsb("yrow", [128, D]) for _ in range(2)]
    b2bc = [xsb("b2bc", [128, D]) for _ in range(2)]
    b1blk = [xsb("b1blk", [128, 16]) for _ in range(2)]
    ectr = {"n": 0, "y": 0}
    def load_weights(blk):
        w1 = W1b[blk % 2]
        b1t, b2t = b1blk[blk % 2], b2bc[blk % 2]
        k.dma(b1t[:], b1_d[:, :], R=[idxb_i], W=[b1t], q="pool", indirect=("gather", idxb_i[:, blk:blk + 1]))
        k.dma(b2t[:], b2_d[:, :], R=[idxb_i], W=[b2t], q="pool", indirect=("gather", idxb_i[:, NBLK + blk:NBLK + blk + 1]))
        for kc in range(8):
            c_ = blk * 8 + kc
            k.dma(w1[:, kc, :], w1_d[:, :], R=[idxW_i], W=[w1.subs[kc]], q="pool",
                  indirect=("gather", idxW_i[:, c_:c_ + 1], E * D - 1))

    def load_w2(blk):
        for fc in range(8):
            c_ = blk * 8 + fc
            k.dma(W2b[:, fc, :], w2_d[:, :], R=[idxW2_i], W=[W2b.subs[fc]], q="pool",
                  indirect=("gather", idxW2_i[:, c_:c_ + 1], E * D - 1))

    def load_x(blk):
        k.dma(xrow[blk % 2][:], xs_d[blk * BLK:(blk + 1) * BLK, :].rearrange("(s p) d -> p s d", p=128), W=[xrow[blk % 2]])

    def transposes(blk):
        xT = xsT[blk % 2]
        xr = xrow[blk % 2]
        for kc in range(8):
            if kc % 2 == 0:
                pb = psum_bf()
            po = (kc % 2) * 512
            for s_ in range(4):
                k.op("pe", lambda e: e.transpose(out=pb[:, po + s_ * 128:po + (s_ + 1) * 128], in_=xr[:, s_, kc * 128:(kc + 1) * 128],
                                                 identity=cmb[:]), R=[xr, cmb], W=[pb])
            k.op("act", lambda e: e.activation(out=xT[:, kc, :], in_=pb[:, po:po + 512], func=AF.Copy), R=[pb], W=[xT.subs[kc]])

    load_weights(0)
    load_x(0)
    transposes(0)
    for blk in range(NBLK):
        w1 = W1b[blk % 2]
        b1t, b2t = b1blk[blk % 2], b2bc[blk % 2]
        load_w2(blk)
        if blk + 1 < NBLK:
            load_weights(blk + 1)
            load_x(blk + 1)
        xT = xsT[blk % 2]
        aT = actT[blk % 2]
        for fc in range(8):
            xg_, sg_, xl_, tt_ = et[ectr["n"] % 2]
            ectr["n"] += 1
            psg = psum()
            mm(psg, psg[:, :], [(w1[:, kc, fc * 128:(fc + 1) * 128], xT[:, kc, :]) for kc in range(8)], R=w1.subs + xT.subs)
            psl = psum()
            mm(psl, psl[:, :], [(w1[:, kc, D + fc * 128:D + (fc + 1) * 128], xT[:, kc, :]) for kc in range(8)], R=w1.subs + xT.subs)
            k.op("dve", lambda e: e.tensor_scalar(out=xg_[:], in0=psg[:, :], scalar1=b1t[:, fc:fc + 1], scalar2=7.0,
                                                  op0=ALU.add, op1=ALU.min), R=[psg, b1t], W=[xg_])
            k.op("act", lambda e: e.activation(out=sg_[:], in_=xg_[:], func=AF.Sigmoid, scale=1.702), R=[xg_], W=[sg_])
            k.op("dve", lambda e: e.tensor_scalar(out=xl_[:], in0=psl[:, :], scalar1=b1t[:, 8 + fc:9 + fc], scalar2=7.0,
                                                  op0=ALU.add, op1=ALU.min), R=[psl, b1t], W=[xl_])
            k.op("dve", lambda e: e.tensor_scalar(out=xl_[:], in0=xl_[:], scalar1=-7.0, scalar2=1.0, op0=ALU.max, op1=ALU.add),
                 R=[], W=[xl_])
            k.op("dve", lambda e: e.tensor_tensor(out=tt_[:], in0=xg_[:], in1=sg_[:], op=ALU.mult), R=[xg_, sg_], W=[tt_])
            k.op("dve", lambda e: e.tensor_tensor(out=aT[:, fc, :], in0=tt_[:], in1=xl_[:], op=ALU.mult), R=[tt_, xl_],
                 W=[aT.subs[fc]])
        if blk + 1 < NBLK:
            transposes(blk + 1)
        for s_ in range(4):
            yr = yrow[ectr["y"] % 2]
            ectr["y"] += 1
            for half in range(2):
                hsl = slice(half * 512, (half + 1) * 512)
                ps = psum()
                mm(ps, ps[:, :], [(aT[:, fc, s_ * 128:(s_ + 1) * 128], W2b[:, fc, hsl]) for fc in range(8)], R=aT.subs + W2b.subs)
                k.op("dve", lambda e: e.tensor_tensor(out=yr[:, hsl], in0=ps[:, :], in1=b2t[:, hsl], op=ALU.add), R=[ps, b2t], W=[yr])
            k.dma(yb_d[blk * BLK + s_ * 128:blk * BLK + (s_ + 1) * 128, :], yr[:], R=[yr])
    k.barrier()
    ME_X.close()

    ME_C = ExitStack()

    def csb(*a, **kw):
        return k.sb(*a, scope=ME_C, **kw)
    ygat = [[csb("ygat", [128, D]) for _ in range(4)] for _ in range(2)]
    h3 = [csb("h3", [128, D]) for _ in range(2)]
    fg = csb("fg", [128, D]); k.dma(fg[:], fg_d[:, :], W=[fg])
    for b in range(NB):
        gtb[b][1] = csb("gtb2", [128, D])
        k.dma(gtb[b][1][:], modscr_d[b, 1, :].partition_broadcast(128), W=[gtb[b][1]])
    def combine_tile(tile):
        b, tt = tile // NT, tile % NT
        ht = h3[tile % 2]
        yg = ygat[tile % 2]
        for kk_ in range(4):
            c_ = tile * 4 + kk_
            k.dma(yg[kk_][:], yb_d[:, :], R=[dest_i], W=[yg[kk_]], q="pool", indirect=("gather", dest_i[:, c_:c_ + 1]))
        k.dma(ht[:], h1_d[b, tt * 128:(tt + 1) * 128, :], W=[ht])
        k.op("dve", lambda e: e.tensor_scalar(out=yg[0][:], in0=yg[0][:], scalar1=g4[:, tile, 0:1], scalar2=None, op0=ALU.mult),
             R=[g4], W=[yg[0]])
        for kk_ in range(1, 4):
            k.op("dve", lambda e: e.scalar_tensor_tensor(out=yg[0][:], in0=yg[kk_][:], scalar=g4[:, tile, kk_:kk_ + 1], in1=yg[0][:],
                                                         op0=ALU.mult, op1=ALU.add), R=[yg[kk_], g4], W=[yg[0]])
        if dbg == "moe":
            k.dma(out_d[b, tt * 128:(tt + 1) * 128, :], yg[0][:], R=[yg[0]])
            return
        k.op("dve", lambda e: e.tensor_tensor(out=yg[0][:], in0=yg[0][:], in1=gtb[b][1][:], op=ALU.mult), R=[gtb[b][1]], W=[yg[0]])
        k.op("dve", lambda e: e.tensor_tensor(out=ht[:], in0=ht[:], in1=yg[0][:], op=ALU.add), R=[yg[0]], W=[ht])
        st, i = rstd_of(ht[:], ht)
        k.op("dve", lambda e: e.scalar_tensor_tensor(out=ht[:], in0=ht[:], scalar=st[:, 3:4], in1=fg[:], op0=ALU.mult, op1=ALU.mult),
             R=[st, fg], W=[ht])
        k.dma(out_d[b, tt * 128:(tt + 1) * 128, :], ht[:], R=[ht])
    for t2 in range(0, NTC, 2):
        recs = []
        for tile in range(t2, min(t2 + 2, NTC)):
            k.begin_rec(); combine_tile(tile); recs.append(k.end_rec())
        k.play_merged(*recs)
    k.finish()
    return nc


def make_consts(NB, GM):
    idx = np.arange(128)
    same = (idx[:, None] // 64) == (idx[None, :] // 64)
    ident = np.eye(128, dtype=np.float32)
    m_su = (same & (idx[:, None] < idx[None, :])).astype(np.float32)
    m_sl = (same & (idx[:, None] > idx[None, :])).astype(np.float32)
    m_ui = (same & (idx[:, None] <= idx[None, :])).astype(np.float32)
    bones = same.astype(np.float32)
    ltri = (idx[:, None] < idx[None, :]).astype(np.float32)
    cm = np.ascontiguousarray(np.stack([ident, m_su, m_sl, m_ui, bones, ltri, np.ones((128, 128), np.float32)], axis=1))
    cm2 = np.ascontiguousarray(np.stack([np.concatenate([m, m], axis=1) for m in (m_su, m_sl, m_ui, ident)], axis=1))
    rm = np.ones((128, GM), np.float32)
    rm[:, ::64] = 0.0
    selb = np.zeros((NB, NB, 128), np.float32)
    for b in range(NB):
        selb[b, b, :] = 1.0
    return cm, rm, selb, np.ones((1, NB), np.float32), cm2


def fm(v, n):
    return np.ascontiguousarray(np.asarray(v, np.float32).reshape(n, 128).T)


def prep_shared(inp, NB, GM, E, T=2048):
    f = lambda a: np.asarray(a, np.float32)
    cm, rm, selb, ones, cm2 = make_consts(NB, GM)
    w1 = f(inp["exp_w1"])[0][:E]
    w1 = np.ascontiguousarray(np.concatenate([w1[:, :, 0::2], w1[:, :, 1::2]], axis=2))
    b1 = f(inp["exp_b1"])[0][:E]
    b1d = np.concatenate([b1[:, 0::2], b1[:, 1::2]], axis=1)
    b1_rows = np.ascontiguousarray(b1d.reshape(E, 16, 128).transpose(0, 2, 1).reshape(E * 128, 16))
    NBLK = (NB * T * 4) // 512 + 32
    blkstart = np.ascontiguousarray(np.broadcast_to((np.arange(NBLK, dtype=np.float32) * 512.0)[None, :], (128, NBLK)))
    iotaP = np.ascontiguousarray((np.arange(8, dtype=np.float32)[None, :] * 128.0 + np.arange(128, dtype=np.float32)[:, None]))
    vecs = np.stack([fm(f(inp[n])[0], 4) for n in
                     ("rwkv_w0", "rwkv_a0", "rwkv_k_k", "rwkv_k_a", "rwkv_r_k", "rwkv_ln_w", "rwkv_ln_b")], axis=1)
    sh = {
        "ada_w": np.ascontiguousarray(f(inp["ada_w"])[0]),
        "ada_b_fm": fm(f(inp["ada_b"])[0], 48),
        "ada_b_row": np.ascontiguousarray(f(inp["ada_b"])[0][None, :]),
        "g12_fm": np.ascontiguousarray(np.stack([fm(f(inp["norm1_g"])[0], 8), fm(f(inp["norm2_g"])[0], 8)], axis=1)),
        "w_in": np.ascontiguousarray(f(inp["w_in"])[0]),
        "conv_w_fm": np.ascontiguousarray(f(inp["conv_w"])[0].reshape(3, 4, 128).transpose(2, 1, 0)),
        "conv_gn_fm": fm(f(inp["conv_gn"])[0], 4),
        "mu_fm": fm(f(inp["rwkv_mu"])[0], 14),
        "vecs_fm": np.ascontiguousarray(vecs),
        "lora_up": np.ascontiguousarray(np.concatenate([f(inp["rwkv_w_up"])[0], f(inp["rwkv_a_up"])[0]], axis=0)),
        "g_up": np.ascontiguousarray(f(inp["rwkv_g_up"])[0]),
        "w_out": np.ascontiguousarray(f(inp["w_out"])[0]),
        "router_w": np.ascontiguousarray(f(inp["router_w"])[0][:, :32]),
        "router_b_bc": np.ascontiguousarray(np.broadcast_to(f(inp["router_b"])[0][None, :32], (128, 32))),
        "w1": w1.reshape(E * D, 2 * D),
        "b1_rows": b1_rows,
        "blkstart": blkstart, "iotaP": iotaP,
        "g2_row": np.ascontiguousarray(np.broadcast_to(f(inp["norm2_g"])[0][None, :], (NB, D))),
        "w2": np.ascontiguousarray(f(inp["exp_w2"])[0][:E]).reshape(E * D, D),
        "b2": np.ascontiguousarray(f(inp["exp_b2"])[0][:E]),
        "final_g_bc": np.ascontiguousarray(np.broadcast_to(f(inp["final_g"])[None, :], (128, D))),
        "cmats": cm, "cmats2": cm2, "resetmask": rm, "selb": selb, "ones_row": ones,
    }
    return sh


def core_inputs(sh, x, c, b0, NB):
    m = dict(sh)
    m["x"] = np.ascontiguousarray(x[b0:b0 + NB])
    cc = np.asarray(c[b0:b0 + NB], np.float32)
    m["cT"] = np.ascontiguousarray(cc.reshape(NB, 8, 128).transpose(2, 1, 0))
    return m


def kernel(**inputs):
    x = np.asarray(inputs["x"], np.float32)
    c = np.asarray(inputs["c"], np.float32)
    B, T, _ = x.shape
    n = 8
    NB = B // n
    E = 32
    GM = 256
    sh = prep_shared(inputs, NB, GM, E, T)
    nc = build(T, NB, E, GM)
    in_maps = [core_inputs(sh, x, c, i * NB, NB) for i in range(n)]
    res = run_bass_kernel_spmd(nc, in_maps, core_ids=list(range(n)))
    return np.concatenate([r["out"] for r in res.results], axis=0).astype(np.float32)
```

```python
import numpy as np
import ml_dtypes
from contextlib import ExitStack
import concourse.bass as bass
import concourse.mybir as mybir
from concourse.bass_utils import run_bass_kernel_spmd

F32 = mybir.dt.float32
BF = mybir.dt.bfloat16
AF = mybir.ActivationFunctionType
ALU = mybir.AluOpType

D = 1024
NORM_EPS = 1e-5
GN_EPS = 64e-5
NDS = 12


class Buf:
    __slots__ = ("lw", "rd")

    def __init__(self):
        self.lw = None
        self.rd = {}


class TT:
    def __init__(self, t, nsub=0):
        self.t = t
        self.b = Buf()
        self.subs = [Buf() for _ in range(nsub)]

    def __getitem__(self, idx):
        return self.t[idx]


class _Rec:
    def __init__(self):
        self.call = None

    def __getattr__(self, name):
        def f(*a, **kw):
            self.call = (name, a, kw)
            return self
        return f


class KB:
    def __init__(self, nc):
        self.nc = nc
        self.eng = {"pe": nc.tensor, "act": nc.scalar, "dve": nc.vector, "pool": nc.gpsimd, "sp": nc.sync}
        self.sem = {n: nc.alloc_semaphore("s_" + n) for n in ["pe", "act", "dve", "pool"]}
        self.cnt = {n: 0 for n in ["pe", "act", "dve", "pool"]}
        self.waited = {}
        self.dsems = {q: [nc.alloc_semaphore("d%s%d" % (q, i)) for i in range(NDS)] for q in ("sp", "pool")}
        self.dtot = {q: [0] * NDS for q in ("sp", "pool")}
        self.dnext = {"sp": 0, "pool": 0}
        self.nid = 0
        self.bregs = {}
        self.rec = None
        self.last_tile = 2

    def _wait(self, eng, tok):
        if tok is None:
            return
        key, sem, val = tok
        if eng == "pe" and key == "pe":
            return
        if self.waited.get((eng, key), 0) >= val:
            return
        self.waited[(eng, key)] = val
        self.eng[eng].wait_ge(sem, val)

    def _deps(self, eng, R, W):
        for b in R:
            self._wait(eng, b.lw)
        for b in W:
            self._wait(eng, b.lw)
            for t in b.rd.values():
                self._wait(eng, t)

    def _post(self, tok, R, W):
        for b in R:
            b.rd[tok[0]] = tok
        for b in W:
            b.lw = tok
            b.rd = {}

    def op(self, eng, fn, R=(), W=()):
        R = [x.b if isinstance(x, TT) else x for x in R]
        W = [x.b if isinstance(x, TT) else x for x in W]
        if self.rec is not None:
            r = _Rec()
            fn(r)
            self.rec.append(("op", eng, r.call, R, W))
            return None
        if eng == "pe":
            tid = self._pe_tile(fn)
            if tid != self.last_tile and tid in (0, 1) and self.last_tile in (0, 1) and self.cnt["pe"] > 0:
                self.eng["pe"].wait_ge(self.sem["pe"], self.cnt["pe"])
            self.last_tile = tid
        self._deps(eng, R, W)
        ins = fn(self.eng[eng])
        self.cnt[eng] += 1
        ins.then_inc(self.sem[eng], 1)
        tok = (eng, self.sem[eng], self.cnt[eng])
        self._post(tok, R, W)
        return tok

    def dma(self, out, in_, R=(), W=(), q="sp", indirect=None):
        R = [x.b if isinstance(x, TT) else x for x in R]
        W = [x.b if isinstance(x, TT) else x for x in W]
        if self.rec is not None:
            self.rec.append(("dma", out, in_, R, W, q, indirect))
            return None
        i = self.dnext[q]
        self.dnext[q] = (i + 1) % NDS
        sem = self.dsems[q][i]
        key = "d%s%d" % (q, i)
        if self.dtot[q][i] > 0:
            self._wait(q, (key, sem, self.dtot[q][i]))
        self._deps(q, R, W)
        if indirect is None:
            ins = self.eng[q].dma_start(out=out, in_=in_)
        else:
            kind, idx = indirect[0], indirect[1]
            off = bass.IndirectOffsetOnAxis(ap=idx, axis=0)
            extra = {}
            if len(indirect) > 2:
                if indirect[2] not in self.bregs:
                    self.bregs[indirect[2]] = self.eng[q].to_reg(indirect[2])
                extra = dict(bounds_check=self.bregs[indirect[2]], oob_is_err=False)
            ins = self.eng[q].indirect_dma_start(out=out, out_offset=(off if kind == "scatter" else None), in_=in_,
                                                 in_offset=(off if kind == "gather" else None), **extra)
        self.dtot[q][i] += 16
        ins.then_inc(sem, 16)
        tok = (key, sem, self.dtot[q][i])
        self._post(tok, R, W)
        return tok

    def _pe_tile(self, fn):
        r = _Rec()
        fn(r)
        name, a, kw = r.call
        ap = kw.get("in_") if name == "transpose" else (kw.get("lhsT") if "lhsT" in kw else (a[1] if len(a) > 1 else None))
        if ap is None:
            return 2
        st, sz = ap.start_partition(), ap.partition_size()
        if sz > 64:
            return 2
        return 0 if st < 64 else 1

    def begin_rec(self):
        self.rec = []

    def end_rec(self):
        r, self.rec = self.rec, None
        return r

    def _play1(self, it):
        if it[0] == "op":
            _, eng, (name, a, kw), R, W = it
            self.op(eng, lambda e: getattr(e, name)(*a, **kw), R, W)
        else:
            _, out, in_, R, W, q, indirect = it
            self.dma(out, in_, R, W, q, indirect)

    def play_merged(self, X, Y=()):
        nx, ny = len(X), len(Y)
        ix = iy = 0
        def is_pe(it):
            return it[0] == "op" and it[1] == "pe"
        while ix < nx or iy < ny:
            if iy >= ny or (ix < nx and ix * ny <= iy * nx):
                self._play1(X[ix]); ix += 1
                while ix < nx and is_pe(X[ix - 1]) and is_pe(X[ix]):
                    self._play1(X[ix]); ix += 1
            else:
                self._play1(Y[iy]); iy += 1
                while iy < ny and is_pe(Y[iy - 1]) and is_pe(Y[iy]):
                    self._play1(Y[iy]); iy += 1

    def sb(self, name, shape, dt=F32, nsub=0, scope=None):
        self.nid += 1
        nm = "%s_%d" % (name, self.nid)
        if scope is None:
            return TT(self.nc.alloc_sbuf_tensor(nm, list(shape), dt), nsub)
        return TT(scope.enter_context(self.nc.sbuf_tensor(nm, list(shape), dt)), nsub)

    def barrier(self):
        names = ["pe", "act", "dve", "pool", "sp"]
        for e in names:
            for n in ["pe", "act", "dve", "pool"]:
                if n != e and self.cnt[n] > 0:
                    self._wait(e, (n, self.sem[n], self.cnt[n]))
            for q in ("sp", "pool"):
                for i in range(NDS):
                    if self.dtot[q][i] > 0:
                        self._wait(e, ("d%s%d" % (q, i), self.dsems[q][i], self.dtot[q][i]))

    def finish(self):
        for q in ("sp", "pool"):
            for i in range(NDS):
                if self.dtot[q][i] > 0:
                    self.eng["sp"].wait_ge(self.dsems[q][i], self.dtot[q][i])
        for n in ["pe", "act", "dve", "pool"]:
            if self.cnt[n] > 0:
                self.eng["sp"].wait_ge(self.sem[n], self.cnt[n])


KSTOP = 0
GVAR = 0
PMODE = 1


def build(T, NB, E, GM=256, dbg=None):
    nc = bass.Bass("TRN2", target_bir_lowering=False)
    k = KB(nc)
    NT = T // 128
    NG = T // GM
    TPG = GM // 128
    EG = min(512, T)

    def din(name, shape):
        return nc.dram_tensor(name, list(shape), F32, kind="ExternalInput").ap()

    x_d = din("x", [NB, T, D])
    cT_d = din("cT", [128, 8, NB])
    adaw_d = din("ada_w", [D, 6 * D])
    adab_fm_d = din("ada_b_fm", [128, 48])
    adab_row_d = din("ada_b_row", [1, 6 * D])
    g12_d = din("g12_fm", [128, 2, 8])
    win_d = din("w_in", [D, 3328])
    convw_d = din("conv_w_fm", [128, 4, 3])
    convgn_d = din("conv_gn_fm", [128, 4])
    mu_d = din("mu_fm", [128, 14])
    vecs_d = din("vecs_fm", [128, 7, 4])
    lora_d = din("lora_up", [128, 512])
    gup_d = din("g_up", [128, 512])
    wout_d = din("w_out", [D, D])
    rw_d = din("router_w", [D, 32])
    rb_d = din("router_b_bc", [128, 32])
    w1_d = din("w1", [E * D, 2 * D])
    b1_d = din("b1_rows", [E * 128, 16])
    w2_d = din("w2", [E * D, D])
    b2_d = din("b2", [E, D])
    NTC = NB * (T // 128)
    BLK = 512
    NBLK = (NTC * 128 * 4) // BLK + 32
    bs_d = din("blkstart", [128, NBLK])
    iop_d = din("iotaP", [128, 8])
    xs_d = nc.dram_tensor("xs_scr", [NBLK * BLK, D], BF, kind="Internal").ap()
    zeros_d = nc.dram_tensor("zeros_blk", [BLK, D], BF, kind="ExternalInput").ap()
    yb_d = nc.dram_tensor("yb_scr", [NBLK * BLK, D], F32, kind="Internal").ap()
    fg_d = din("final_g_bc", [128, D])
    cm_d = din("cmats", [128, 7, 128])
    rm_d = din("resetmask", [128, GM])
    cm2_d = din("cmats2", [128, 4, 256])
    sel_d = din("selb", [NB, NB, 128])
    ones_d = din("ones_row", [1, NB])
    g2row_d = din("g2_row", [NB, D])
    modscr_d = nc.dram_tensor("modscr", [NB, 4, D], F32, kind="Internal").ap()
    out_d = nc.dram_tensor("out", [NB, T, D], F32, kind="ExternalOutput").ap()
    h1_d = nc.dram_tensor("h1_scratch", [NB, T, D], F32, kind=("ExternalOutput" if dbg else "Internal")).ap()

    banks = [TT(nc.alloc_psum_tensor("psb%d" % i, [128, 512], F32)) for i in range(6)]
    PSX = ExitStack()
    banks += [TT(PSX.enter_context(nc.psum_tensor("psbx%d" % i, [128, 512], F32))) for i in range(2)]
    bbanks = []
    bstate = {"n": 0, "b": 0, "pool": None, "nx": 0, "ny": 0}

    reserved = set()

    def psum():
        if bstate["pool"] == "X":
            bstate["nx"] += 1
            return banks[bstate["nx"] % 6]
        if bstate["pool"] == "Y":
            bstate["ny"] += 1
            return banks[6 + bstate["ny"] % 2]
        while True:
            i = bstate["n"] % len(banks)
            bstate["n"] += 1
            if i not in reserved:
                return banks[i]

    def psum_bf():
        b = bbanks[bstate["b"] % 2]
        bstate["b"] += 1
        return b

    def mm(ps, out_ap, pairs, R):
        n = len(pairs)
        tok = None
        for i, (l, r) in enumerate(pairs):
            tok = k.op("pe", lambda e, l=l, r=r, i=i: e.matmul(out_ap, l, r, start=(i == 0), stop=(i == n - 1)),
                       R=R, W=[ps])
        return tok

    P0 = ExitStack()
    MX = ExitStack()
    ME = ExitStack()
    cm = k.sb("cm", [128, 7, 128])
    k.dma(cm[:], cm_d[:, :, :], W=[cm])
    ident, m_su, m_sl, m_ui, bones = (cm[:, i, :] for i in range(5))
    cmb = k.sb("cmb", [128, 128], BF)
    k.op("dve", lambda e: e.tensor_copy(out=cmb[:], in_=ident), R=[cm], W=[cmb])
    smallw = k.sb("smallw", [128, 48 + 16 + 12 + 4 + 14 + 28])
    o = 0
    adab_fm = smallw[:, o:o + 48]; k.dma(adab_fm, adab_fm_d[:, :], W=[smallw]); o += 48
    g12 = smallw[:, o:o + 16]; k.dma(g12, g12_d.rearrange("p a b -> p (a b)"), W=[smallw]); o += 16
    cw = smallw[:, o:o + 12]; k.dma(cw, convw_d.rearrange("p a b -> p (a b)"), W=[smallw]); o += 12
    cgn = smallw[:, o:o + 4]; k.dma(cgn, convgn_d[:, :], W=[smallw]); o += 4
    mu = smallw[:, o:o + 14]; k.dma(mu, mu_d[:, :], W=[smallw]); o += 14
    vecs = smallw[:, o:o + 28]; k.dma(vecs, vecs_d.rearrange("p a b -> p (a b)"), W=[smallw]); o += 28

    def vec(v, i):
        return vecs[:, v * 4 + i:v * 4 + i + 1]

    rw = k.sb("rw", [128, 8, 32]); k.dma(rw[:], rw_d.rearrange("(c p) n -> p c n", p=128), W=[rw])
    rb = k.sb("rb", [128, 32]); k.dma(rb[:], rb_d[:, :], W=[rb])
    cT = k.sb("cT", [128, 8 * NB]); k.dma(cT[:], cT_d.rearrange("p a b -> p (a b)"), W=[cT])

    small = k.sb("small", [128, 64])
    gtb = [[None, None] for _ in range(NB)]
    GS = [k.sb("GS", [128, 32]) for _ in range(NB)]
    nstat = [k.sb("nstat", [128, 4]) for _ in range(2)]
    xn = [k.sb("xn", [128, D])]
    junk = k.sb("junk", [128, D], BF)
    cact = k.sb("cact", [128, 8 * NB])
    csg = k.sb("csg", [128, 8 * NB])
    modfm = [k.sb("modfm", [128, 48]) for _ in range(NB)]
    winb = k.sb("winb", [128, 8, 3328], BF, scope=MX)
    woutb = k.sb("woutb", [128, 8, D], BF, scope=MX)
    rmask = k.sb("rmask", [128, GM], scope=MX)
    k.dma(rmask[:], rm_d[:, :], W=[rmask])
    lora = k.sb("lora", [128, 512], scope=MX); k.dma(lora[:], lora_d[:, :], W=[lora])
    gup = k.sb("gup", [128, 512], scope=MX); k.dma(gup[:], gup_d[:, :], W=[gup])
    gt1t = k.sb("gtb1", [128, D], scope=MX)
    selb = k.sb("selb", [NB, NB, 128], scope=P0)
    k.dma(selb[:], sel_d[:, :, :], W=[selb])
    stage = [k.sb("stage", [128, 2048], scope=P0) for _ in range(2)]
    sidx = {"n": 0}

    def next_stage():
        s = stage[sidx["n"] % 2]
        sidx["n"] += 1
        return s
    awbs = [k.sb("awb", [128, 8, 512], scope=P0) for _ in range(2)]
    modrow = k.sb("modrow", [NB, 4, D], scope=P0)
    adabr = k.sb("adabr", [NB, 4, D], scope=P0)
    g2row = k.sb("g2row", [NB, D], scope=P0)
    k.dma(g2row[:], g2row_d[:, :], W=[g2row])

    k.op("act", lambda e: e.activation(out=csg[:], in_=cT[:], func=AF.Sigmoid), R=[cT], W=[csg])
    k.op("dve", lambda e: e.tensor_tensor(out=cact[:], in0=cT[:], in1=csg[:], op=ALU.mult), R=[cT, csg], W=[cact])
    ps_fm = psum()
    reserved.add(banks.index(ps_fm))
    for blk in range(12):
        awb = awbs[blk % 2]
        for h in range(4):
            st = next_stage()
            k.dma(st[:, 0:1024].rearrange("p (c n) -> p c n", c=2),
                  adaw_d[h * 256:(h + 1) * 256, blk * 512:(blk + 1) * 512].rearrange("(c p) n -> p c n", p=128),
                  W=[st])
            k.op("act", lambda e, st=st, h=h, awb=awb: e.activation(
                out=awb[:, 2 * h:2 * h + 2, :], in_=st[:, 0:1024].rearrange("p (c n) -> p c n", c=2), func=AF.Copy),
                R=[st], W=[awb])
        for jj in range(4):
            j = blk * 4 + jj
            mm(ps_fm, ps_fm[:, j * NB:(j + 1) * NB],
               [(awb[:, kc, jj * 128:(jj + 1) * 128], cact[:, kc * NB:(kc + 1) * NB]) for kc in range(8)],
               R=[awb, cact])
        if blk in (4, 5, 6, 7, 8, 9, 10, 11):
            ps_r = psum()
            pairs = [(cact[:, kc * NB:(kc + 1) * NB], awb[:, kc, :]) for kc in range(8)]
            mm(ps_r, ps_r[0:NB, :], pairs, R=[awb, cact])
            which = {4: 0, 5: 0, 10: 1, 11: 1, 6: 2, 7: 2, 8: 3, 9: 3}[blk]
            half = blk % 2
            k.op("act", lambda e, ps_r=ps_r, which=which, half=half: e.activation(
                out=modrow[:, which, half * 512:(half + 1) * 512], in_=ps_r[0:NB, :], func=AF.Copy),
                R=[ps_r], W=[modrow])
    for b in range(NB):
        k.op("dve", lambda e, b=b: e.tensor_tensor(out=modfm[b][:], in0=ps_fm[:, b:48 * NB:NB], in1=adab_fm, op=ALU.add),
             R=[ps_fm, smallw], W=[modfm[b]])
    reserved.clear()
    for b in range(NB):
        for which, c0 in ((0, 2 * D), (1, 5 * D), (2, 3 * D), (3, 4 * D)):
            k.dma(adabr[b:b + 1, which, :], adab_row_d[0:1, c0:c0 + D], W=[adabr])
    k.op("dve", lambda e: e.tensor_tensor(out=modrow[:], in0=modrow[:], in1=adabr[:], op=ALU.add),
         R=[adabr], W=[modrow])
    k.op("dve", lambda e: e.scalar_tensor_tensor(out=modrow[:, 3, :], in0=modrow[:, 3, :], scalar=1.0, in1=g2row[:],
                                                 op0=ALU.add, op1=ALU.mult), R=[g2row], W=[modrow])
    k.dma(modscr_d[:, :, :], modrow[:, 0:4, :], R=[modrow])
    for b in range(NB):
        for n, (sc0, sh0) in enumerate(((8, 0), (32, 24))):
            k.op("dve", lambda e, b=b, n=n, sc0=sc0: e.scalar_tensor_tensor(
                out=GS[b][:, n * 16:n * 16 + 8], in0=modfm[b][:, sc0:sc0 + 8], scalar=1.0,
                in1=g12[:, n * 8:(n + 1) * 8], op0=ALU.add, op1=ALU.mult), R=[modfm[b], smallw], W=[GS[b]])
            k.op("dve", lambda e, b=b, n=n, sh0=sh0: e.tensor_copy(
                out=GS[b][:, n * 16 + 8:n * 16 + 16], in_=modfm[b][:, sh0:sh0 + 8]), R=[modfm[b]], W=[GS[b]])

    nctr = {"n": 0}

    def rstd_of(src_ap, srcbuf):
        i = nctr["n"] % 2
        nctr["n"] += 1
        st = nstat[i]
        k.op("act", lambda e: e.activation(out=junk[:], in_=src_ap, func=AF.Square, accum_out=st[:, 0:1]),
             R=[srcbuf], W=[junk, st])
        k.op("dve", lambda e: e.tensor_scalar(out=st[:, 1:2], in0=st[:, 0:1], scalar1=1.0 / D, scalar2=NORM_EPS,
                                              op0=ALU.mult, op1=ALU.add), R=[st], W=[st])
        k.op("act", lambda e: e.activation(out=st[:, 2:3], in_=st[:, 1:2], func=AF.Sqrt), R=[st], W=[st])
        k.op("dve", lambda e: e.reciprocal(out=st[:, 3:4], in_=st[:, 2:3]), R=[st], W=[st])
        return st, i

    def norm_T(src_ap, srcbuf, gs, goff, evac, xi=0):
        st, i = rstd_of(src_ap, srcbuf)
        i = xi
        k.op("dve", lambda e: e.tensor_scalar(out=xn[i][:], in0=src_ap, scalar1=st[:, 3:4], scalar2=None, op0=ALU.mult),
             R=[srcbuf, st], W=[xn[i]])
        for hb in range(2):
            ps = psum()
            for c in range(4):
                kc = hb * 4 + c
                k.op("pe", lambda e, ps=ps, c=c, kc=kc: e.transpose(out=ps[:, c * 128:(c + 1) * 128],
                                                                     in_=xn[i][:, kc * 128:(kc + 1) * 128], identity=ident),
                     R=[xn[i], cm], W=[ps])
            for c in range(4):
                kc = hb * 4 + c
                evac(kc, ps[:, c * 128:(c + 1) * 128], gs[:, goff + kc:goff + kc + 1], gs[:, goff + 8 + kc:goff + 9 + kc], ps)

    for kc in range(8):
        for half in range(2):
            st = next_stage()
            k.dma(st[:, 0:1664], win_d[kc * 128:(kc + 1) * 128, half * 1664:(half + 1) * 1664], W=[st])
            k.op("act" if half else "dve", lambda e, st=st, kc=kc, half=half: (
                e.activation(out=winb[:, kc, half * 1664:(half + 1) * 1664], in_=st[:, 0:1664], func=AF.Copy) if half else
                e.tensor_copy(out=winb[:, kc, half * 1664:(half + 1) * 1664], in_=st[:, 0:1664])), R=[st], W=[winb])
    for kc in range(8):
        st = next_stage()
        k.dma(st[:, 0:D], wout_d[kc * 128:(kc + 1) * 128, :], W=[st])
        k.op("act", lambda e, st=st, kc=kc: e.activation(out=woutb[:, kc, :], in_=st[:, 0:D], func=AF.Copy), R=[st], W=[woutb])

    k.barrier()
    P0.close()
    def ksb(*a, **kw):
        return k.sb(*a, scope=MX, **kw)
    xgs = [ksb("xg", [128, TPG, D], nsub=TPG) for _ in range(2)]
    uT = ksb("uT", [128, 8, GM], BF)
    mixT = ksb("mixT", [128, 8, GM], BF, nsub=8)
    pbuf = [ksb("pbuf", [128, GM + 1]) for _ in range(14)]
    pl12, pl13 = ksb("pl12", [128, GM]), ksb("pl13", [128, GM])
    rkv = [[ksb("rkv", [128, GM]) for _ in range(3)] for _ in range(2)]
    ggs = [ksb("gg", [128, GM]) for _ in range(2)]
    bonuss = [ksb("bonus", [128, GM]) for _ in range(2)]
    PLs = [ksb("PL", [128, 2 * TPG]) for _ in range(2)]
    ucv = [ksb("ucv", [128, GM + 2]) for _ in range(4)]
    NTMP = 26
    tmps = [ksb("tmp", [128, GM]) for _ in range(NTMP)]
    Rtbs = [ksb("Rtb", [128, GM], BF) for _ in range(2)]
    TTts = [[ksb("TTt", [128, 512], BF) for _ in range(TPG)] for _ in range(2)]
    padss = [[[ksb("pads", [128, 3, 128], BF) for _ in range(TPG)] for _ in range(2)] for _ in range(2)]
    UTpad = [ksb("UTpad", [128, 128], BF) for _ in range(2)]
    Xs = [[ksb("Xs", [128, 2, 128], BF) for _ in range(2)] for _ in range(TPG)]
    XTs = [[ksb("XTs", [128, 2, 128], BF) for _ in range(2)] for _ in range(TPG)]
    Tms = [[ksb("Tms", [128, 2, 128], BF) for _ in range(2)] for _ in range(TPG)]
    MakT = [ksb("MakT", [128, 2, 128], BF) for _ in range(TPG)]
    MT = [ksb("MT", [128, 2, 128], BF) for _ in range(TPG)]
    Mbr = [ksb("Mbr", [128, 2, 128], BF) for _ in range(TPG)]
    Mkr = [ksb("Mkr", [128, 2, 128], BF) for _ in range(TPG)]
    Atbs = [ksb("Atb", [128, GM], BF) for _ in range(2)]
    Btbs = [ksb("Btb", [128, GM], BF) for _ in range(2)]
    Ktbs = [ksb("Ktb", [128, GM], BF) for _ in range(2)]
    Sdec = ksb("Sdec", [128, 128])
    cm2 = ksb("cm2", [128, 4, 256])
    k.dma(cm2[:], cm2_d[:, :, :], W=[cm2])
    mk2_su, mk2_sl, mk2_ui, mk2_id = (cm2[:, i_, :] for i_ in range(4))
    Ah = [ksb("Ah", [128, 128], BF) for _ in range(TPG)]
    WT2 = [ksb("WT2", [128, 128]) for _ in range(TPG)]
    STf = [ksb("STf", [128, 128]) for _ in range(4)]
    STb = [ksb("STb", [128, 128], BF) for _ in range(4)]

    for hh in range(2):
        for tb in range(TPG):
            for par_ in range(2):
                k.op("dve", lambda e: e.memset(padss[par_][hh][tb][:], 0.0), W=[padss[par_][hh][tb]])
        k.op("dve", lambda e, hh=hh: e.memset(UTpad[hh][:], 0.0), W=[UTpad[hh]])

    def proj(j):
        ps = psum()
        mm(ps, ps[:, 0:GM], [(winb[:, kc, j * 128:(j + 1) * 128], uT[:, kc, :]) for kc in range(8)], R=[winb, uT])
        return ps

    def bsum(src, srcbuf):
        ps = psum()
        mm(ps, ps[:, 0:GM], [(bones, src)], R=[cm, srcbuf])
        return ps

    def rsqrt_ps(ps, scale, eps, dst):
        k.op("dve", lambda e: e.tensor_scalar(out=dst[:], in0=ps[:, 0:GM], scalar1=scale, scalar2=eps, op0=ALU.mult, op1=ALU.add),
             R=[ps], W=[dst])
        k.op("act", lambda e: e.activation(out=dst[:], in_=dst[:], func=AF.Sqrt), R=[dst], W=[dst])
        k.op("dve", lambda e: e.reciprocal(out=dst[:], in_=dst[:]), R=[dst], W=[dst])

    def stage_U(b_, g_):
        xg_ = xgs[(b_ * NG + g_) % 2]
        for tt in range(TPG):
            k.dma(xg_[:, tt, :], x_d[b_, g_ * GM + tt * 128:g_ * GM + (tt + 1) * 128, :], W=[xg_.subs[tt]])

            def ev(kc, psap, sc, bi, ps, tt=tt):
                k.op("act", lambda e: e.activation(out=uT[:, kc, tt * 128:(tt + 1) * 128], in_=psap, func=AF.Identity,
                                                   scale=sc, bias=bi), R=[ps, GS[b_]], W=[uT])
            norm_T(xg_[:, tt, :], xg_.subs[tt], GS[b_], 0, ev)

    h1ctr = {"n": 0}
    for b in range(NB):
        k.dma(gt1t[:], modscr_d[b, 0, :].partition_broadcast(128), W=[gt1t])
        for q in range(14):
            k.op("dve", lambda e, q=q: e.memset(pbuf[q][:, 0:1], 0.0), W=[pbuf[q]])
        for ci in range(4):
            k.op("dve", lambda e, ci=ci: e.memset(ucv[ci][:, 0:2], 0.0), W=[ucv[ci]])
            k.op("dve", lambda e, ci=ci: e.memset(STf[ci][:], 0.0), W=[STf[ci]])
            k.op("dve", lambda e, ci=ci: e.memset(STb[ci][:], 0.0), W=[STb[ci]])
        for g in range(NG):
            t0 = g * GM
            xg = xgs[(b * NG + g) % 2]
            if b == 0 and g == 0:
                stage_U(0, 0)
            for ci in range(4):
                tB, tC, t1, yb, ysq, rr = tmps[6 * (ci % 2) + 3:6 * (ci % 2) + 9]
                ps = proj(ci)
                k.op("act", lambda e, ps=ps: e.activation(out=tB[:], in_=ps[:, 0:GM], func=AF.Copy), R=[ps], W=[tB])
                ps = proj(4 + ci)
                k.op("act", lambda e, ps=ps: e.activation(out=tC[:], in_=ps[:, 0:GM], func=AF.Copy), R=[ps], W=[tC])
                ps = proj(8 + ci)
                u = ucv[ci]
                k.op("dve", lambda e, ps=ps: e.tensor_tensor(out=u[:, 2:2 + GM], in0=tC[:], in1=ps[:, 0:GM], op=ALU.mult),
                     R=[tC, ps], W=[u])
                k.op("dve", lambda e: e.tensor_scalar(out=t1[:], in0=u[:, 0:GM], scalar1=cw[:, ci * 3:ci * 3 + 1], scalar2=None,
                                                      op0=ALU.mult), R=[u, smallw], W=[t1])
                for kk_ in (1, 2):
                    k.op("dve", lambda e, kk_=kk_: e.scalar_tensor_tensor(out=t1[:], in0=u[:, kk_:kk_ + GM],
                                                                           scalar=cw[:, ci * 3 + kk_:ci * 3 + kk_ + 1], in1=t1[:],
                                                                           op0=ALU.mult, op1=ALU.add), R=[u, smallw], W=[t1])
                k.op("dve", lambda e: e.tensor_tensor(out=yb[:], in0=tB[:], in1=t1[:], op=ALU.mult), R=[tB, t1], W=[yb])
                k.op("act", lambda e: e.activation(out=ysq[:], in_=yb[:], func=AF.Square), R=[yb], W=[ysq])
                psn = bsum(ysq[:], ysq)
                rsqrt_ps(psn, 1.0 / 64, NORM_EPS, rr)
                k.op("dve", lambda e: e.scalar_tensor_tensor(out=mixT[:, ci, :], in0=yb[:], scalar=cgn[:, ci:ci + 1], in1=rr[:],
                                                             op0=ALU.mult, op1=ALU.mult), R=[yb, rr, smallw], W=[mixT.subs[ci]])
                k.op("dve", lambda e: e.tensor_copy(out=u[:, 0:2], in_=u[:, GM:GM + 2]), R=[], W=[u])
            def lerp_tile(q, dst):
                ps = proj(12 + q)
                pb = pbuf[q]
                d_ = tmps[24 + (q % 2)]
                k.op("act", lambda e: e.activation(out=pb[:, 1:GM + 1], in_=ps[:, 0:GM], func=AF.Copy), R=[ps], W=[pb])
                k.op("dve", lambda e: e.tensor_tensor(out=d_[:], in0=pb[:, 0:GM], in1=pb[:, 1:GM + 1], op=ALU.subtract),
                     R=[pb], W=[d_])
                k.op("dve", lambda e: e.scalar_tensor_tensor(out=dst[:], in0=d_[:], scalar=mu[:, q:q + 1], in1=pb[:, 1:GM + 1],
                                                             op0=ALU.mult, op1=ALU.add), R=[d_, pb, smallw], W=[dst])
                k.op("dve", lambda e: e.tensor_copy(out=pb[:, 0:1], in_=pb[:, GM:GM + 1]), R=[], W=[pb])
            lerp_tile(12, pl12)
            lerp_tile(13, pl13)
            th, sgm = tmps[1], tmps[2]
            k.op("act", lambda e: e.activation(out=th[0:64, :], in_=pl12[0:64, :], func=AF.Tanh), R=[pl12], W=[th])
            k.op("act", lambda e: e.activation(out=sgm[:], in_=pl13[:], func=AF.Sigmoid), R=[pl13], W=[sgm])

            def tile_A1(i, par):
                (ld, av, kk, kksq, rinv, kkn, tq, kmod, prod, lcum, Pinc, Pexc, Pinv, At, Bt, Kt, Rt) = tmps[3:20]
                gg, bonus, PL = ggs[par], bonuss[par], PLs[par]
                Atb, Btb, Ktb, Rtb, TTt, pads = Atbs[par], Btbs[par], Ktbs[par], Rtbs[par], TTts[par], padss[par]
                rl, kl, vl = rkv[par]
                lerp_tile(i, rl)
                lerp_tile(4 + i, kl)
                lerp_tile(8 + i, vl)
                cs = slice(i * 128, (i + 1) * 128)
                ps = psum()
                mm(ps, ps[:, 0:GM], [(lora[0:64, cs], th[0:64, :])], R=[lora, th])
                k.op("act", lambda e, ps=ps: e.activation(out=ld[:], in_=ps[:, 0:GM], func=AF.Sigmoid, bias=vec(0, i), scale=1.0),
                     R=[ps, smallw], W=[ld])
                k.op("dve", lambda e: e.tensor_scalar(out=ld[:], in0=ld[:], scalar1=-0.6065306597126334, scalar2=None, op0=ALU.mult),
                     R=[], W=[ld])
                ps = psum()
                mm(ps, ps[:, 0:GM], [(lora[64:128, cs], pl12[64:128, :])], R=[lora, pl12])
                k.op("act", lambda e, ps=ps: e.activation(out=av[:], in_=ps[:, 0:GM], func=AF.Sigmoid, bias=vec(1, i), scale=1.0),
                     R=[ps, smallw], W=[av])
                ps = psum()
                mm(ps, ps[:, 0:GM], [(gup[:, cs], sgm[:])], R=[gup, sgm])
                k.op("act", lambda e, ps=ps: e.activation(out=gg[:], in_=ps[:, 0:GM], func=AF.Copy), R=[ps], W=[gg])
                k.op("dve", lambda e: e.tensor_scalar(out=kk[:], in0=kl[:], scalar1=vec(2, i), scalar2=None, op0=ALU.mult),
                     R=[kl, smallw], W=[kk])
                k.op("act", lambda e: e.activation(out=kksq[:], in_=kk[:], func=AF.Square), R=[kk], W=[kksq])
                psn = bsum(kksq[:], kksq)
                k.op("act", lambda e, psn=psn: e.activation(out=rinv[:], in_=psn[:, 0:GM], func=AF.Sqrt), R=[psn], W=[rinv])
                k.op("dve", lambda e: e.tensor_scalar(out=rinv[:], in0=rinv[:], scalar1=1e-12, scalar2=None, op0=ALU.max), R=[], W=[rinv])
                k.op("dve", lambda e: e.reciprocal(out=rinv[:], in_=rinv[:]), R=[], W=[rinv])
                k.op("dve", lambda e: e.tensor_tensor(out=kkn[:], in0=kk[:], in1=rinv[:], op=ALU.mult), R=[kk, rinv], W=[kkn])
                k.op("dve", lambda e: e.tensor_scalar(out=tq[:], in0=av[:], scalar1=-1.0, scalar2=vec(3, i), op0=ALU.add, op1=ALU.mult),
                     R=[av, smallw], W=[tq])
                k.op("dve", lambda e: e.scalar_tensor_tensor(out=kmod[:], in0=tq[:], scalar=1.0, in1=kl[:], op0=ALU.add, op1=ALU.mult),
                     R=[tq, kl], W=[kmod])
                k.op("dve", lambda e: e.scalar_tensor_tensor(out=prod[:], in0=rl[:], scalar=vec(4, i), in1=kmod[:], op0=ALU.mult,
                                                             op1=ALU.mult), R=[rl, kmod, smallw], W=[prod])
                psrk = bsum(prod[:], prod)
                k.op("dve", lambda e, psrk=psrk: e.tensor_tensor(out=bonus[:], in0=psrk[:, 0:GM], in1=vl[:], op=ALU.mult),
                     R=[psrk, vl], W=[bonus])
                k.op("dve", lambda e: e.tensor_tensor_scan(out=lcum[:], data0=rmask[:], data1=ld[:], initial=0.0, op0=ALU.mult,
                                                           op1=ALU.add), R=[rmask, ld], W=[lcum])
                k.op("act", lambda e: e.activation(out=Pinc[:], in_=lcum[:], func=AF.Exp), R=[lcum], W=[Pinc])
                k.op("dve", lambda e: e.tensor_tensor(out=Pexc[:], in0=lcum[:], in1=ld[:], op=ALU.subtract), R=[lcum, ld], W=[Pexc])
                k.op("act", lambda e: e.activation(out=Pexc[:], in_=Pexc[:], func=AF.Exp), R=[], W=[Pexc])
                k.op("act", lambda e: e.activation(out=Pinv[:], in_=lcum[:], func=AF.Exp, scale=-1.0), R=[lcum], W=[Pinv])
                k.op("dve", lambda e: e.scalar_tensor_tensor(out=At[:], in0=kkn[:], scalar=-1.0, in1=Pexc[:], op0=ALU.mult, op1=ALU.mult),
                     R=[kkn, Pexc], W=[At])
                k.op("dve", lambda e: e.tensor_tensor(out=Bt[:], in0=kkn[:], in1=av[:], op=ALU.mult), R=[kkn, av], W=[Bt])
                k.op("dve", lambda e: e.tensor_tensor(out=Bt[:], in0=Bt[:], in1=Pinv[:], op=ALU.mult), R=[Pinv], W=[Bt])
                k.op("dve", lambda e: e.tensor_tensor(out=Kt[:], in0=kmod[:], in1=Pinv[:], op=ALU.mult), R=[kmod, Pinv], W=[Kt])
                k.op("dve", lambda e: e.tensor_tensor(out=Rt[:], in0=rl[:], in1=Pinc[:], op=ALU.mult), R=[rl, Pinc], W=[Rt])
                k.op("act", lambda e: e.activation(out=Rtb[:], in_=Rt[:], func=AF.Copy), R=[Rt], W=[Rtb])
                for src_, dst_ in ((At, Atb), (Bt, Btb), (Kt, Ktb)):
                    k.op("pool", lambda e: e.tensor_copy(out=dst_[:], in_=src_[:]), R=[src_], W=[dst_])
                for tb in range(TPG):
                    cl = slice(tb * 128, (tb + 1) * 128)
                    ps = psum()
                    for n_, src in enumerate((At, Bt, Kt, vl)):
                        k.op("pe", lambda e: e.transpose(out=ps[:, n_ * 128:(n_ + 1) * 128], in_=src[:, cl], identity=ident),
                             R=[src, cm], W=[ps])
                    k.op("act", lambda e: e.activation(out=TTt[tb][:], in_=ps[:, :], func=AF.Copy), R=[ps], W=[TTt[tb]])
                    for hh in range(2):
                        k.op("dve" if hh else "act", lambda e: (e.tensor_copy(
                            out=pads[hh][tb][:, :, hh * 64:hh * 64 + 64],
                            in_=TTt[tb][:, 128:512].rearrange("p (a c) -> p a c", a=3)[:, :, hh * 64:hh * 64 + 64]) if hh else
                            e.activation(out=pads[hh][tb][:, :, hh * 64:hh * 64 + 64],
                                         in_=TTt[tb][:, 128:512].rearrange("p (a c) -> p a c", a=3)[:, :, hh * 64:hh * 64 + 64],
                                         func=AF.Copy)),
                            R=[TTt[tb]], W=[pads[hh][tb]])
                k.op("act", lambda e: e.activation(out=PL[:], in_=Pinc[:, 63:GM:64], func=AF.Copy), R=[Pinc], W=[PL])

            def tile_X(i, par):
                Osb, dd, dsq, rs = tmps[20:24]
                gg, bonus, PL = ggs[par], bonuss[par], PLs[par]
                Atb, Btb, Ktb, Rtb, TTt, pads = Atbs[par], Btbs[par], Ktbs[par], Rtbs[par], TTts[par], padss[par]
                for tb in range(TPG):
                    cl = slice(tb * 128, (tb + 1) * 128)
                    specs = [(Btb, Atb, mk2_su, Xs[tb][0]), (Atb, Btb, mk2_sl, XTs[tb][0]), (Atb, Ktb, mk2_sl, MakT[tb]),
                             (Btb, Rtb, mk2_ui, Mbr[tb]), (Ktb, Rtb, mk2_ui, Mkr[tb])]
                    gps = [psum() for _ in specs]
                    for hh in range(2):
                        pr = slice(hh * 64, hh * 64 + 64)
                        for (l, r, mask2, dst), ps in zip(specs, gps):
                            mm(ps, ps[:, hh * 128:(hh + 1) * 128], [(l[pr, cl], r[pr, cl])], R=[l, r])
                    for (l, r, mask2, dst), ps in zip(specs, gps):
                        k.op("dve", lambda e: e.tensor_tensor(out=dst[:].rearrange("p a b -> p (a b)"), in0=ps[:, 0:256], in1=mask2,
                                                              op=ALU.mult), R=[ps, cm2], W=[dst])
                    k.op("dve", lambda e: e.tensor_tensor(out=Tms[tb][0][:].rearrange("p a b -> p (a b)"),
                                                          in0=Xs[tb][0][:].rearrange("p a b -> p (a b)"), in1=mk2_id, op=ALU.add),
                         R=[Xs[tb][0], cm2], W=[Tms[tb][0]])
                cur = 0
                for lvl in range(1, 6):
                    nxt = 1 - cur
                    pss = {}
                    for tb in range(TPG):
                        if lvl < 5:
                            ps = psum()
                            for hh in range(2):
                                mm(ps, ps[:, hh * 128:(hh + 1) * 128], [(XTs[tb][cur][:, hh, :], Xs[tb][cur][:, hh, :])],
                                   R=[XTs[tb][cur], Xs[tb][cur]])
                            pss[(tb, 0)] = ps
                        ps = psum()
                        for hh in range(2):
                            mm(ps, ps[:, hh * 128:(hh + 1) * 128], [(Xs[tb][cur][:, hh, :], XTs[tb][cur][:, hh, :])],
                               R=[XTs[tb][cur], Xs[tb][cur]])
                        pss[(tb, 1)] = ps
                    for tb in range(TPG):
                        if lvl < 5:
                            ps = pss[(tb, 0)]
                            k.op("act", lambda e: e.activation(out=Xs[tb][nxt][:].rearrange("p a b -> p (a b)"), in_=ps[:, 0:256],
                                                               func=AF.Copy), R=[ps], W=[Xs[tb][nxt]])
                        ps = pss[(tb, 1)]
                        k.op("act", lambda e: e.activation(out=XTs[tb][nxt][:].rearrange("p a b -> p (a b)"), in_=ps[:, 0:256],
                                                           func=AF.Copy), R=[ps], W=[XTs[tb][nxt]])
                    for tb in range(TPG):
                        ps = psum()
                        for hh in range(2):
                            mm(ps, ps[:, hh * 128:(hh + 1) * 128], [(XTs[tb][nxt][:, hh, :], Tms[tb][cur][:, hh, :])],
                               R=[XTs[tb][nxt], Tms[tb][cur]])
                        pss[(tb, 2)] = ps
                    for tb in range(TPG):
                        ps = pss[(tb, 2)]
                        k.op("dve", lambda e: e.tensor_tensor(out=Tms[tb][nxt][:].rearrange("p a b -> p (a b)"), in0=ps[:, 0:256],
                                                              in1=Tms[tb][cur][:].rearrange("p a b -> p (a b)"), op=ALU.add),
                             R=[ps, Tms[tb][cur]], W=[Tms[tb][nxt]])
                    cur = nxt
                pA, pM = {}, {}
                for tb in range(TPG):
                    ps = psum()
                    for hh in range(2):
                        mm(ps, ps[:, hh * 128:(hh + 1) * 128], [(TTt[tb][:, 0:128], Tms[tb][cur][:, hh, :])], R=[TTt[tb], Tms[tb][cur]])
                    pA[tb] = ps
                    ps = psum()
                    for hh in range(2):
                        mm(ps, ps[:, hh * 128:(hh + 1) * 128], [(MakT[tb][:, hh, :], Tms[tb][cur][:, hh, :])], R=[MakT[tb], Tms[tb][cur]])
                    pM[tb] = ps
                for tb in range(TPG):
                    for hh in range(2):
                        pr = slice(hh * 64, hh * 64 + 64)
                        k.op("act", lambda e: e.activation(out=Ah[tb][pr, :], in_=pA[tb][pr, hh * 128:(hh + 1) * 128], func=AF.Copy),
                             R=[pA[tb]], W=[Ah[tb]])
                    k.op("act", lambda e: e.activation(out=MT[tb][:].rearrange("p a b -> p (a b)"), in_=pM[tb][:, 0:256], func=AF.Copy),
                         R=[pM[tb]], W=[MT[tb]])
                for tb in range(TPG):
                    ps = psum()
                    for hh in range(2):
                        mm(ps, ps[:, hh * 64:(hh + 1) * 64], [(MT[tb][:, hh, :], TTt[tb][:, 384 + hh * 64:384 + hh * 64 + 64])],
                           R=[MT[tb], TTt[tb]])
                    k.op("act", lambda e: e.activation(out=WT2[tb][:], in_=ps[:, 0:128], func=AF.Copy), R=[ps], W=[WT2[tb]])
                for tb in range(TPG):
                    for c in range(2):
                        ro = slice(64 * c, 64 * c + 64)
                        cc0 = tb * 128 + 64 * c
                        k.op("dve", lambda e: e.tensor_scalar(out=Sdec[:], in0=STf[i][:], scalar1=PL[:, 2 * tb + c:2 * tb + c + 1], scalar2=None,
                                                              op0=ALU.mult), R=[STf[i], PL], W=[Sdec])
                        ps = psum()
                        mm(ps, ps[:, 0:128], [(Ah[tb][:], STb[i][:])], R=[Ah[tb], STb[i]])
                        for hh in range(2):
                            hs = slice(hh * 64, hh * 64 + 64)
                            k.op("dve", lambda e: e.tensor_tensor(out=UTpad[hh][ro, hs], in0=ps[ro, hs], in1=WT2[tb][ro, hs], op=ALU.add),
                                 R=[ps, WT2[tb]], W=[UTpad[hh]])
                        pss_ = psum()
                        mm(pss_, pss_[:, 0:128],
                           [(pads[0][tb][ro, 0, :], UTpad[0][ro, :]), (pads[1][tb][ro, 0, :], UTpad[1][ro, :]),
                            (pads[0][tb][ro, 1, :], pads[0][tb][ro, 2, :]), (pads[1][tb][ro, 1, :], pads[1][tb][ro, 2, :])],
                           R=[pads[0][tb], pads[1][tb], UTpad[0], UTpad[1]])
                        k.op("dve", lambda e: e.scalar_tensor_tensor(out=STf[i][:], in0=pss_[:, 0:128], scalar=PL[:, 2 * tb + c:2 * tb + c + 1],
                                                                     in1=Sdec[:], op0=ALU.mult, op1=ALU.add),
                             R=[pss_, PL, Sdec], W=[STf[i]])
                        pso = psum()
                        mm(pso, pso[:, 0:64],
                           [(STb[i][:], Rtb[:, cc0:cc0 + 64]),
                            (UTpad[0][ro, :], Mbr[tb][ro, 0, ro]), (UTpad[1][ro, :], Mbr[tb][ro, 1, ro]),
                            (pads[0][tb][ro, 2, :], Mkr[tb][ro, 0, ro]), (pads[1][tb][ro, 2, :], Mkr[tb][ro, 1, ro])],
                           R=[STb[i], Rtb, UTpad[0], UTpad[1], Mbr[tb], Mkr[tb], pads[0][tb], pads[1][tb]])
                        k.op("act", lambda e: e.activation(out=STb[i][:], in_=STf[i][:], func=AF.Copy), R=[STf[i]], W=[STb[i]])
                        k.op("act", lambda e: e.activation(out=Osb[:, cc0:cc0 + 64], in_=pso[:, 0:64], func=AF.Copy), R=[pso], W=[Osb])
                psm = bsum(Osb[:], Osb)
                k.op("dve", lambda e, psm=psm: e.scalar_tensor_tensor(out=dd[:], in0=psm[:, 0:GM], scalar=-1.0 / 64, in1=Osb[:],
                                                                        op0=ALU.mult, op1=ALU.add), R=[psm, Osb], W=[dd])
                k.op("act", lambda e: e.activation(out=dsq[:], in_=dd[:], func=AF.Square), R=[dd], W=[dsq])
                psv = bsum(dsq[:], dsq)
                rsqrt_ps(psv, 1.0 / 64, GN_EPS, rs)
                k.op("dve", lambda e: e.tensor_tensor(out=dd[:], in0=dd[:], in1=rs[:], op=ALU.mult), R=[rs], W=[dd])
                k.op("dve", lambda e: e.tensor_scalar(out=dd[:], in0=dd[:], scalar1=vec(5, i), scalar2=vec(6, i), op0=ALU.mult, op1=ALU.add),
                     R=[smallw], W=[dd])
                k.op("dve", lambda e: e.tensor_tensor(out=dd[:], in0=dd[:], in1=bonus[:], op=ALU.add), R=[bonus], W=[dd])
                k.op("dve", lambda e: e.tensor_tensor(out=mixT[:, 4 + i, :], in0=dd[:], in1=gg[:], op=ALU.mult), R=[dd, gg],
                     W=[mixT.subs[4 + i]])
            bstate["pool"] = "Y"
            k.begin_rec(); tile_A1(0, 0); Y_ = k.end_rec()
            bstate["pool"] = None
            k.play_merged(Y_)
            for i in range(4):
                bstate["pool"] = "X"
                k.begin_rec(); tile_X(i, i % 2); X_ = k.end_rec()
                bstate["pool"] = "Y"
                k.begin_rec()
                if i < 3:
                    tile_A1(i + 1, (i + 1) % 2)
                else:
                    nxt_ = b * NG + g + 1
                    if nxt_ < NB * NG:
                        stage_U(nxt_ // NG, nxt_ % NG)
                Y_ = k.end_rec()
                bstate["pool"] = None
                if PMODE == 0:
                    k.play_merged(X_)
                    k.play_merged(Y_)
                else:
                    k.play_merged(X_, Y_)
            for tt in range(TPG):
                for half in range(2):
                    hsl = slice(half * 512, (half + 1) * 512)
                    ps = psum()
                    mm(ps, ps[:, :], [(mixT[:, c_, tt * 128:(tt + 1) * 128], woutb[:, c_, hsl]) for c_ in range(8)],
                       R=[woutb] + mixT.subs)
                    k.op("dve", lambda e, ps=ps, hsl=hsl: e.tensor_tensor(out=ps[:, :], in0=ps[:, :], in1=gt1t[:, hsl],
                                                                          op=ALU.mult), R=[gt1t], W=[ps])
                    k.op("dve", lambda e, ps=ps, hsl=hsl, tt=tt: e.tensor_tensor(out=xg[:, tt, hsl], in0=ps[:, :], in1=xg[:, tt, hsl],
                                                                                 op=ALU.add), R=[ps], W=[xg.subs[tt]])
                k.dma(h1_d[b, t0 + tt * 128:t0 + (tt + 1) * 128, :], xg[:, tt, :], R=[xg.subs[tt]], q="pool")
            bpg_ = -(-NBLK // (NB * NG))
            n_cur_ = b * NG + g
            for blk_ in range(n_cur_ * bpg_, min(NBLK, (n_cur_ + 1) * bpg_)):
                k.dma(xs_d[blk_ * BLK:(blk_ + 1) * BLK, :], zeros_d[:, :])

    if dbg == "h1":
        k.finish()
        return nc

    k.barrier()
    MX.close()
    PSX.close()
    del banks[6:]
    bbanks.extend(TT(nc.alloc_psum_tensor("psbb%d" % i, [128, 1024], BF)) for i in range(2))
    ME_P = ExitStack()
    I32 = mybir.dt.int32
    ltri_f, ones_f = cm[:, 5, :], cm[:, 6, :]

    def psb(*a, **kw):
        return k.sb(*a, scope=ME_P, **kw)
    cmb2 = psb("cmb2", [128, 2, 128], BF)
    k.op("dve", lambda e: e.tensor_copy(out=cmb2[:], in_=cm[:, 5:7, :]), R=[cm], W=[cmb2])
    lgs = psb("lgs", [128, NTC, 32])
    ranks = psb("ranks", [128, NTC, 32])
    m8s = psb("m8s", [128, NTC, 8])
    g4 = psb("g4", [128, NTC, 4])
    dest_f = psb("dest_f", [128, NTC * 4])
    dest_i = psb("dest_i", [128, NTC * 4], I32)
    cnt = psb("cnt", [128, 32])
    lay = psb("lay", [128, 5, 32])
    bs = psb("bs", [128, NBLK]); k.dma(bs[:], bs_d[:, :], W=[bs])
    iop = psb("iop", [128, 8]); k.dma(iop[:], iop_d[:, :], W=[iop])
    be = psb("be", [128, 2, NBLK])
    idxW_f = psb("idxW_f", [128, NBLK, 8])
    idxW_i = psb("idxW_i", [128, NBLK * 8], I32)
    idxW2_f = psb("idxW2_f", [128, NBLK, 8])
    idxW2_i = psb("idxW2_i", [128, NBLK * 8], I32)
    skp = psb("skp", [128, 2, NBLK])
    idxb_f = psb("idxb_f", [128, 2, NBLK])
    idxb_i = psb("idxb_i", [128, 2 * NBLK], I32)
    k.op("dve", lambda e: e.memset(cnt[:], 0.0), W=[cnt])
    k.op("dve", lambda e: e.memset(dest_f[:], 0.0), W=[dest_f])

    ME_R = ExitStack()

    def rsb(*a, **kw):
        return k.sb(*a, scope=ME_R, **kw)
    u2tm = rsb("u2tm", [128, NTC, D], BF, nsub=NTC)
    u2fs = [rsb("u2f", [128, 8, 128]) for _ in range(2)]
    xn.append(rsb("xn2", [128, D]))
    h2 = [rsb("h2", [128, D]) for _ in range(2)]
    G2bc = [rsb("G2bc", [128, D]) for _ in range(NB)]
    SH2bc = [rsb("SH2bc", [128, D]) for _ in range(NB)]
    maskbs = [rsb("maskb", [128, 32], BF) for _ in range(2)]
    rts = [rsb("rt", [128, 64]) for _ in range(2)]
    for b in range(NB):
        k.dma(SH2bc[b][:], modscr_d[b, 2, :].partition_broadcast(128), W=[SH2bc[b]])
        k.dma(G2bc[b][:], modscr_d[b, 3, :].partition_broadcast(128), W=[G2bc[b]])
    def router_A(tile):
        b, tt = tile // NT, tile % NT
        ht = h2[tile % 2]
        u2f, maskb, rt_ = u2fs[tile % 2], maskbs[tile % 2], rts[tile % 2]
        k.dma(ht[:], h1_d[b, tt * 128:(tt + 1) * 128, :], W=[ht])

        def ev2(kc, psap, sc, bi, ps):
            k.op("act", lambda e: e.activation(out=u2f[:, kc, :], in_=psap, func=AF.Identity, scale=sc, bias=bi),
                 R=[ps, GS[b]], W=[u2f])
        norm_T(ht[:], ht, GS[b], 16, ev2, xi=tile % 2)
        k.op("dve", lambda e: e.tensor_tensor(out=ht[:], in0=xn[tile % 2][:], in1=G2bc[b][:], op=ALU.mult),
             R=[xn[tile % 2], G2bc[b]], W=[ht])
        k.op("dve", lambda e: e.tensor_tensor(out=u2tm[:, tile, :], in0=ht[:], in1=SH2bc[b][:], op=ALU.add),
             R=[ht, SH2bc[b]], W=[u2tm.subs[tile]])
        ps = psum()
        mm(ps, ps[:, 0:32], [(u2f[:, kc, :], rw[:, kc, :]) for kc in range(8)], R=[u2f, rw])
        lg = lgs[:, tile, :]
        m8 = m8s[:, tile, :]
        k.op("dve", lambda e: e.tensor_tensor(out=lg, in0=ps[:, 0:32], in1=rb[:], op=ALU.add), R=[ps, rb], W=[lgs])
        k.op("dve", lambda e: e.max(out=m8, in_=lg), R=[lgs], W=[m8s])
        k.op("dve", lambda e: e.tensor_scalar(out=maskb[:], in0=lg, scalar1=m8s[:, tile, 3:4], scalar2=None, op0=ALU.is_ge),
             R=[lgs, m8s], W=[maskb])

    def router_B(tile):
        b, tt = tile // NT, tile % NT
        maskb, rt_ = maskbs[tile % 2], rts[tile % 2]
        psr = psum()
        mm(psr, psr[:, 0:32], [(cmb2[:, 0, :], maskb[:])], R=[cmb2, maskb])
        k.op("dve", lambda e: e.tensor_tensor(out=ranks[:, tile, :], in0=psr[:, 0:32], in1=cnt[:], op=ALU.add),
             R=[psr, cnt], W=[ranks])
        psc = psum()
        mm(psc, psc[:, 0:32], [(cmb2[:, 1, :], maskb[:])], R=[cmb2, maskb])
        k.op("dve", lambda e: e.tensor_tensor(out=cnt[:], in0=psc[:, 0:32], in1=cnt[:], op=ALU.add), R=[psc], W=[cnt])
        negm, ssum, ex4 = rt_[:, 0:1], rt_[:, 1:2], rt_[:, 4:8]
        k.op("dve", lambda e: e.tensor_scalar(out=negm, in0=m8s[:, tile, 0:1], scalar1=-1.0, scalar2=None, op0=ALU.mult),
             R=[m8s], W=[rt_])
        k.op("act", lambda e: e.activation(out=ex4, in_=m8s[:, tile, 0:4], func=AF.Exp, bias=negm, scale=1.0), R=[m8s], W=[rt_])
        k.op("dve", lambda e: e.tensor_reduce(out=ssum, in_=ex4, axis=mybir.AxisListType.X, op=ALU.add), R=[], W=[rt_])
        k.op("dve", lambda e: e.reciprocal(out=ssum, in_=ssum), R=[], W=[rt_])
        k.op("dve", lambda e: e.tensor_scalar(out=g4[:, tile, :], in0=ex4, scalar1=ssum, scalar2=None, op0=ALU.mult),
             R=[rt_], W=[g4])
    for t2 in range(0, NTC, 2):
        recs = []
        for tile in range(t2, min(t2 + 2, NTC)):
            k.begin_rec(); router_A(tile); recs.append(k.end_rec())
        k.play_merged(*recs)
        for tile in range(t2, min(t2 + 2, NTC)):
            router_B(tile)
    nbt, padded, pend, pstart, ones32 = (lay[:, i, :] for i in range(5))
    k.op("dve", lambda e: e.memset(ones32, 1.0), W=[lay])
    k.op("dve", lambda e: e.tensor_scalar(out=nbt, in0=cnt[:], scalar1=0.0, scalar2=None, op0=ALU.is_gt), R=[cnt], W=[lay])
    for j in range(1, (NTC * 128) // BLK):
        k.op("dve", lambda e: e.scalar_tensor_tensor(out=nbt, in0=cnt[:], scalar=float(BLK * j), in1=nbt, op0=ALU.is_gt,
                                                     op1=ALU.add), R=[cnt], W=[lay])
    k.op("dve", lambda e: e.tensor_scalar(out=padded, in0=nbt, scalar1=float(BLK), scalar2=None, op0=ALU.mult), R=[], W=[lay])
    k.op("dve", lambda e: e.tensor_tensor_scan(out=pend, data0=ones32, data1=padded, initial=0.0, op0=ALU.mult, op1=ALU.add),
         R=[], W=[lay])
    k.op("dve", lambda e: e.tensor_tensor(out=pstart, in0=pend, in1=padded, op=ALU.subtract), R=[], W=[lay])
    k.op("dve", lambda e: e.memset(be[:, 0, :], 0.0), W=[be])
    for e_ in range(32):
        k.op("dve", lambda e: e.scalar_tensor_tensor(out=be[:, 0, :], in0=bs[:], scalar=lay[:, 2, e_:e_ + 1], in1=be[:, 0, :],
                                                     op0=ALU.is_ge, op1=ALU.add), R=[bs, lay], W=[be])
    k.op("dve", lambda e: e.tensor_scalar(out=be[:, 0, :], in0=be[:, 0, :], scalar1=float(E - 1), scalar2=None, op0=ALU.min),
         R=[], W=[be])
    k.op("dve", lambda e: e.tensor_scalar(out=be[:, 1, :], in0=be[:, 0, :], scalar1=float(D), scalar2=None, op0=ALU.mult),
         R=[], W=[be])
    for kc in range(8):
        k.op("dve", lambda e: e.tensor_scalar(out=idxW_f[:, :, kc], in0=be[:, 1, :], scalar1=iop[:, kc:kc + 1], scalar2=None,
                                              op0=ALU.add), R=[be, iop], W=[idxW_f])
    BIG = 1.0e6
    k.op("dve", lambda e: e.memset(skp[:], 0.0), W=[skp])
    k.op("dve", lambda e: e.tensor_tensor(out=skp[:, 0, 2:NBLK], in0=be[:, 0, 2:NBLK], in1=be[:, 0, 0:NBLK - 2], op=ALU.is_equal),
         R=[be], W=[skp])
    k.op("dve", lambda e: e.tensor_tensor(out=skp[:, 1, 1:NBLK], in0=be[:, 0, 1:NBLK], in1=be[:, 0, 0:NBLK - 1], op=ALU.is_equal),
         R=[be], W=[skp])
    k.op("dve", lambda e: e.tensor_scalar(out=skp[:], in0=skp[:], scalar1=BIG, scalar2=None, op0=ALU.mult), R=[], W=[skp])
    for kc in range(8):
        k.op("dve", lambda e: e.tensor_tensor(out=idxW2_f[:, :, kc], in0=idxW_f[:, :, kc], in1=skp[:, 1, :], op=ALU.add),
             R=[idxW_f, skp], W=[idxW2_f])
    k.op("dve", lambda e: e.tensor_copy(out=idxW2_i[:], in_=idxW2_f[:].rearrange("p a b -> p (a b)")), R=[idxW2_f], W=[idxW2_i])
    for kc in range(8):
        k.op("dve", lambda e: e.tensor_tensor(out=idxW_f[:, :, kc], in0=idxW_f[:, :, kc], in1=skp[:, 0, :], op=ALU.add),
             R=[skp], W=[idxW_f])
    k.op("dve", lambda e: e.tensor_copy(out=idxW_i[:], in_=idxW_f[:].rearrange("p a b -> p (a b)")), R=[idxW_f], W=[idxW_i])
    k.op("dve", lambda e: e.tensor_scalar(out=idxb_f[:, 0, :], in0=be[:, 0, :], scalar1=128.0, scalar2=iop[:, 0:1],
                                          op0=ALU.mult, op1=ALU.add), R=[be, iop], W=[idxb_f])
    k.op("dve", lambda e: e.tensor_copy(out=idxb_f[:, 1, :], in_=be[:, 0, :]), R=[be], W=[idxb_f])
    k.op("dve", lambda e: e.tensor_copy(out=idxb_i[:], in_=idxb_f[:].rearrange("p a b -> p (a b)")), R=[idxb_f], W=[idxb_i])
    rt_ = rts[0]
    for tile in range(NTC):
        k.op("dve", lambda e: e.tensor_tensor(out=ranks[:, tile, :], in0=ranks[:, tile, :], in1=pstart, op=ALU.add),
             R=[lay], W=[ranks])
        for kk_ in range(4):
            k.op("dve", lambda e: e.scalar_tensor_tensor(out=rt_[:, 32:64], in0=lgs[:, tile, :], scalar=m8s[:, tile, kk_:kk_ + 1],
                                                         in1=ranks[:, tile, :], op0=ALU.is_equal, op1=ALU.mult,
                                                         accum_out=dest_f[:, tile * 4 + kk_:tile * 4 + kk_ + 1]),
                 R=[lgs, m8s, ranks], W=[rt_, dest_f])
    k.op("dve", lambda e: e.tensor_copy(out=dest_i[:], in_=dest_f[:]), R=[dest_f], W=[dest_i])
    if dbg == "moe":
        dbg_d = nc.dram_tensor("dbg", [128, NTC * 4 + NTC * 4 + 32 + NBLK], F32, kind="ExternalOutput").ap()
        k.dma(dbg_d[:, 0:NTC * 4], dest_f[:], R=[dest_f])
        k.dma(dbg_d[:, NTC * 4:NTC * 8], g4[:].rearrange("p a b -> p (a b)"), R=[g4])
        k.dma(dbg_d[:, NTC * 8:NTC * 8 + 32], cnt[:], R=[cnt])
        k.dma(dbg_d[:, NTC * 8 + 32:NTC * 8 + 32 + NBLK], be[:, 0, :], R=[be])
    for tile in range(NTC):
        for kk_ in range(4):
            c_ = tile * 4 + kk_
            k.dma(xs_d[:, :], u2tm[:, tile, :], R=[u2tm.subs[tile], dest_i], q="pool", indirect=("scatter", dest_i[:, c_:c_ + 1]))
    k.barrier()
    xn.pop()
    ME_R.close()

    ME_X = ExitStack()

    def xsb(*a, **kw):
        return k.sb(*a, scope=ME_X, **kw)
    W1b = [xsb("W1b", [128, 8, 2 * D], BF, nsub=8) for _ in range(2)]
    W2b = xsb("W2b", [128, 8, D], BF, nsub=8)
    xrow = [xsb("xrow", [128, 4, D], BF) for _ in range(2)]
    xsT = [xsb("xsT", [128, 8, BLK], BF, nsub=8) for _ in range(2)]
    actT = [xsb("actT", [128, 8, BLK], BF, nsub=8) for _ in range(2)]
    et = [[xsb("et", [128, BLK]) for _ in range(4)] for _ in range(2)]
    yrow = [xsb("yrow", [128, D]) for _ in range(2)]
    b2bc = [xsb("b2bc", [128, D]) for _ in range(2)]
    b1blk = [xsb("b1blk", [128, 16]) for _ in range(2)]
    ectr = {"n": 0, "y": 0}
    def load_weights(blk):
        w1 = W1b[blk % 2]
        b1t, b2t = b1blk[blk % 2], b2bc[blk % 2]
        k.dma(b1t[:], b1_d[:, :], R=[idxb_i], W=[b1t], q="pool", indirect=("gather", idxb_i[:, blk:blk + 1]))
        k.dma(b2t[:], b2_d[:, :], R=[idxb_i], W=[b2t], q="pool", indirect=("gather", idxb_i[:, NBLK + blk:NBLK + blk + 1]))
        for kc in range(8):
            c_ = blk * 8 + kc
            k.dma(w1[:, kc, :], w1_d[:, :], R=[idxW_i], W=[w1.subs[kc]], q="pool",
                  indirect=("gather", idxW_i[:, c_:c_ + 1], E * D - 1))

    def load_w2(blk):
        for fc in range(8):
            c_ = blk * 8 + fc
            k.dma(W2b[:, fc, :], w2_d[:, :], R=[idxW2_i], W=[W2b.subs[fc]], q="pool",
                  indirect=("gather", idxW2_i[:, c_:c_ + 1], E * D - 1))

    def load_x(blk):
        k.dma(xrow[blk % 2][:], xs_d[blk * BLK:(blk + 1) * BLK, :].rearrange("(s p) d -> p s d", p=128), W=[xrow[blk % 2]])

    def transposes(blk):
        xT = xsT[blk % 2]
        xr = xrow[blk % 2]
        for kc in range(8):
            if kc % 2 == 0:
                pb = psum_bf()
            po = (kc % 2) * 512
            for s_ in range(4):
                k.op("pe", lambda e: e.transpose(out=pb[:, po + s_ * 128:po + (s_ + 1) * 128], in_=xr[:, s_, kc * 128:(kc + 1) * 128],
                                                 identity=cmb[:]), R=[xr, cmb], W=[pb])
            k.op("act", lambda e: e.activation(out=xT[:, kc, :], in_=pb[:, po:po + 512], func=AF.Copy), R=[pb], W=[xT.subs[kc]])

    load_weights(0)
    load_x(0)
    transposes(0)
    for blk in range(NBLK):
        w1 = W1b[blk % 2]
        b1t, b2t = b1blk[blk % 2], b2bc[blk % 2]
        load_w2(blk)
        if blk + 1 < NBLK:
            load_weights(blk + 1)
            load_x(blk + 1)
        xT = xsT[blk % 2]
        aT = actT[blk % 2]
        for fc in range(8):
            xg_, sg_, xl_, tt_ = et[ectr["n"] % 2]
            ectr["n"] += 1
            psg = psum()
            mm(psg, psg[:, :], [(w1[:, kc, fc * 128:(fc + 1) * 128], xT[:, kc, :]) for kc in range(8)], R=w1.subs + xT.subs)
            psl = psum()
            mm(psl, psl[:, :], [(w1[:, kc, D + fc * 128:D + (fc + 1) * 128], xT[:, kc, :]) for kc in range(8)], R=w1.subs + xT.subs)
            k.op("dve", lambda e: e.tensor_scalar(out=xg_[:], in0=psg[:, :], scalar1=b1t[:, fc:fc + 1], scalar2=7.0,
                                                  op0=ALU.add, op1=ALU.min), R=[psg, b1t], W=[xg_])
            k.op("act", lambda e: e.activation(out=sg_[:], in_=xg_[:], func=AF.Sigmoid, scale=1.702), R=[xg_], W=[sg_])
            k.op("dve", lambda e: e.tensor_scalar(out=xl_[:], in0=psl[:, :], scalar1=b1t[:, 8 + fc:9 + fc], scalar2=7.0,
                                                  op0=ALU.add, op1=ALU.min), R=[psl, b1t], W=[xl_])
            k.op("dve", lambda e: e.tensor_scalar(out=xl_[:], in0=xl_[:], scalar1=-7.0, scalar2=1.0, op0=ALU.max, op1=ALU.add),
                 R=[], W=[xl_])
            k.op("dve", lambda e: e.tensor_tensor(out=tt_[:], in0=xg_[:], in1=sg_[:], op=ALU.mult), R=[xg_, sg_], W=[tt_])
            k.op("dve", lambda e: e.tensor_tensor(out=aT[:, fc, :], in0=tt_[:], in1=xl_[:], op=ALU.mult), R=[tt_, xl_],
                 W=[aT.subs[fc]])
        if blk + 1 < NBLK:
            transposes(blk + 1)
        for s_ in range(4):
            yr = yrow[ectr["y"] % 2]
            ectr["y"] += 1
            for half in range(2):
                hsl = slice(half * 512, (half + 1) * 512)
                ps = psum()
                mm(ps, ps[:, :], [(aT[:, fc, s_ * 128:(s_ + 1) * 128], W2b[:, fc, hsl]) for fc in range(8)], R=aT.subs + W2b.subs)
                k.op("dve", lambda e: e.tensor_tensor(out=yr[:, hsl], in0=ps[:, :], in1=b2t[:, hsl], op=ALU.add), R=[ps, b2t], W=[yr])
            k.dma(yb_d[blk * BLK + s_ * 128:blk * BLK + (s_ + 1) * 128, :], yr[:], R=[yr])
    k.barrier()
    ME_X.close()

    ME_C = ExitStack()

    def csb(*a, **kw):
        return k.sb(*a, scope=ME_C, **kw)
    ygat = [[csb("ygat", [128, D]) for _ in range(4)] for _ in range(2)]
    h3 = [csb("h3", [128, D]) for _ in range(2)]
    fg = csb("fg", [128, D]); k.dma(fg[:], fg_d[:, :], W=[fg])
    for b in range(NB):
        gtb[b][1] = csb("gtb2", [128, D])
        k.dma(gtb[b][1][:], modscr_d[b, 1, :].partition_broadcast(128), W=[gtb[b][1]])
    def combine_tile(tile):
        b, tt = tile // NT, tile % NT
        ht = h3[tile % 2]
        yg = ygat[tile % 2]
        for kk_ in range(4):
            c_ = tile * 4 + kk_
            k.dma(yg[kk_][:], yb_d[:, :], R=[dest_i], W=[yg[kk_]], q="pool", indirect=("gather", dest_i[:, c_:c_ + 1]))
        k.dma(ht[:], h1_d[b, tt * 128:(tt + 1) * 128, :], W=[ht])
        k.op("dve", lambda e: e.tensor_scalar(out=yg[0][:], in0=yg[0][:], scalar1=g4[:, tile, 0:1], scalar2=None, op0=ALU.mult),
             R=[g4], W=[yg[0]])
        for kk_ in range(1, 4):
            k.op("dve", lambda e: e.scalar_tensor_tensor(out=yg[0][:], in0=yg[kk_][:], scalar=g4[:, tile, kk_:kk_ + 1], in1=yg[0][:],
                                                         op0=ALU.mult, op1=ALU.add), R=[yg[kk_], g4], W=[yg[0]])
        if dbg == "moe":
            k.dma(out_d[b, tt * 128:(tt + 1) * 128, :], yg[0][:], R=[yg[0]])
            return
        k.op("dve", lambda e: e.tensor_tensor(out=yg[0][:], in0=yg[0][:], in1=gtb[b][1][:], op=ALU.mult), R=[gtb[b][1]], W=[yg[0]])
        k.op("dve", lambda e: e.tensor_tensor(out=ht[:], in0=ht[:], in1=yg[0][:], op=ALU.add), R=[yg[0]], W=[ht])
        st, i = rstd_of(ht[:], ht)
        k.op("dve", lambda e: e.scalar_tensor_tensor(out=ht[:], in0=ht[:], scalar=st[:, 3:4], in1=fg[:], op0=ALU.mult, op1=ALU.mult),
             R=[st, fg], W=[ht])
        k.dma(out_d[b, tt * 128:(tt + 1) * 128, :], ht[:], R=[ht])
    for t2 in range(0, NTC, 2):
        recs = []
        for tile in range(t2, min(t2 + 2, NTC)):
            k.begin_rec(); combine_tile(tile); recs.append(k.end_rec())
        k.play_merged(*recs)
    k.finish()
    return nc


def make_consts(NB, GM):
    idx = np.arange(128)
    same = (idx[:, None] // 64) == (idx[None, :] // 64)
    ident = np.eye(128, dtype=np.float32)
    m_su = (same & (idx[:, None] < idx[None, :])).astype(np.float32)
    m_sl = (same & (idx[:, None] > idx[None, :])).astype(np.float32)
    m_ui = (same & (idx[:, None] <= idx[None, :])).astype(np.float32)
    bones = same.astype(np.float32)
    ltri = (idx[:, None] < idx[None, :]).astype(np.float32)
    cm = np.ascontiguousarray(np.stack([ident, m_su, m_sl, m_ui, bones, ltri, np.ones((128, 128), np.float32)], axis=1))
    cm2 = np.ascontiguousarray(np.stack([np.concatenate([m, m], axis=1) for m in (m_su, m_sl, m_ui, ident)], axis=1))
    rm = np.ones((128, GM), np.float32)
    rm[:, ::64] = 0.0
    selb = np.zeros((NB, NB, 128), np.float32)
    for b in range(NB):
        selb[b, b, :] = 1.0
    return cm, rm, selb, np.ones((1, NB), np.float32), cm2


def fm(v, n):
    return np.ascontiguousarray(np.asarray(v, np.float32).reshape(n, 128).T)


def prep_shared(inp, NB, GM, E, T=2048):
    f = lambda a: np.asarray(a, np.float32)
    cm, rm, selb, ones, cm2 = make_consts(NB, GM)
    w1 = f(inp["exp_w1"])[0][:E]
    w1 = np.ascontiguousarray(np.concatenate([w1[:, :, 0::2], w1[:, :, 1::2]], axis=2))
    b1 = f(inp["exp_b1"])[0][:E]
    b1d = np.concatenate([b1[:, 0::2], b1[:, 1::2]], axis=1)
    b1_rows = np.ascontiguousarray(b1d.reshape(E, 16, 128).transpose(0, 2, 1).reshape(E * 128, 16))
    NBLK = (NB * T * 4) // 512 + 32
    blkstart = np.ascontiguousarray(np.broadcast_to((np.arange(NBLK, dtype=np.float32) * 512.0)[None, :], (128, NBLK)))
    iotaP = np.ascontiguousarray((np.arange(8, dtype=np.float32)[None, :] * 128.0 + np.arange(128, dtype=np.float32)[:, None]))
    vecs = np.stack([fm(f(inp[n])[0], 4) for n in
                     ("rwkv_w0", "rwkv_a0", "rwkv_k_k", "rwkv_k_a", "rwkv_r_k", "rwkv_ln_w", "rwkv_ln_b")], axis=1)
    sh = {
        "ada_w": np.ascontiguousarray(f(inp["ada_w"])[0]),
        "ada_b_fm": fm(f(inp["ada_b"])[0], 48),
        "ada_b_row": np.ascontiguousarray(f(inp["ada_b"])[0][None, :]),
        "g12_fm": np.ascontiguousarray(np.stack([fm(f(inp["norm1_g"])[0], 8), fm(f(inp["norm2_g"])[0], 8)], axis=1)),
        "w_in": np.ascontiguousarray(f(inp["w_in"])[0]),
        "conv_w_fm": np.ascontiguousarray(f(inp["conv_w"])[0].reshape(3, 4, 128).transpose(2, 1, 0)),
        "conv_gn_fm": fm(f(inp["conv_gn"])[0], 4),
        "mu_fm": fm(f(inp["rwkv_mu"])[0], 14),
        "vecs_fm": np.ascontiguousarray(vecs),
        "lora_up": np.ascontiguousarray(np.concatenate([f(inp["rwkv_w_up"])[0], f(inp["rwkv_a_up"])[0]], axis=0)),
        "g_up": np.ascontiguousarray(f(inp["rwkv_g_up"])[0]),
        "w_out": np.ascontiguousarray(f(inp["w_out"])[0]),
        "router_w": np.ascontiguousarray(f(inp["router_w"])[0][:, :32]),
        "router_b_bc": np.ascontiguousarray(np.broadcast_to(f(inp["router_b"])[0][None, :32], (128, 32))),
        "w1": w1.reshape(E * D, 2 * D),
        "b1_rows": b1_rows,
        "blkstart": blkstart, "iotaP": iotaP,
        "g2_row": np.ascontiguousarray(np.broadcast_to(f(inp["norm2_g"])[0][None, :], (NB, D))),
        "w2": np.ascontiguousarray(f(inp["exp_w2"])[0][:E]).reshape(E * D, D),
        "b2": np.ascontiguousarray(f(inp["exp_b2"])[0][:E]),
        "final_g_bc": np.ascontiguousarray(np.broadcast_to(f(inp["final_g"])[None, :], (128, D))),
        "cmats": cm, "cmats2": cm2, "resetmask": rm,
        "zeros_blk": np.zeros((512, D), ml_dtypes.bfloat16), "selb": selb, "ones_row": ones,
    }
    return sh


def core_inputs(sh, x, c, b0, NB):
    m = dict(sh)
    m["x"] = np.ascontiguousarray(x[b0:b0 + NB])
    cc = np.asarray(c[b0:b0 + NB], np.float32)
    m["cT"] = np.ascontiguousarray(cc.reshape(NB, 8, 128).transpose(2, 1, 0))
    return m


def kernel(**inputs):
    x = np.asarray(inputs["x"], np.float32)
    c = np.asarray(inputs["c"], np.float32)
    B, T, _ = x.shape
    n = 8
    NB = B // n
    E = 32
    GM = 256
    sh = prep_shared(inputs, NB, GM, E, T)
    nc = build(T, NB, E, GM)
    in_maps = [core_inputs(sh, x, c, i * NB, NB) for i in range(n)]
    res = run_bass_kernel_spmd(nc, in_maps, core_ids=list(range(n)))
    return np.concatenate([r["out"] for r in res.results], axis=0).astype(np.float32)
```
